# Optimizing a Trainium2 kernel written in Bass

```python
import jax
import jax.numpy as jnp
from jax import lax
import numpy as np

D_MODEL = 4096
BATCH = 4
SEQ = 2048
DEPTH = 2

GRID_W = 64
CTX_LEN = 256
NORM_EPS = 1e-6
ATTN_HEAD_DIM = 128
ATTN_HEADS = D_MODEL // 2 // ATTN_HEAD_DIM
ATTN_KV_HEADS = ATTN_HEADS // 4
ATTN_WINDOW = 128
ATTN_BLOCK = 128
ROPE_THETA = 10000.0
MLSTM_HEADS = 4
MLSTM_V_DIM = D_MODEL // 2 // MLSTM_HEADS
MLSTM_QK_DIM = MLSTM_V_DIM // 2
MLSTM_CHUNK = 128
GLA_QK_DIM = 256
GLA_V_DIM = 512
GLA_HEADS = D_MODEL // GLA_V_DIM
GLA_GATE_RANK = 16
GLA_GATE_TEMP = 16.0
GLA_CHUNK = 64
D_FF = 11008
CONV_WIDTH = 3

AB_SPLITS = (ATTN_HEADS * ATTN_HEAD_DIM, ATTN_KV_HEADS * ATTN_HEAD_DIM, ATTN_KV_HEADS * ATTN_HEAD_DIM,
             MLSTM_HEADS * MLSTM_QK_DIM, MLSTM_HEADS * MLSTM_QK_DIM, MLSTM_HEADS * MLSTM_V_DIM,
             MLSTM_HEADS * MLSTM_V_DIM, 4 * MLSTM_HEADS)
AB_IN = sum(AB_SPLITS)
AB_OUT = ATTN_HEADS * ATTN_HEAD_DIM + MLSTM_HEADS * MLSTM_V_DIM
GLA_SPLITS = (GLA_HEADS * GLA_QK_DIM, GLA_HEADS * GLA_QK_DIM, GLA_HEADS * GLA_V_DIM, GLA_HEADS * GLA_V_DIM,
              GLA_GATE_RANK, GLA_GATE_RANK)
GLA_IN = sum(GLA_SPLITS)

kernel_name = 'hybrid_swa_mlstm_gla_convffn_prefix_dit'


def rmsnorm(x, w):
    xf = x.astype(jnp.float32)
    y = xf * lax.rsqrt(jnp.mean(xf * xf, axis=-1, keepdims=True) + NORM_EPS)
    return (y * w.astype(jnp.float32)).astype(x.dtype)


def modulate(h, shift, scale):
    return (h * (1 + scale) + shift).astype(h.dtype)


def split_cols(a, sizes):
    idx, acc = [], 0
    for s in sizes[:-1]:
        acc += s
        idx.append(acc)
    return jnp.split(a, idx, axis=-1)


def to_heads(a, n_heads):
    b, l, _ = a.shape
    return a.reshape(b, l, n_heads, -1).transpose(0, 2, 1, 3)


def axial_rope_tables(seq_len, head_dim):
    rows = seq_len // GRID_W
    row = jnp.repeat(jnp.arange(rows), GRID_W).astype(jnp.float32)
    col = jnp.tile(jnp.arange(GRID_W), rows).astype(jnp.float32)
    n_freq = head_dim // 4
    inv = ROPE_THETA ** (-jnp.arange(n_freq, dtype=jnp.float32) / n_freq)
    ang_r = row[:, None] * inv
    ang_c = col[:, None] * inv
    return (jnp.cos(ang_r), jnp.sin(ang_r), jnp.cos(ang_c), jnp.sin(ang_c))


def apply_axial_rope(x, tables):
    cr, sr, cc, sc = (t[None, :, None, :] for t in tables)
    xr, xcol = jnp.split(x.astype(jnp.float32), 2, axis=-1)

    def rot(z, cos, sin):
        z1, z2 = jnp.split(z, 2, axis=-1)
        return jnp.concatenate([z1 * cos - z2 * sin, z2 * cos + z1 * sin], axis=-1)

    return jnp.concatenate([rot(xr, cr, sr), rot(xcol, cc, sc)], axis=-1).astype(x.dtype)


def softmax_with_sink(logits, sink):
    m = jnp.maximum(logits.max(axis=-1, keepdims=True), sink)
    e = jnp.exp(logits - m)
    return e / (e.sum(axis=-1, keepdims=True) + jnp.exp(sink - m))


def window_attention(q, k, v, k_ctx, v_ctx, sink):
    b, l, h, dh = q.shape
    hkv = k.shape[2]
    g = h // hkv
    blk = ATTN_BLOCK
    nb = l // blk
    qb = q.reshape(b, nb, blk, hkv, g, dh)

    def band(a):
        ap = jnp.pad(a, ((0, 0), (blk, blk), (0, 0), (0, 0))).reshape(b, nb + 2, blk, hkv, dh)
        return jnp.concatenate([ap[:, :-2], ap[:, 1:-1], ap[:, 2:]], axis=2)

    kb, vb = band(k), band(v)
    qpos = jnp.arange(l).reshape(nb, blk)
    kpos = (jnp.arange(nb)[:, None] - 1) * blk + jnp.arange(3 * blk)[None, :]
    mask = ((jnp.abs(qpos[:, :, None] - kpos[:, None, :]) <= ATTN_WINDOW)
            & (kpos[:, None, :] >= 0) & (kpos[:, None, :] < l))
    scale = dh ** -0.5
    s_lat = jnp.einsum('bnqhgd,bnkhd->bnhgqk', qb, kb).astype(jnp.float32) * scale
    s_lat = jnp.where(mask[None, :, None, None], s_lat, -jnp.inf)
    s_ctx = jnp.einsum('bnqhgd,bchd->bnhgqc', qb, k_ctx).astype(jnp.float32) * scale
    sink_b = sink.astype(jnp.float32).reshape(hkv, g)[:, :, None, None]
    p = softmax_with_sink(jnp.concatenate([s_lat, s_ctx], axis=-1), sink_b)
    p_lat = p[..., :3 * blk].astype(v.dtype)
    p_ctx = p[..., 3 * blk:].astype(v.dtype)
    o = (jnp.einsum('bnhgqk,bnkhd->bnqhgd', p_lat, vb)
         + jnp.einsum('bnhgqc,bchd->bnqhgd', p_ctx, v_ctx))
    return o.reshape(b, l, h * dh)


def context_attention(q, k, v, sink):
    b, lc, h, dh = q.shape
    hkv = k.shape[2]
    g = h // hkv
    qg = q.reshape(b, lc, hkv, g, dh)
    s = jnp.einsum('bqhgd,bkhd->bhgqk', qg, k).astype(jnp.float32) * dh ** -0.5
    p = softmax_with_sink(s, sink.astype(jnp.float32).reshape(hkv, g)[:, :, None, None]).astype(v.dtype)
    return jnp.einsum('bhgqk,bkhd->bqhgd', p, v).reshape(b, lc, h * dh)


def mlstm_scan(q, k, v, ig, lf, state, chunk=MLSTM_CHUNK):
    q, k, v, ig, lf = (a.astype(jnp.float32) for a in (q, k, v, ig, lf))
    b, h, l, dk = q.shape
    dv = v.shape[-1]
    nc = l // chunk

    def to_chunks(a):
        return jnp.moveaxis(a.reshape(b, h, nc, chunk, *a.shape[3:]), 2, 0)

    causal = jnp.tril(jnp.ones((chunk, chunk), dtype=bool))

    def step(carry, inp):
        c_st, n_st, m_st = carry
        qb, kb, vb, ib, fb = inp
        bc = jnp.cumsum(fb, axis=-1)
        log_d = jnp.where(causal, bc[..., :, None] - bc[..., None, :] + ib[..., None, :], -jnp.inf)
        inter = bc + m_st[..., None]
        m_t = jnp.maximum(inter, log_d.max(axis=-1))
        s = jnp.einsum('bhtd,bhsd->bhts', qb, kb) * jnp.exp(log_d - m_t[..., None])
        w_inter = jnp.exp(inter - m_t)
        num = (jnp.einsum('bhts,bhsv->bhtv', s, vb)
               + w_inter[..., None] * jnp.einsum('bhtd,bhvd->bhtv', qb, c_st))
        den = s.sum(axis=-1) + w_inter * jnp.einsum('bhtd,bhd->bht', qb, n_st)
        h_t = num / jnp.maximum(jnp.abs(den), jnp.exp(-m_t))[..., None]
        b_end = bc[..., -1]
        m_new = m_t[..., -1]
        wk = jnp.exp(b_end[..., None] - bc + ib - m_new[..., None])
        decay = jnp.exp(b_end + m_st - m_new)
        c_new = decay[..., None, None] * c_st + jnp.einsum('bhsv,bhsd->bhvd', vb * wk[..., None], kb)
        n_new = decay[..., None] * n_st + jnp.einsum('bhs,bhsd->bhd', wk, kb)
        return (c_new, n_new, m_new), h_t

    state, hs = lax.scan(step, state, tuple(map(to_chunks, (q, k, v, ig, lf))))
    return jnp.moveaxis(hs, 0, 2).reshape(b, h, l, dv), state


def gla_scan(q, k, v, lg, state, chunk=GLA_CHUNK):
    q, k, v, lg = (a.astype(jnp.float32) for a in (q, k, v, lg))
    b, h, l, dk = q.shape
    dv = v.shape[-1]
    nc = l // chunk

    def to_chunks(a):
        return jnp.moveaxis(a.reshape(b, h, nc, chunk, a.shape[-1]), 2, 0)

    causal = jnp.tril(jnp.ones((chunk, chunk), dtype=bool))

    def step(s_st, inp):
        qb, kb, vb, gb = inp
        gcum = jnp.cumsum(gb, axis=2)
        q_dec = qb * jnp.exp(gcum)
        k_inv = kb * jnp.exp(-gcum)
        att = jnp.where(causal, jnp.einsum('bhtd,bhsd->bhts', q_dec, k_inv), 0.0)
        o = jnp.einsum('bhts,bhsv->bhtv', att, vb) + jnp.einsum('bhtd,bhdv->bhtv', q_dec, s_st)
        g_end = gcum[:, :, -1:, :]
        k_end = kb * jnp.exp(g_end - gcum)
        s_new = jnp.exp(g_end[:, :, 0, :])[..., None] * s_st + jnp.einsum('bhsd,bhsv->bhdv', k_end, vb)
        return s_new, o

    state, os_ = lax.scan(step, state, tuple(map(to_chunks, (q, k, v, lg))))
    return jnp.moveaxis(os_, 0, 2).reshape(b, h, l, dv), state


def two_segment_scan(scan_fn, ctx_in, lat_in, state0, reverse):
    flip = (lambda a: jnp.flip(a, axis=2)) if reverse else (lambda a: a)
    h_ctx, st = scan_fn(*map(flip, ctx_in), state0)
    h_lat, _ = scan_fn(*map(flip, lat_in), st)
    return flip(h_ctx), flip(h_lat)


def head_output(h, gain, gate):
    b, nh, l, dv = h.shape
    hn = h * lax.rsqrt(jnp.mean(h * h, axis=-1, keepdims=True) + NORM_EPS)
    hn = hn.transpose(0, 2, 1, 3).reshape(b, l, nh * dv)
    return hn * gain.astype(jnp.float32) * gate


def mlstm_gates(g, gate_b):
    b, l, _ = g.shape
    g = (g.astype(jnp.float32) + gate_b.astype(jnp.float32)).reshape(b, l, 4, MLSTM_HEADS).transpose(2, 0, 3, 1)
    return g[0], jax.nn.log_sigmoid(g[1]), g[2], jax.nn.log_sigmoid(g[3])


def mixer_attn_mlstm(h_lat, h_ctx, w_in, gate_b, sink, head_norm, w_out, rope, with_ctx):
    b, l, _ = h_lat.shape
    lc = h_ctx.shape[1]
    lat = split_cols(h_lat @ w_in, AB_SPLITS)
    cx = split_cols(h_ctx @ w_in, AB_SPLITS)

    def attn_qkv(parts, n):
        return (parts[0].reshape(b, n, ATTN_HEADS, ATTN_HEAD_DIM),
                parts[1].reshape(b, n, ATTN_KV_HEADS, ATTN_HEAD_DIM),
                parts[2].reshape(b, n, ATTN_KV_HEADS, ATTN_HEAD_DIM))

    qa_l, ka_l, va_l = attn_qkv(lat, l)
    qa_l = apply_axial_rope(qa_l, rope)
    ka_l = apply_axial_rope(ka_l, rope)
    qa_c, ka_c, va_c = attn_qkv(cx, lc)
    a_lat = window_attention(qa_l, ka_l, va_l, ka_c, va_c, sink)

    def mlstm_qkv(parts):
        return (to_heads(parts[3], MLSTM_HEADS),
                to_heads(parts[4], MLSTM_HEADS) * MLSTM_QK_DIM ** -0.5,
                to_heads(parts[5], MLSTM_HEADS))

    qkv_c, qkv_l = mlstm_qkv(cx), mlstm_qkv(lat)
    if_c, lf_c, ib_c, lb_c = mlstm_gates(cx[7], gate_b)
    if_l, lf_l, ib_l, lb_l = mlstm_gates(lat[7], gate_b)
    zero = jnp.zeros((b, MLSTM_HEADS), jnp.float32)
    state0 = (jnp.zeros((b, MLSTM_HEADS, MLSTM_V_DIM, MLSTM_QK_DIM), jnp.float32),
              jnp.zeros((b, MLSTM_HEADS, MLSTM_QK_DIM), jnp.float32), zero)
    hc_f, hl_f = two_segment_scan(mlstm_scan, (*qkv_c, if_c, lf_c), (*qkv_l, if_l, lf_l), state0, False)
    hc_b, hl_b = two_segment_scan(mlstm_scan, (*qkv_c, ib_c, lb_c), (*qkv_l, ib_l, lb_l), state0, True)
    m_lat = head_output(hl_f + hl_b, head_norm, jax.nn.sigmoid(lat[6].astype(jnp.float32)))
    y_lat = jnp.concatenate([a_lat, m_lat.astype(a_lat.dtype)], axis=-1) @ w_out
    if not with_ctx:
        return y_lat, None
    a_ctx = context_attention(qa_c, ka_c, va_c, sink)
    m_ctx = head_output(hc_f + hc_b, head_norm, jax.nn.sigmoid(cx[6].astype(jnp.float32)))
    y_ctx = jnp.concatenate([a_ctx, m_ctx.astype(a_ctx.dtype)], axis=-1) @ w_out
    return y_lat, y_ctx


def mixer_gla(h_lat, h_ctx, w_in, gate_w2, gate_b, head_norm, w_out, with_ctx):
    b = h_lat.shape[0]
    lat = split_cols(h_lat @ w_in, GLA_SPLITS)
    cx = split_cols(h_ctx @ w_in, GLA_SPLITS)

    def qkv_gates(parts):
        q = to_heads(parts[0], GLA_HEADS).astype(jnp.float32) * GLA_QK_DIM ** -0.5
        k = to_heads(parts[1], GLA_HEADS)
        v = to_heads(parts[2], GLA_HEADS)
        lg = [to_heads(jax.nn.log_sigmoid(parts[4 + d].astype(jnp.float32) @ gate_w2[d] + gate_b[d]) / GLA_GATE_TEMP,
                       GLA_HEADS) for d in range(2)]
        return (q, k, v), lg

    qkv_c, lg_c = qkv_gates(cx)
    qkv_l, lg_l = qkv_gates(lat)
    state0 = jnp.zeros((b, GLA_HEADS, GLA_QK_DIM, GLA_V_DIM), jnp.float32)
    oc_f, ol_f = two_segment_scan(gla_scan, (*qkv_c, lg_c[0]), (*qkv_l, lg_l[0]), state0, False)
    oc_b, ol_b = two_segment_scan(gla_scan, (*qkv_c, lg_c[1]), (*qkv_l, lg_l[1]), state0, True)
    y_lat = head_output(ol_f + ol_b, head_norm, jax.nn.silu(lat[3].astype(jnp.float32))).astype(h_lat.dtype) @ w_out
    if not with_ctx:
        return y_lat, None
    y_ctx = head_output(oc_f + oc_b, head_norm, jax.nn.silu(cx[3].astype(jnp.float32))).astype(h_ctx.dtype) @ w_out
    return y_lat, y_ctx


def conv_ffn(h, w_up, conv_w, conv_b, w_down):
    u = h @ w_up
    l = u.shape[1]
    pad = CONV_WIDTH // 2
    up = jnp.pad(u, ((0, 0), (pad, pad), (0, 0)))
    u = conv_b + sum(up[:, j:j + l] * conv_w[j] for j in range(CONV_WIDTH))
    gate, val = jnp.split(u, 2, axis=-1)
    return (jax.nn.silu(gate) * val) @ w_down


def setup_inputs(seed: int = 0) -> dict:
    key = jax.random.key(seed)
    ks = jax.random.split(key, 24)
    n_even = (DEPTH + 1) // 2
    n_odd = DEPTH // 2
    d = D_MODEL

    def nrm(k, shape, scale):
        return jax.random.normal(k, shape, jnp.float32) * scale

    return {
        'x': nrm(ks[0], (BATCH, SEQ, d), 1.0),
        'c': nrm(ks[1], (BATCH, d), 1.0),
        'ctx': nrm(ks[2], (BATCH, CTX_LEN, d), 1.0),
        'c_ctx': nrm(ks[3], (d,), 1.0),
        'ada_w': nrm(ks[4], (DEPTH, d, 6 * d), d ** -0.5),
        'ada_b': nrm(ks[5], (DEPTH, 6 * d), 0.01),
        'norm_w': 1.0 + nrm(ks[6], (DEPTH, 4, d), 0.02),
        'ab_w_in': nrm(ks[7], (n_even, d, AB_IN), d ** -0.5),
        'ab_gate_b': jnp.repeat(jnp.array([0.0, 3.0, 0.0, 3.0], jnp.float32), MLSTM_HEADS)
                     + nrm(ks[8], (n_even, 4 * MLSTM_HEADS), 0.1),
        'ab_sink': nrm(ks[9], (n_even, ATTN_HEADS), 0.5),
        'ab_head_norm': 1.0 + nrm(ks[10], (n_even, MLSTM_HEADS * MLSTM_V_DIM), 0.02),
        'ab_w_out': nrm(ks[11], (n_even, AB_OUT, d), AB_OUT ** -0.5),
        'gla_w_in': nrm(ks[12], (n_odd, d, GLA_IN), d ** -0.5),
        'gla_gate_w2': nrm(ks[13], (n_odd, 2, GLA_GATE_RANK, GLA_HEADS * GLA_QK_DIM), GLA_GATE_RANK ** -0.5),
        'gla_gate_b': nrm(ks[14], (n_odd, 2, GLA_HEADS * GLA_QK_DIM), 0.1),
        'gla_head_norm': 1.0 + nrm(ks[15], (n_odd, GLA_HEADS * GLA_V_DIM), 0.02),
        'gla_w_out': nrm(ks[16], (n_odd, GLA_HEADS * GLA_V_DIM, d), (GLA_HEADS * GLA_V_DIM) ** -0.5),
        'ffn_w_up': nrm(ks[17], (DEPTH, d, 2 * D_FF), d ** -0.5),
        'ffn_conv_w': nrm(ks[18], (DEPTH, CONV_WIDTH, 2 * D_FF), CONV_WIDTH ** -0.5),
        'ffn_conv_b': nrm(ks[19], (DEPTH, 2 * D_FF), 0.01),
        'ffn_w_down': nrm(ks[20], (DEPTH, D_FF, d), D_FF ** -0.5),
    }


def reference(x, c, ctx, c_ctx, ada_w, ada_b, norm_w, ab_w_in, ab_gate_b, ab_sink, ab_head_norm, ab_w_out,
              gla_w_in, gla_gate_w2, gla_gate_b, gla_head_norm, gla_w_out,
              ffn_w_up, ffn_conv_w, ffn_conv_b, ffn_w_down):
    rope = axial_rope_tables(x.shape[1], ATTN_HEAD_DIM)
    silu_c = jax.nn.silu(c)
    silu_cc = jax.nn.silu(c_ctx)
    for i in range(DEPTH):
        last = i == DEPTH - 1
        j = i // 2
        mod_l = jnp.split((silu_c @ ada_w[i] + ada_b[i])[:, None, :], 6, axis=-1)
        mod_c = jnp.split(silu_cc @ ada_w[i] + ada_b[i], 6, axis=-1)
        nw = norm_w[i]
        h_l = modulate(rmsnorm(x, nw[0]), mod_l[0], mod_l[1])
        h_c = modulate(rmsnorm(ctx, nw[0]), mod_c[0], mod_c[1])
        if i % 2 == 0:
            y_l, y_c = mixer_attn_mlstm(h_l, h_c, ab_w_in[j], ab_gate_b[j], ab_sink[j], ab_head_norm[j],
                                        ab_w_out[j], rope, not last)
        else:
            y_l, y_c = mixer_gla(h_l, h_c, gla_w_in[j], gla_gate_w2[j], gla_gate_b[j], gla_head_norm[j],
                                 gla_w_out[j], not last)
        x = x + mod_l[2] * rmsnorm(y_l, nw[1])
        f_l = conv_ffn(modulate(rmsnorm(x, nw[2]), mod_l[3], mod_l[4]),
                       ffn_w_up[i], ffn_conv_w[i], ffn_conv_b[i], ffn_w_down[i])
        x = x + mod_l[5] * rmsnorm(f_l, nw[3])
        if not last:
            ctx = ctx + mod_c[2] * rmsnorm(y_c, nw[1])
            f_c = conv_ffn(modulate(rmsnorm(ctx, nw[2]), mod_c[3], mod_c[4]),
                           ffn_w_up[i], ffn_conv_w[i], ffn_conv_b[i], ffn_w_down[i])
            ctx = ctx + mod_c[5] * rmsnorm(f_c, nw[3])
    return x
```

```python
import numpy as np
import ml_dtypes
import concourse.bass as bass
import concourse.mybir as mybir
from concourse.bass_utils import run_bass_kernel_spmd

F32 = mybir.dt.float32
BF16 = mybir.dt.bfloat16
AF = mybir.ActivationFunctionType
ALU = mybir.AluOpType
AX = mybir.AxisListType

D = 4096
NDC = 32
LC = 256
LL = 2048
T = LC + LL
NB = T // 128
DFF = 11008
NFC = DFF // 128
EPS = 1e-6
TT = [(0, 256), (256, 768), (768, 1280), (1280, 1792), (1792, 2304)]
AB_IN = 9232
GLA_IN = 12320
AB_G = 37
GLA_G = 49


class Sem:
    def __init__(self, nc, name):
        self.h = nc.alloc_semaphore(name)
        self.v = 0
        self.name = name


class Buf:
    __slots__ = ("name", "w", "r", "multi")

    def __init__(self, name, multi=False):
        self.name = name
        self.w = {}
        self.r = {}
        self.multi = multi


class Eng:
    def __init__(self, kb, name, e):
        self.kb = kb
        self.name = name
        self.e = e
        self.sem = Sem(kb.nc, "p_" + name)
        self.seen = {}
        self.nsem = 0

    def tick(self):
        if self.sem.v >= 60000:
            self.nsem += 1
            self.sem = Sem(self.kb.nc, "p_%s%d" % (self.name, self.nsem))
        self.sem.v += 1
        return (self.sem, self.sem.v)


class KB:
    def __init__(self, nc):
        self.nc = nc
        self.pe = Eng(self, "pe", nc.tensor)
        self.act = Eng(self, "act", nc.scalar)
        self.dve = Eng(self, "dve", nc.vector)
        self.pool = Eng(self, "pool", nc.gpsimd)
        self.sp = Eng(self, "sp", nc.sync)
        self.engs = [self.pe, self.act, self.dve, self.pool, self.sp]
        self.dsems = []
        self.free_sems = []
        self.cur_phase = None
        self.phase_stack = []
        self.nbuf = 0

    def buf(self, name=None, multi=False):
        self.nbuf += 1
        return Buf(name or ("b%d" % self.nbuf), multi)

    def dsem(self, name):
        if self.free_sems:
            s = self.free_sems.pop()
        else:
            s = Sem(self.nc, "d_%d" % len(self.dsems))
            self.dsems.append(s)
        if self.cur_phase is not None:
            self.cur_phase.sems.append(s)
        return s

    def _waits(self, eng, reads, writes):
        need = {}
        for b in reads:
            for s, v in b.w.items():
                if need.get(s, 0) < v:
                    need[s] = v
        for b in writes:
            if not b.multi:
                for s, v in b.w.items():
                    if need.get(s, 0) < v:
                        need[s] = v
            for s, v in b.r.items():
                if need.get(s, 0) < v:
                    need[s] = v
        for s, v in need.items():
            if eng.seen.get(s, 0) < v:
                eng.e.wait_ge(s.h, v)
                eng.seen[s] = v

    def _mark(self, tok, reads, writes):
        s, v = tok
        for b in writes:
            if b.multi:
                if b.w.get(s, 0) < v:
                    b.w[s] = v
            else:
                b.w = {s: v}
                b.r = {}
        for b in reads:
            if b.r.get(s, 0) < v:
                b.r[s] = v

    def op(self, eng, fn, reads=(), writes=()):
        self._waits(eng, reads, writes)
        ins = fn(eng.e)
        tok = eng.tick()
        ins.then_inc(tok[0].h, 1)
        self._mark(tok, reads, writes)
        return ins

    def mm(self, out_ap, pairs, reads, writes, fp32=False):
        eng = self.pe
        self._waits(eng, reads, writes)
        n = len(pairs)
        ins = None
        for i, (l, r) in enumerate(pairs):
            ins = eng.e.matmul(out_ap, lhsT=l, rhs=r, start=(i == 0), stop=(i == n - 1))
        tok = eng.tick()
        ins.then_inc(tok[0].h, 1)
        self._mark(tok, reads, writes)

    def mm_multi(self, outs, nk, lhs_fn, rhs_fn, reads, writes):
        eng = self.pe
        self._waits(eng, reads, writes)
        ins = None
        for kc in range(nk):
            l = lhs_fn(kc)
            for ti, o in enumerate(outs):
                ins = eng.e.matmul(o, lhsT=l, rhs=rhs_fn(kc, ti), start=(kc == 0), stop=(kc == nk - 1))
        tok = eng.tick()
        ins.then_inc(tok[0].h, 1)
        self._mark(tok, reads, writes)

    def dma(self, q, out_ap, in_ap, sem, reads=(), writes=()):
        self._waits(q, reads, writes)
        ins = q.e.dma_start(out=out_ap, in_=in_ap)
        sem.v += 16
        ins.then_inc(sem.h, 16)
        self._mark((sem, sem.v), reads, writes)

    def barrier(self):
        toks = {}
        for e in self.engs:
            if e.sem.v > 0:
                toks[e.sem] = e.sem.v
        for s in self.dsems:
            if s.v > 0:
                toks[s] = s.v
        for e in self.engs:
            for s, v in toks.items():
                if s is e.sem:
                    continue
                if e.seen.get(s, 0) < v:
                    e.e.wait_ge(s.h, v)
                    e.seen[s] = v


class Ring:
    def __init__(self, kb, name, tiles, with_sem=False, multi=False):
        self.tiles = tiles
        self.bufs = [kb.buf("%s%d" % (name, i), multi) for i in range(len(tiles))]
        self.sems = [kb.dsem("%s%d" % (name, i)) for i in range(len(tiles))] if with_sem else None
        self.i = -1

    def next(self):
        self.i = (self.i + 1) % len(self.tiles)
        if self.sems:
            return self.tiles[self.i], self.bufs[self.i], self.sems[self.i]
        return self.tiles[self.i], self.bufs[self.i]


class Phase:
    def __init__(self, kb, name):
        self.kb = kb
        self.name = name
        self.guards = []
        self.sems = []
        kb.phase_stack.append(self)
        kb.cur_phase = self

    def sb(self, name, shape, dt):
        g = self.kb.nc.sbuf_tensor(self.name + "_" + name, shape, dt)
        t = g.__enter__()
        self.guards.append(g)
        return t

    def ps(self, name, shape, dt=F32):
        g = self.kb.nc.psum_tensor(self.name + "_" + name, shape, dt)
        t = g.__enter__()
        self.guards.append(g)
        return t

    def close(self):
        self.kb.barrier()
        for g in reversed(self.guards):
            g.__exit__(None, None, None)
        self.guards = []
        self.kb.free_sems.extend(self.sems)
        self.sems = []
        assert self.kb.phase_stack[-1] is self
        self.kb.phase_stack.pop()
        self.kb.cur_phase = self.kb.phase_stack[-1] if self.kb.phase_stack else None


def cls_cols(t0, t1):
    out = []
    if t0 < LC:
        out.append((1, t0, min(t1, LC)))
    if t1 > LC:
        out.append((0, max(t0, LC), t1))
    return out


class Prog:
    LEVELS = {"ab_gate_b": 2, "ab_sink": 2, "ab_head_norm": 2, "ab_w_out": 3, "ffn_w_up": 4, "ffn_cw": 4,
              "ffn_cb": 4, "ffn_w_down": 4, "gla_w_in": 5, "gla_gw2": 5, "gla_head_norm": 5, "gla_w_out": 5}

    def __init__(self, nc, level=5, dbg=()):
        self.nc = nc
        self.kb = KB(nc)
        self.level = level
        self.dbg = dbg
        self.in_names = []
        kb = self.kb
        dt = nc.dram_tensor

        def din(name, shape, dtp=F32):
            if self.LEVELS.get(name, 1) > level:
                return None
            self.in_names.append(name)
            return dt(name, list(shape), dtp, kind="ExternalInput").ap()

        def dscr(name, shape, dtp):
            return dt(name, list(shape), dtp).ap()

        self.xT = din("xT", [NDC, 128, T])
        self.csT = din("csT", [128, NDC, 2])
        self.ada_w = din("ada_w", [2, D, 6 * D])
        self.ada_b = din("ada_b", [2, 128, 192])
        self.norm_w = din("norm_w", [2, 128, 4, NDC])
        self.ab_w_in = din("ab_w_in", [AB_G, 128, NDC, 256])
        self.ab_gate_b = din("ab_gate_b", [1, 16])
        self.ab_sink = din("ab_sink", [1, 16])
        self.ab_head_norm = din("ab_head_norm", [1, 2048])
        self.ab_w_out = din("ab_w_out", [NDC, 128, NDC, 128])
        self.gla_w_in = din("gla_w_in", [GLA_G, 128, NDC, 256])
        self.gla_gw2 = din("gla_gw2", [2, 17, 2048])
        self.gla_head_norm = din("gla_head_norm", [1, 4096])
        self.gla_w_out = din("gla_w_out", [NDC, 128, NDC, 128])
        self.ffn_w_up = din("ffn_w_up", [2, 2 * NFC, 128, NDC, 128])
        self.ffn_cw = din("ffn_cw", [2, 128, 3, 2 * NFC])
        self.ffn_cb = din("ffn_cb", [2, 128, 2 * NFC])
        self.ffn_w_down = din("ffn_w_down", [2, NDC, 128, NFC, 128])
        self.ropeC = din("ropeC", [128, LL])
        self.ropeS = din("ropeS", [128, LL])
        self.cmat = din("cmat", [6, 128, 128])
        self.out = dt("out", [NDC, 128, LL], F32, kind="ExternalOutput").ap()
        self.xA = dscr("xA", [NDC, 128, T], F32)
        self.xB = dscr("xB", [NDC, 128, T], F32)
        self.yscr = dscr("yscr", [NDC, 128, T], F32)
        self.projFM = dscr("projFM", [40, 128, T], BF16)
        self.projTM = dscr("projTM", [32, T, 256], BF16)
        self.gatesTM = dscr("gatesTM", [T, 16], F32)
        self.rlowFM = dscr("rlowFM", [32, T], F32)
        self.mixTM = dscr("mixTM", [T, D], BF16)
        self.actscr = dscr("actscr", [NFC, 128, T], BF16)
        self.dbgout = {}
        for name, shape, dtp in dbg:
            self.dbgout[name] = dt("dbg_" + name, list(shape), dtp, kind="ExternalOutput").ap()

        a = nc.alloc_sbuf_tensor
        self.cm = a("cm", [128, 6, 128], F32)
        self.cmb = a("cmb", [128, 6, 128], BF16)
        self.mods = a("mods", [128, 2, 192, 2], F32)
        self.nw = a("nw", [128, 2, 4, NDC], F32)
        self.der = a("der", [128, 2, 6, NDC, 2], F32)
        self.sc = a("sc", [128, NDC, 2], F32)
        self.scb = a("scb", [128, NDC, 2], BF16)
        self.adab = a("adab", [128, 2, 192], F32)
        self.b_cs = kb.buf("cs")
        self.b_adab = kb.buf("adab")
        self.b_nw = kb.buf("nw")
        self.b_cm = kb.buf("cm")
        self.b_mods = kb.buf("mods", multi=True)
        self.b_der = kb.buf("der")
        self.s_misc = kb.dsem("misc")
        self.s_out = kb.dsem("out")
        self.b_scr = {}

    def sbuf_of(self, name):
        if name not in self.b_scr:
            self.b_scr[name] = self.kb.buf("scr_" + name, multi=True)
        return self.b_scr[name]

    def ada_group(self, l, g, wr, psr, ptr, rowr):
        kb = self.kb
        w, bw, sw = wr.next()
        kb.dma(kb.pool, w[:], self.ada_w[l].rearrange("(kc p) n -> p kc n", p=128)[:, :, g * 512:(g + 1) * 512], sw, writes=[bw])
        p, bp = psr.next()
        kb.mm(p[0:2, :], [(self.scb[:, kc, :], w[:, kc, :]) for kc in range(NDC)], reads=[bw, self.b_cs], writes=[bp])
        row, brow_ = rowr.next()
        kb.op(kb.act, lambda e: e.activation(out=row[:], in_=p[0:2, :], func=AF.Copy), reads=[bp], writes=[brow_])
        pt, bpt = ptr.next()
        for m in range(4):
            kb.mm(pt[:, 2 * m:2 * m + 2], [(row[0:2, m * 128:(m + 1) * 128], self.cm[0:2, 0, 0:2])], reads=[brow_, self.b_cm], writes=[bpt])
        for cl in range(2):
            kb.op(kb.dve, lambda e, cl=cl: e.tensor_tensor(
                out=self.mods[:, l, g * 4:(g + 1) * 4, cl], in0=pt[:, 0:8].rearrange("p (j c) -> p j c", c=2)[:, :, cl],
                in1=self.adab[:, l, g * 4:(g + 1) * 4], op=ALU.add), reads=[bpt, self.b_adab], writes=[self.b_mods])

    def ada_derived(self, l):
        kb = self.kb
        for cl in range(2):
            md = lambda w: self.mods[:, l, w * 32:(w + 1) * 32, cl]
            dr = lambda k: self.der[:, l, k, :, cl]
            ops = [
                lambda e: e.scalar_tensor_tensor(out=dr(0), in0=md(1), scalar=1.0, in1=self.nw[:, l, 0, :], op0=ALU.add, op1=ALU.mult),
                lambda e: e.tensor_copy(out=dr(1), in_=md(0)),
                lambda e: e.tensor_tensor(out=dr(2), in0=md(2), in1=self.nw[:, l, 1, :], op=ALU.mult),
                lambda e: e.scalar_tensor_tensor(out=dr(3), in0=md(4), scalar=1.0, in1=self.nw[:, l, 2, :], op0=ALU.add, op1=ALU.mult),
                lambda e: e.tensor_copy(out=dr(4), in_=md(3)),
                lambda e: e.tensor_tensor(out=dr(5), in0=md(5), in1=self.nw[:, l, 3, :], op=ALU.mult),
            ]
            for f in ops:
                kb.op(kb.dve, f, reads=[self.b_mods, self.b_nw], writes=[self.b_der])

    def phase_consts_ada(self):
        kb, nc = self.kb, self.nc
        ph = Phase(kb, "ada")
        kb.dma(kb.sp, self.cm[:], self.cmat.rearrange("k p n -> p k n"), kb.dsem('m'), writes=[self.b_cm])
        kb.op(kb.dve, lambda e: e.tensor_copy(out=self.cmb[:], in_=self.cm[:]), reads=[self.b_cm], writes=[self.b_cm])
        kb.dma(kb.sp, self.nw[:], self.norm_w.rearrange("l p f d -> p l f d"), kb.dsem('m'), writes=[self.b_nw])
        kb.dma(kb.sp, self.sc[:], self.csT, kb.dsem('m'), writes=[self.b_cs])
        kb.dma(kb.sp, self.adab[:], self.ada_b.rearrange("l p j -> p l j"), kb.dsem('m'), writes=[self.b_adab])
        kb.op(kb.act, lambda e: e.activation(out=self.sc[:], in_=self.sc[:], func=AF.Silu), reads=[self.b_cs], writes=[self.b_cs])
        kb.op(kb.dve, lambda e: e.tensor_copy(out=self.scb[:], in_=self.sc[:]), reads=[self.b_cs], writes=[self.b_cs])
        wr = Ring(kb, "adaw", [ph.sb("w%d" % i, [128, NDC, 512], BF16) for i in range(3)], with_sem=True)
        psr = Ring(kb, "adap", [ph.ps("p%d" % i, [128, 512], F32) for i in range(2)])
        ptr = Ring(kb, "adat", [ph.ps("t%d" % i, [128, 512], F32) for i in range(2)])
        rowr = Ring(kb, "adar", [ph.sb("row%d" % i, [2, 512], F32) for i in range(2)])
        for g in range(48):
            self.ada_group(0, g, wr, psr, ptr, rowr)
        self.ada_derived(0)
        ph.close()

    def norm_mod(self, ph, xsrc, b_x, l, kg, ksh, hT, b_hT, lo, hi):
        kb = self.kb
        n = hi - lo
        xr = Ring(kb, "nx", [ph.sb("nx%d" % i, [128, n], F32) for i in range(2)], with_sem=True)
        acc = ph.sb("nacc", [128, n], F32)
        tmp = ph.sb("ntmp", [128, n], F32)
        rstd = ph.sb("nrstd", [128, n], F32)
        b_acc, b_tmp, b_rstd = kb.buf("nacc"), kb.buf("ntmp"), kb.buf("nrstd")
        pss = [ph.ps("nps%d" % i, [128, 512], F32) for i in range(2)]
        b_pss = [kb.buf("nps%d" % i) for i in range(2)]
        for dc in range(NDC):
            x, bx, sx = xr.next()
            kb.dma(kb.sp, x[:], xsrc[dc][:, lo:hi], sx, reads=[b_x], writes=[bx])
            if dc == 0:
                kb.op(kb.dve, lambda e: e.tensor_tensor(out=acc[:], in0=x[:], in1=x[:], op=ALU.mult), reads=[bx], writes=[b_acc])
            else:
                kb.op(kb.act, lambda e: e.activation(out=tmp[:], in_=x[:], func=AF.Square), reads=[bx], writes=[b_tmp])
                kb.op(kb.dve, lambda e: e.tensor_tensor(out=acc[:], in0=acc[:], in1=tmp[:], op=ALU.add), reads=[b_tmp], writes=[b_acc])
        i = 0
        for c0 in range(0, n, 512):
            c1 = min(n, c0 + 512)
            p = pss[i % 2]
            kb.mm(p[:, 0:c1 - c0], [(self.cm[:, 1, :], acc[:, c0:c1])], reads=[self.b_cm, b_acc], writes=[b_pss[i % 2]])
            kb.op(kb.act, lambda e: e.activation(out=tmp[:, c0:c1], in_=p[:, 0:c1 - c0], func=AF.Sqrt, scale=1.0 / D, bias=EPS),
                  reads=[b_pss[i % 2]], writes=[b_tmp])
            i += 1
        kb.op(kb.dve, lambda e: e.reciprocal(out=rstd[:], in_=tmp[:]), reads=[b_tmp], writes=[b_rstd])
        for dc in range(NDC):
            x, bx, sx = xr.next()
            kb.dma(kb.sp, x[:], xsrc[dc][:, lo:hi], sx, reads=[b_x], writes=[bx])
            kb.op(kb.dve, lambda e: e.tensor_tensor(out=tmp[:], in0=x[:], in1=rstd[:], op=ALU.mult), reads=[bx, b_rstd], writes=[b_tmp])
            for cl, a0, a1 in cls_cols(lo, hi):
                kb.op(kb.act, lambda e: e.activation(out=hT[:, dc, a0 - lo:a1 - lo], in_=tmp[:, a0 - lo:a1 - lo], func=AF.Identity,
                                                     scale=self.der[:, l, kg, dc, cl:cl + 1], bias=self.der[:, l, ksh, dc, cl:cl + 1]),
                      reads=[b_tmp, self.b_der], writes=[b_hT[dc]])

    def phase_inproj(self, l, xsrc, b_x):
        kb = self.kb
        w_dram = self.ab_w_in if l == 0 else self.gla_w_in
        ngroups = AB_G if l == 0 else GLA_G
        if l == 0:
            def plan(g):
                if g < 8: return ("FM", 2 * g, 128 ** -0.5, True)
                if g < 10: return ("FM", 16 + 2 * (g - 8), 1.0, True)
                if g < 12: return ("TM", g - 10, 1.0, False)
                if g < 16: return ("FM", 20 + 2 * (g - 12), 1.0, False)
                if g < 20: return ("FM", 28 + 2 * (g - 16), 1.0 / 16, False)
                if g < 36: return ("TM", 2 + (g - 20), 1.0, False)
                return ("GT", 0, 1.0, False)
        else:
            def plan(g):
                if g < 8: return ("FM", 2 * g, 1.0 / 16, False)
                if g < 16: return ("FM", 16 + 2 * (g - 8), 1.0, False)
                if g < 48: return ("TM", g - 16, 1.0, False)
                return ("GF", 0, 1.0, False)
        b_pFM = self.sbuf_of("projFM")
        b_pTM = self.sbuf_of("projTM")
        b_gT = self.sbuf_of("gatesTM")
        b_rl = self.sbuf_of("rlowFM")
        HALVES = [(0, 1152, [(0, 256), (256, 768), (768, 1152)]), (1152, 2304, [(1152, 1664), (1664, 2176), (2176, 2304)])]
        for hi_, (lo, hi, tiles) in enumerate(HALVES):
            nh = hi - lo
            nbh = nh // 128
            ph = Phase(kb, "inp%d_%d" % (l, hi_))
            hT = ph.sb("hT", [128, NDC, nh], BF16)
            b_hT = [kb.buf("hT%d" % i) for i in range(NDC)]
            ph2 = Phase(kb, "inpn%d_%d" % (l, hi_))
            self.norm_mod(ph2, xsrc, b_x, l, 0, 1, hT, b_hT, lo, hi)
            ph2.close()
            tg = "%d_%d" % (l, hi_)
            wr = Ring(kb, "inw" + tg, [ph.sb("w%d" % i, [128, NDC, 256], BF16) for i in range(2)], with_sem=True)
            psr = Ring(kb, "inps" + tg, [ph.ps("p%d" % i, [128, 512], F32) for i in range(5)])
            psq = Ring(kb, "inpq" + tg, [ph.ps("q%d" % i, [128, 512], F32) for i in range(1)])
            psT = Ring(kb, "inpT" + tg, [ph.ps("T%d" % i, [128, 512], BF16) for i in range(2)])
            fmr = Ring(kb, "infm" + tg, [ph.sb("fm%d" % i, [128, nh], BF16) for i in range(4)], multi=True)
            stF = Ring(kb, "stF" + tg, [ph.sb("stF%d" % i, [128, nh], BF16) for i in range(2)], with_sem=True, multi=True)
            stT = Ring(kb, "stT" + tg, [ph.sb("stT%d" % i, [128, nbh, 256], BF16) for i in range(2)], with_sem=True, multi=True)
            stG = ph.sb("stG", [128, nbh, 64], F32)
            b_stG = kb.buf("stG", multi=True)
            s_stG = kb.dsem("stG" + tg)
            rC = Ring(kb, "rC" + tg, [ph.sb("rC%d" % i, [128, 512], F32) for i in range(2)], with_sem=True)
            rS = Ring(kb, "rS" + tg, [ph.sb("rS%d" % i, [128, 512], F32) for i in range(2)], with_sem=True)
            qb_r = Ring(kb, "qb" + tg, [ph.sb("qb%d" % i, [128, 512], BF16) for i in range(2)])
            t1_r = Ring(kb, "t1" + tg, [ph.sb("t1%d" % i, [128, 512], F32) for i in range(2)])
            t2_r = Ring(kb, "t2" + tg, [ph.sb("t2%d" % i, [128, 512], F32) for i in range(2)])
            stGf = stG[0:32, :, :].rearrange("p a b -> p (a b)")
            for g in range(ngroups):
                form, dst, scale, rope = plan(g)
                w, bw, sw = wr.next()
                kb.dma(kb.pool, w[:], w_dram[g], sw, writes=[bw])
                if form in ("FM", "GF"):
                    for m in range(2 if form == "FM" else 1):
                        if form == "FM":
                            st, bst, sst = stF.next()
                        pbs = [psr.next() for _ in tiles]
                        kb.mm_multi([pb[0][:, 0:t1 - t0] for pb, (t0, t1) in zip(pbs, tiles)], NDC,
                                    lambda kc: w[:, kc, m * 128:(m + 1) * 128],
                                    lambda kc, ti: hT[:, kc, tiles[ti][0] - lo:tiles[ti][1] - lo],
                                    reads=[bw] + b_hT, writes=[pb[1] for pb in pbs])
                        for ti, (t0, t1) in enumerate(tiles):
                            n = t1 - t0
                            p, bp = pbs[ti]
                            if form == "GF":
                                kb.op(kb.act, lambda e: e.activation(out=stGf[:, 0:n], in_=p[0:32, 0:n], func=AF.Copy),
                                      reads=[bp], writes=[b_stG])
                                kb.dma(kb.sp, self.rlowFM[:, t0:t1], stGf[:, 0:n], s_stG, reads=[b_stG], writes=[b_rl])
                                continue
                            if rope and t0 >= LC:
                                qb, bqb = qb_r.next()
                                kb.op(kb.act, lambda e: e.activation(out=qb[:, 0:n], in_=p[:, 0:n], func=AF.Copy, scale=scale),
                                      reads=[bp], writes=[bqb])
                                c_, bc_, sc_ = rC.next()
                                s_, bs_, ss_ = rS.next()
                                kb.dma(kb.sp, c_[:, 0:n], self.ropeC[:, t0 - LC:t1 - LC], sc_, writes=[bc_])
                                kb.dma(kb.sp, s_[:, 0:n], self.ropeS[:, t0 - LC:t1 - LC], ss_, writes=[bs_])
                                q2, bq2 = psq.next()
                                kb.mm(q2[:, 0:n], [(self.cmb[:, 4, :], qb[:, 0:n])], reads=[bqb, self.b_cm], writes=[bq2])
                                a1, ba1 = t1_r.next()
                                a2, ba2 = t2_r.next()
                                kb.op(kb.dve, lambda e: e.tensor_tensor(out=a1[:, 0:n], in0=qb[:, 0:n], in1=c_[:, 0:n], op=ALU.mult),
                                      reads=[bqb, bc_], writes=[ba1])
                                kb.op(kb.dve, lambda e: e.tensor_tensor(out=a2[:, 0:n], in0=q2[:, 0:n], in1=s_[:, 0:n], op=ALU.mult),
                                      reads=[bq2, bs_], writes=[ba2])
                                kb.op(kb.dve, lambda e: e.tensor_tensor(out=st[:, t0 - lo:t1 - lo], in0=a1[:, 0:n], in1=a2[:, 0:n], op=ALU.add),
                                      reads=[ba1, ba2], writes=[bst])
                            else:
                                kb.op(kb.act, lambda e: e.activation(out=st[:, t0 - lo:t1 - lo], in_=p[:, 0:n], func=AF.Copy, scale=scale),
                                      reads=[bp], writes=[bst])
                        if form == "FM":
                            kb.dma(kb.sp, self.projFM[dst + m][:, lo:hi], st[:], sst, reads=[bst], writes=[b_pFM])
                elif form == "TM":
                    st, bst, sst = stT.next()
                    fms = []
                    for m in range(2):
                        pbs = [psr.next() for _ in tiles]
                        kb.mm_multi([pb[0][:, 0:t1 - t0] for pb, (t0, t1) in zip(pbs, tiles)], NDC,
                                    lambda kc: w[:, kc, m * 128:(m + 1) * 128],
                                    lambda kc, ti: hT[:, kc, tiles[ti][0] - lo:tiles[ti][1] - lo],
                                    reads=[bw] + b_hT, writes=[pb[1] for pb in pbs])
                        fm, bfm = fmr.next()
                        for ti, (t0, t1) in enumerate(tiles):
                            p, bp = pbs[ti]
                            if ti % 2 == 0:
                                kb.op(kb.act, lambda e: e.activation(out=fm[:, t0 - lo:t1 - lo], in_=p[:, 0:t1 - t0], func=AF.Copy), reads=[bp], writes=[bfm])
                            else:
                                kb.op(kb.dve, lambda e: e.tensor_copy(out=fm[:, t0 - lo:t1 - lo], in_=p[:, 0:t1 - t0]), reads=[bp], writes=[bfm])
                        fms.append((fm, bfm))
                    for tb in range(nbh):
                        T_, bT = psT.next()
                        for m in range(2):
                            fm, bfm = fms[m]
                            kb.op(kb.pe, lambda e: e.transpose(out=T_[:, m * 128:(m + 1) * 128], in_=fm[:, tb * 128:(tb + 1) * 128], identity=self.cmb[:, 0, :]),
                                  reads=[bfm, self.b_cm], writes=[bT])
                        if tb % 2 == 0:
                            kb.op(kb.act, lambda e: e.activation(out=st[:, tb, :], in_=T_[:, 0:256], func=AF.Copy), reads=[bT], writes=[bst])
                        else:
                            kb.op(kb.dve, lambda e: e.tensor_copy(out=st[:, tb, :], in_=T_[:, 0:256]), reads=[bT], writes=[bst])
                    kb.dma(kb.sp, self.projTM[dst][lo:hi].rearrange("(tb p) c -> p tb c", p=128), st[:], sst, reads=[bst], writes=[b_pTM])
                else:
                    for tb in range(nbh):
                        p, bp = psr.next()
                        kb.mm(p[:, 0:256], [(hT[:, kc, tb * 128:(tb + 1) * 128], w[:, kc, :]) for kc in range(NDC)],
                              reads=[bw] + b_hT, writes=[bp])
                        if form == "TM":
                            if tb % 2 == 0:
                                kb.op(kb.act, lambda e: e.activation(out=st[:, tb, :], in_=p[:, 0:256], func=AF.Copy), reads=[bp], writes=[bst])
                            else:
                                kb.op(kb.dve, lambda e: e.tensor_copy(out=st[:, tb, :], in_=p[:, 0:256]), reads=[bp], writes=[bst])
                        else:
                            kb.op(kb.act, lambda e: e.activation(out=stG[:, tb, 0:16], in_=p[:, 0:16], func=AF.Copy), reads=[bp], writes=[b_stG])
                    if form == "TM":
                        kb.dma(kb.sp, self.projTM[dst][lo:hi].rearrange("(tb p) c -> p tb c", p=128), st[:], sst, reads=[bst], writes=[b_pTM])
                    else:
                        kb.dma(kb.sp, self.gatesTM[lo:hi].rearrange("(tb p) c -> p tb c", p=128), stG[:, :, 0:16], s_stG, reads=[b_stG], writes=[b_gT])
            ph.close()

    def phase_mix_ab(self):
        kb = self.kb
        b_pFM, b_pTM, b_gT, b_mix = (self.sbuf_of(n) for n in ("projFM", "projTM", "gatesTM", "mixTM"))
        ph = Phase(kb, "att")
        kT = ph.sb("kT", [128, T], BF16); b_kT = kb.buf("kT"); s_kT = kb.dsem("kT")
        va = ph.sb("va", [128, NB, 132], BF16); b_va = kb.buf("va"); s_va = kb.dsem("va")
        qr = Ring(kb, "aq", [ph.sb("q%d" % i, [128, T], BF16) for i in range(2)], with_sem=True)
        ost = Ring(kb, "aost", [ph.sb("ost%d" % i, [128, NB, 512], BF16) for i in range(2)], with_sem=True, multi=True)
        esk = ph.sb("esk", [128, 16], F32); b_esk = kb.buf("esk")
        kb.dma(kb.sp, esk[:], self.ab_sink.partition_broadcast(128), kb.dsem('m'), writes=[b_esk])
        kb.op(kb.act, lambda e: e.activation(out=esk[:], in_=esk[:], func=AF.Exp), reads=[b_esk], writes=[b_esk])
        psS = Ring(kb, "aS", [ph.ps("S%d" % i, [128, 512], F32) for i in range(4)])
        psN = Ring(kb, "aN", [ph.ps("N%d" % i, [128, 512], F32) for i in range(2)])
        PT = Ring(kb, "aPT", [ph.sb("PT%d" % i, [128, 5, 128], BF16) for i in range(3)])
        rr = Ring(kb, "arr", [ph.sb("rr%d" % i, [128, 2], F32) for i in range(3)])
        a_wr = Ring(kb, "adaw1", [ph.sb("aw%d" % i, [128, NDC, 512], BF16) for i in range(2)], with_sem=True)
        a_psr = Ring(kb, "adap1", [ph.ps("ap%d" % i, [128, 512], F32) for i in range(1)])
        a_ptr = Ring(kb, "adat1", [ph.ps("at%d" % i, [128, 512], F32) for i in range(1)])
        a_rowr = Ring(kb, "adar1", [ph.sb("arow%d" % i, [2, 512], F32) for i in range(2)])
        ada_g = 0
        for kvh in range(4):
            kb.dma(kb.sp, kT[:], self.projFM[16 + kvh], s_kT, reads=[b_pFM], writes=[b_kT])
            kb.op(kb.pool, lambda e: e.memset(va[:, :, 128:129], 1.0), writes=[b_va])
            kb.dma(kb.sp, va[:, :, 0:128], self.projTM[kvh // 2].rearrange("(tb p) c -> p tb c", p=128)[:, :, (kvh % 2) * 128:(kvh % 2 + 1) * 128],
                   s_va, reads=[b_pTM], writes=[b_va])
            o_t, b_o, s_o = ost.next()
            for hq in range(4):
                head = kvh * 4 + hq
                q, bq, sq = qr.next()
                kb.dma(kb.sp, q[:], self.projFM[head], sq, reads=[b_pFM], writes=[bq])
                for i in range(NB):
                    if i < 2:
                        js = [(0, None), (1, None)]
                    else:
                        js = [(0, None), (1, None)]
                        if i - 1 >= 2: js.append((i - 1, 3))
                        js.append((i, None))
                        if i + 1 < NB: js.append((i + 1, 2))
                    pt, bpt = PT.next()
                    for c0 in range(0, len(js), 4):
                        part = js[c0:c0 + 4]
                        S, bS = psS.next()
                        for jj, (j, mk) in enumerate(part):
                            kb.mm(S[:, jj * 128:(jj + 1) * 128], [(kT[:, j * 128:(j + 1) * 128], q[:, i * 128:(i + 1) * 128])],
                                  reads=[b_kT, bq], writes=[bS])
                        npc = len(part)
                        kb.op(kb.act, lambda e: e.activation(out=pt[:, c0:c0 + npc, :].rearrange("p a b -> p (a b)"), in_=S[:, 0:npc * 128], func=AF.Exp),
                              reads=[bS], writes=[bpt])
                    for jj, (j, mk) in enumerate(js):
                        if mk is not None:
                            kb.op(kb.dve, lambda e: e.tensor_tensor(out=pt[:, jj, :], in0=pt[:, jj, :], in1=self.cmb[:, mk, :], op=ALU.mult),
                                  reads=[bpt, self.b_cm], writes=[bpt])
                    N_, bN = psN.next()
                    kb.mm(N_[:, 0:129], [(pt[:, jj, :], va[:, j, 0:129]) for jj, (j, mk) in enumerate(js)], reads=[bpt, b_va], writes=[bN])
                    r_, br = rr.next()
                    kb.op(kb.dve, lambda e: e.tensor_scalar(out=r_[:, 0:1], in0=N_[:, 128:129], scalar1=esk[:, head:head + 1], scalar2=None, op0=ALU.add),
                          reads=[bN, b_esk], writes=[br])
                    kb.op(kb.dve, lambda e: e.reciprocal(out=r_[:, 1:2], in_=r_[:, 0:1]), reads=[br], writes=[br])
                    kb.op(kb.act, lambda e: e.activation(out=o_t[:, i, hq * 128:(hq + 1) * 128], in_=N_[:, 0:128], func=AF.Copy, scale=r_[:, 1:2]),
                          reads=[bN, br], writes=[b_o])
                    if i % 6 == 5 and ada_g < 48:
                        self.ada_group(1, ada_g, a_wr, a_psr, a_ptr, a_rowr)
                        ada_g += 1
            kb.dma(kb.sp, self.mixTM.rearrange("(tb p) c -> p tb c", p=128)[:, :, kvh * 512:(kvh + 1) * 512], o_t[:], s_o, reads=[b_o], writes=[b_mix])
        while ada_g < 48:
            self.ada_group(1, ada_g, a_wr, a_psr, a_ptr, a_rowr)
            ada_g += 1
        self.ada_derived(1)
        ph.close()
        ph = Phase(kb, "mls")
        G = ph.sb("G", [128, NB, 16], F32); b_G = kb.buf("G")
        gb = ph.sb("gb", [128, 16], F32); b_gb = kb.buf("gb")
        sp_ = ph.sb("sp", [128, NB, 16], F32); b_sp = kb.buf("sp")
        csum = ph.sb("csum", [128, NB, 8], F32); b_cs = kb.buf("csum")
        cb = ph.sb("cb", [128, NB, 8], F32); b_cb = kb.buf("cb")
        gain = ph.sb("gain", [128, 2048], F32); b_gain = kb.buf("gain")
        kb.dma(kb.sp, G[:], self.gatesTM.rearrange("(tb p) c -> p tb c", p=128), kb.dsem('m'), reads=[b_gT], writes=[b_G])
        kb.dma(kb.sp, gb[:], self.ab_gate_b.partition_broadcast(128), kb.dsem('m'), writes=[b_gb])
        kb.dma(kb.sp, gain[:], self.ab_head_norm.partition_broadcast(128), kb.dsem('m'), writes=[b_gain])
        for tb in range(NB):
            kb.op(kb.dve, lambda e: e.tensor_tensor(out=G[:, tb, :], in0=G[:, tb, :], in1=gb[:], op=ALU.add), reads=[b_gb, b_G], writes=[b_G])
        kb.op(kb.act, lambda e: e.activation(out=sp_[:], in_=G[:], func=AF.Exp, scale=-1.0), reads=[b_G], writes=[b_sp])
        kb.op(kb.act, lambda e: e.activation(out=sp_[:], in_=sp_[:], func=AF.Ln, bias=1.0), reads=[b_sp], writes=[b_sp])
        ORD = [list(range(NB)), [1, 0] + list(range(NB - 1, 1, -1))]
        MK = [2, 3]
        psC = ph.ps("C", [128, 512], F32); b_psC = kb.buf("psC")
        for d in range(2):
            for pos, i in enumerate(ORD[d]):
                pairs = [(self.cm[:, 1, :], sp_[:, j, 4 + 8 * d:8 + 8 * d]) for j in ORD[d][:pos]]
                pairs.append((self.cm[:, MK[d], :], sp_[:, i, 4 + 8 * d:8 + 8 * d]))
                kb.mm(psC[:, 0:4], pairs, reads=[b_sp, self.b_cm], writes=[b_psC])
                kb.op(kb.dve, lambda e: e.tensor_copy(out=csum[:, i, 4 * d:4 * d + 4], in_=psC[:, 0:4]), reads=[b_psC], writes=[b_cs])
                kb.op(kb.dve, lambda e: e.tensor_tensor(out=cb[:, i, 4 * d:4 * d + 4], in0=psC[:, 0:4], in1=G[:, i, 8 * d:8 * d + 4], op=ALU.add),
                      reads=[b_psC, b_G], writes=[b_cb])
        qT = ph.sb("qT", [128, 2, T], BF16); b_qT = kb.buf("qT"); s_qT = kb.dsem("qT")
        kT = ph.sb("kT", [128, 2, T], BF16); b_kT = kb.buf("kT"); s_kT = kb.dsem("kT")
        v = ph.sb("v", [128, NB, 512], BF16); b_v = kb.buf("v"); s_v = kb.dsem("v")
        og = ph.sb("og", [128, NB, 512], BF16); b_og = kb.buf("og"); s_og = kb.dsem("og")
        hsum = ph.sb("hsum", [128, NB, 512], F32); b_hs = [kb.buf("hs%d" % i) for i in range(NB)]
        brow = ph.sb("brow", [128, NB, 128], F32); b_brow = [kb.buf("brow%d" % i) for i in range(NB)]
        dg = Ring(kb, "dg", [ph.sb("dg%d" % i, [128, 128], F32) for i in range(2)])
        ost = ph.sb("ost", [128, NB, 512], BF16); b_ost = kb.buf("most", multi=True); s_ost = kb.dsem("most")
        ss = ph.sb("ss", [128, NB], F32); b_ss = kb.buf("ss")
        rs = ph.sb("rs", [128, NB], F32); b_rs = kb.buf("rs")
        junk = ph.sb("junk", [128, 512], F32); b_junk = kb.buf("junk")
        tmpo = Ring(kb, "tmpo", [ph.sb("tmpo%d" % i, [128, 512], F32) for i in range(2)])
        DT = Ring(kb, "DT", [ph.sb("DT%d" % i, [128, 4, 128], F32) for i in range(3)])
        PT = Ring(kb, "mPT", [ph.sb("PT%d" % i, [128, 4, 128], BF16) for i in range(3)])
        psS = Ring(kb, "mS", [ph.ps("S%d" % i, [128, 512], F32) for i in range(3)])
        psN = Ring(kb, "mN", [ph.ps("N%d" % i, [128, 512], F32) for i in range(2)])
        psD = Ring(kb, "mD", [ph.ps("D%d" % i, [128, 512], F32) for i in range(2)])
        rr = Ring(kb, "mrr", [ph.sb("rr%d" % i, [128, 2], F32) for i in range(3)])
        onesb = self.cmb[:, 1, 0:1]
        for h in range(4):
            for c in range(2):
                kb.dma(kb.sp, qT[:, c, :], self.projFM[20 + 2 * h + c], s_qT, reads=[b_pFM], writes=[b_qT])
                kb.dma(kb.sp, kT[:, c, :], self.projFM[28 + 2 * h + c], s_kT, reads=[b_pFM], writes=[b_kT])
                kb.dma(kb.sp, v[:, :, c * 256:(c + 1) * 256], self.projTM[2 + 2 * h + c].rearrange("(tb p) c -> p tb c", p=128), s_v,
                       reads=[b_pTM], writes=[b_v])
                kb.dma(kb.sp, og[:, :, c * 256:(c + 1) * 256], self.projTM[10 + 2 * h + c].rearrange("(tb p) c -> p tb c", p=128), s_og,
                       reads=[b_pTM], writes=[b_og])
            kb.op(kb.act, lambda e: e.activation(out=og[:], in_=og[:], func=AF.Sigmoid), reads=[b_og], writes=[b_og])
            for d in range(2):
                col = 4 * d + h
                for i in range(NB):
                    dt_, bdt = dg.next()
                    kb.op(kb.dve, lambda e: e.tensor_scalar(out=dt_[:], in0=self.cm[:, 0, :], scalar1=csum[:, i, col:col + 1], scalar2=None, op0=ALU.mult),
                          reads=[b_cs, self.b_cm], writes=[bdt])
                    kb.mm(psC[:, 0:128], [(self.cm[:, 1, :], dt_[:])], reads=[bdt, self.b_cm], writes=[b_psC])
                    kb.op(kb.act, lambda e: e.activation(out=brow[:, i, :], in_=psC[:, 0:128], func=AF.Copy), reads=[b_psC], writes=[b_brow[i]])
                for pos, i in enumerate(ORD[d]):
                    js = ORD[d][:pos + 1]
                    N_, bN = psN.next()
                    Dn, bDn = psD.next()
                    ngr = (len(js) + 3) // 4
                    for gi in range(ngr):
                        part = js[gi * 4:gi * 4 + 4]
                        npc = len(part)
                        S, bS = psS.next()
                        D_, bD = DT.next()
                        P_, bP = PT.next()
                        for jj, j in enumerate(part):
                            kb.mm(S[:, jj * 128:(jj + 1) * 128],
                                  [(kT[:, c, j * 128:(j + 1) * 128], qT[:, c, i * 128:(i + 1) * 128]) for c in range(2)],
                                  reads=[b_kT, b_qT], writes=[bS])
                            kb.op(kb.act, lambda e: e.activation(out=D_[:, jj, :], in_=brow[:, i, :], func=AF.Exp, scale=-1.0, bias=cb[:, j, col:col + 1]),
                                  reads=[b_brow[i], b_cb], writes=[bD])
                            if j == i:
                                kb.op(kb.pool, lambda e: e.tensor_tensor(out=D_[:, jj, :], in0=D_[:, jj, :], in1=self.cm[:, MK[d], :], op=ALU.mult),
                                      reads=[bD, self.b_cm], writes=[bD])
                        kb.op(kb.dve, lambda e: e.tensor_tensor(out=P_[:, 0:npc, :].rearrange("p a b -> p (a b)"), in0=S[:, 0:npc * 128],
                                                               in1=D_[:, 0:npc, :].rearrange("p a b -> p (a b)"), op=ALU.mult),
                              reads=[bS, bD], writes=[bP])
                        first = (gi == 0)
                        last = (gi == ngr - 1)
                        kb._waits(kb.pe, [bP, b_v], [bN, bDn] if first else [])
                        ins = None
                        for jj, j in enumerate(part):
                            kb.pe.e.matmul(N_[:, 0:512], lhsT=P_[:, jj, :], rhs=v[:, j, :], start=(first and jj == 0), stop=(last and jj == npc - 1))
                            ins = kb.pe.e.matmul(Dn[:, 0:1], lhsT=P_[:, jj, :], rhs=onesb, start=(first and jj == 0), stop=(last and jj == npc - 1))
                        tok = kb.pe.tick()
                        ins.then_inc(tok[0].h, 1)
                        kb._mark(tok, [bP, b_v], [bN, bDn])
                    r_, br = rr.next()
                    kb.op(kb.act, lambda e: e.activation(out=r_[:, 0:1], in_=Dn[:, 0:1], func=AF.Abs), reads=[bDn], writes=[br])
                    kb.op(kb.dve, lambda e: e.tensor_scalar_max(out=r_[:, 0:1], in0=r_[:, 0:1], scalar1=1.0), reads=[br], writes=[br])
                    kb.op(kb.dve, lambda e: e.reciprocal(out=r_[:, 1:2], in_=r_[:, 0:1]), reads=[br], writes=[br])
                    if d == 0:
                        kb.op(kb.act, lambda e: e.activation(out=hsum[:, i, :], in_=N_[:, 0:512], func=AF.Copy, scale=r_[:, 1:2]),
                              reads=[bN, br], writes=[b_hs[i]])
                    else:
                        kb.op(kb.dve, lambda e: e.scalar_tensor_tensor(out=hsum[:, i, :], in0=N_[:, 0:512], scalar=r_[:, 1:2], in1=hsum[:, i, :],
                                                                        op0=ALU.mult, op1=ALU.add), reads=[bN, br, b_hs[i]], writes=[b_hs[i]])
            kb.op(kb.dve, lambda e: e.memset(ss[:], 0.0), writes=[b_ss])
            for i in range(NB):
                kb.op(kb.act, lambda e: e.activation(out=junk[:], in_=hsum[:, i, :], func=AF.Square, accum_out=ss[:, i:i + 1]),
                      reads=[b_hs[i]], writes=[b_junk, b_ss])
            kb.op(kb.act, lambda e: e.activation(out=rs[:], in_=ss[:], func=AF.Sqrt, scale=1.0 / 512, bias=EPS), reads=[b_ss], writes=[b_rs])
            kb.op(kb.dve, lambda e: e.reciprocal(out=rs[:], in_=rs[:]), reads=[b_rs], writes=[b_rs])
            for i in range(NB):
                t_, bt = tmpo.next()
                kb.op(kb.dve, lambda e: e.scalar_tensor_tensor(out=t_[:], in0=hsum[:, i, :], scalar=rs[:, i:i + 1], in1=gain[:, h * 512:(h + 1) * 512],
                                                                op0=ALU.mult, op1=ALU.mult), reads=[b_hs[i], b_rs, b_gain], writes=[bt])
                kb.op(kb.pool, lambda e: e.tensor_tensor(out=ost[:, i, :], in0=t_[:], in1=og[:, i, :], op=ALU.mult), reads=[bt, b_og], writes=[b_ost])
            kb.dma(kb.sp, self.mixTM.rearrange("(tb p) c -> p tb c", p=128)[:, :, 2048 + h * 512:2048 + (h + 1) * 512], ost[:], s_ost,
                   reads=[b_ost], writes=[b_mix])
        ph.close()

    def phase_mix_gla(self):
        kb = self.kb
        b_pFM, b_pTM, b_rl, b_mix = (self.sbuf_of(n) for n in ("projFM", "projTM", "rlowFM", "mixTM"))
        ph = Phase(kb, "gla")
        ORD = [list(range(NB)), [1, 0] + list(range(NB - 1, 1, -1))]
        MK = [2, 3]
        raug = ph.sb("raug", [32, T], F32); b_raug = kb.buf("raug"); s_raug = kb.dsem("raug")
        gw = ph.sb("gw", [32, 256], F32); b_gw = kb.buf("gw"); s_gw = kb.dsem("gw")
        sp_ = ph.sb("sp", [128, NB, 256], F32); b_sp = [kb.buf("gsp%d" % i) for i in range(NB)]
        Gp = ph.sb("Gp", [128, 2, T], F32); b_Gp = kb.buf("Gp", multi=True)
        qT = ph.sb("qT", [128, 2, T], BF16); b_qT = kb.buf("gqT", multi=True); s_qT = kb.dsem("gqT")
        kT = ph.sb("kT", [128, 2, T], BF16); b_kT = kb.buf("gkT", multi=True); s_kT = kb.dsem("gkT")
        qd = ph.sb("qd", [128, 2, T], BF16); b_qd = kb.buf("qd", multi=True)
        ke = ph.sb("ke", [128, 2, T], BF16); b_ke = kb.buf("ke", multi=True)
        ki = ph.sb("ki", [128, 2, T], BF16); b_ki = kb.buf("ki", multi=True)
        v = ph.sb("v", [128, NB, 512], BF16); b_v = kb.buf("gv", multi=True); s_v = kb.dsem("gv")
        rg = ph.sb("rg", [128, NB, 512], BF16); b_rg = kb.buf("grg", multi=True); s_rg = kb.dsem("grg")
        hsum = ph.sb("hsum", [128, NB, 512], F32); b_hs = [kb.buf("ghs%d" % i) for i in range(NB)]
        gain = ph.sb("gain", [128, 512], F32); b_gain = kb.buf("ggain"); s_gain = kb.dsem("ggain")
        g16 = ph.sb("g16", [128, 4, 2, NB], F32); b_g16 = kb.buf("g16")
        gam = ph.sb("gam", [128, 2, NB], F32); b_gam = kb.buf("gam")
        gtm = ph.sb("gtm", [128, 2, NB], F32); b_gtm = kb.buf("gtm")
        ss = ph.sb("ss", [128, NB], F32); b_ss = kb.buf("gss")
        rs = ph.sb("rs", [128, NB], F32); b_rs = kb.buf("grs")
        junk = ph.sb("junk", [128, 512], F32); b_junk = kb.buf("gjunk")
        Er = Ring(kb, "gE", [ph.sb("E%d" % i, [128, 128], F32) for i in range(3)])
        PT = Ring(kb, "gPT", [ph.sb("PT%d" % i, [128, 128], BF16) for i in range(3)])
        keTr = Ring(kb, "gkeT", [ph.sb("keT%d" % i, [128, 256], BF16) for i in range(2)])
        S_f = ph.sb("S_f", [128, 2, 512], F32); S_b = ph.sb("S_b", [128, 2, 512], BF16)
        b_Sf = kb.buf("S_f"); b_Sb = kb.buf("S_b")
        gmm = ph.sb("gmm", [128, 2, NB], F32); b_gmm = kb.buf("gmm")
        tot = ph.sb("tot", [128, 2, NB], F32); gsr = ph.sb("gsr", [128, 2, NB], F32); b_tot = kb.buf("tot")
        tmpo = Ring(kb, "gtmpo", [ph.sb("tmpo%d" % i, [128, 512], F32) for i in range(2)])
        ost = Ring(kb, "gost", [ph.sb("ost%d" % i, [128, 512], BF16) for i in range(2)], with_sem=True)
        psZ = Ring(kb, "gZ", [ph.ps("Z%d" % i, [128, 512], F32) for i in range(2)])
        psS = Ring(kb, "gS", [ph.ps("S%d" % i, [128, 512], F32) for i in range(2)])
        psT = Ring(kb, "gT", [ph.ps("T%d" % i, [128, 512], BF16) for i in range(1)])
        psN = Ring(kb, "gN", [ph.ps("N%d" % i, [128, 512], F32) for i in range(2)])
        mixv = self.mixTM.rearrange("(tb p) c -> p tb c", p=128)
        for h in range(8):
            for c in range(2):
                kb.dma(kb.sp, v[:, :, c * 256:(c + 1) * 256], self.projTM[2 * h + c].rearrange("(tb p) c -> p tb c", p=128), s_v, reads=[b_pTM], writes=[b_v])
                kb.dma(kb.sp, rg[:, :, c * 256:(c + 1) * 256], self.projTM[16 + 2 * h + c].rearrange("(tb p) c -> p tb c", p=128), s_rg, reads=[b_pTM], writes=[b_rg])
            kb.op(kb.act, lambda e: e.activation(out=rg[:], in_=rg[:], func=AF.Silu), reads=[b_rg], writes=[b_rg])
            kb.dma(kb.sp, gain[:], self.gla_head_norm[:, h * 512:(h + 1) * 512].partition_broadcast(128), s_gain, writes=[b_gain])
            for d in range(2):
                kb.op(kb.dve, lambda e: e.memset(raug[:], 1.0), writes=[b_raug])
                kb.dma(kb.sp, raug[0:16, :], self.rlowFM[16 * d:16 * d + 16, :], s_raug, reads=[b_rl], writes=[b_raug])
                kb.dma(kb.sp, gw[0:17, :], self.gla_gw2[d][:, h * 256:(h + 1) * 256], s_gw, writes=[b_gw])
                for i in range(NB):
                    Z, bZ = psZ.next()
                    kb.mm(Z[:, 0:256], [(raug[0:17, i * 128:(i + 1) * 128], gw[0:17, :])], reads=[b_raug, b_gw], writes=[bZ])
                    kb.op(kb.act, lambda e: e.activation(out=sp_[:, i, :], in_=Z[:, 0:256], func=AF.Exp, scale=-1.0), reads=[bZ], writes=[b_sp[i]])
                    kb.op(kb.act, lambda e: e.activation(out=sp_[:, i, :], in_=sp_[:, i, :], func=AF.Ln, bias=1.0), reads=[b_sp[i]], writes=[b_sp[i]])
                for c in range(2):
                    Z, bZ = psZ.next()
                    for i in range(NB):
                        kb.mm(Z[:, i:i + 1], [(sp_[:, i, c * 128:(c + 1) * 128], self.cm[:, 1, 0:1])], reads=[b_sp[i], self.b_cm], writes=[bZ])
                    kb.op(kb.dve, lambda e: e.tensor_copy(out=tot[:, c, :], in_=Z[:, 0:NB]), reads=[bZ], writes=[b_tot])
                    prev = None
                    for i in ORD[d]:
                        if prev is None:
                            kb.op(kb.dve, lambda e: e.memset(gsr[:, c, i:i + 1], 0.0), writes=[b_tot])
                        else:
                            kb.op(kb.dve, lambda e: e.tensor_tensor(out=gsr[:, c, i:i + 1], in0=gsr[:, c, prev:prev + 1], in1=tot[:, c, prev:prev + 1], op=ALU.add),
                                  reads=[b_tot], writes=[b_tot])
                        prev = i
                kb.op(kb.dve, lambda e: e.tensor_scalar(out=g16[:, 0, :, :], in0=gsr[:], scalar1=1.0 / 16, scalar2=None, op0=ALU.mult), reads=[b_tot], writes=[b_g16])
                kb.op(kb.dve, lambda e: e.tensor_tensor(out=tot[:], in0=tot[:], in1=gsr[:], op=ALU.add), reads=[b_tot], writes=[b_tot])
                kb.op(kb.dve, lambda e: e.tensor_scalar(out=g16[:, 1, :, :], in0=tot[:], scalar1=1.0 / 16, scalar2=None, op0=ALU.mult), reads=[b_tot], writes=[b_g16])
                for c in range(2):
                    for i in range(NB):
                        Z, bZ = psZ.next()
                        kb.mm(Z[:, 0:128], [(sp_[:, i, c * 128:(c + 1) * 128], self.cm[:, MK[d], :])], reads=[b_sp[i], self.b_cm], writes=[bZ])
                        dst = Gp[:, c, i * 128:(i + 1) * 128]
                        if i % 2 == 0:
                            kb.op(kb.dve, lambda e: e.tensor_scalar(out=dst, in0=Z[:, 0:128], scalar1=gsr[:, c, i:i + 1], scalar2=None, op0=ALU.add),
                                  reads=[bZ, b_tot], writes=[b_Gp])
                        else:
                            kb.op(kb.act, lambda e: e.activation(out=dst, in_=Z[:, 0:128], func=AF.Identity, bias=gsr[:, c, i:i + 1]),
                                  reads=[bZ, b_tot], writes=[b_Gp])
                kb.op(kb.dve, lambda e: e.tensor_scalar(out=g16[:, 2:4, :, :], in0=g16[:, 0:2, :, :], scalar1=-1.0, scalar2=None, op0=ALU.mult),
                      reads=[b_g16], writes=[b_g16])
                kb.op(kb.dve, lambda e: e.tensor_tensor(out=gmm[:], in0=g16[:, 0, :, :], in1=g16[:, 1, :, :], op=ALU.subtract), reads=[b_g16], writes=[b_gmm])
                kb.op(kb.act, lambda e: e.activation(out=gmm[:], in_=gmm[:], func=AF.Exp), reads=[b_gmm], writes=[b_gmm])
                for c in range(2):
                    kb.dma(kb.sp, qT[:, c, :], self.projFM[2 * h + c], s_qT, reads=[b_pFM], writes=[b_qT])
                    kb.dma(kb.sp, kT[:, c, :], self.projFM[16 + 2 * h + c], s_kT, reads=[b_pFM], writes=[b_kT])
                for c in range(2):
                    for i in range(NB):
                        blk = slice(i * 128, (i + 1) * 128)
                        for which, src, dstt, bd, sc_, bias in ((0, qT, qd, b_qd, -1.0 / 16, g16[:, 0, c, i:i + 1]),
                                                               (1, kT, ke, b_ke, 1.0 / 16, g16[:, 3, c, i:i + 1]),
                                                               (2, kT, ki, b_ki, 1.0 / 16, g16[:, 2, c, i:i + 1])):
                            E, bE = Er.next()
                            kb.op(kb.act, lambda e: e.activation(out=E[:], in_=Gp[:, c, blk], func=AF.Exp, scale=sc_, bias=bias),
                                  reads=[b_Gp, b_g16], writes=[bE])
                            kb.op(kb.dve, lambda e: e.tensor_tensor(out=dstt[:, c, blk], in0=src[:, c, blk], in1=E[:], op=ALU.mult),
                                  reads=[bE, b_qT if which == 0 else b_kT], writes=[bd])
                kb.op(kb.dve, lambda e: e.memset(S_f[:], 0.0), writes=[b_Sf])
                for pos, i in enumerate(ORD[d]):
                    blk = slice(i * 128, (i + 1) * 128)
                    S, bS = psS.next()
                    kb.mm(S[:, 0:128], [(ki[:, c, blk], qd[:, c, blk]) for c in range(2)], reads=[b_ki, b_qd], writes=[bS])
                    P_, bP = PT.next()
                    kb.op(kb.dve, lambda e: e.tensor_tensor(out=P_[:], in0=S[:, 0:128], in1=self.cm[:, MK[d], :], op=ALU.mult),
                          reads=[bS, self.b_cm], writes=[bP])
                    N_, bN = psN.next()
                    pairs = [(P_[:], v[:, i, :])]
                    rd = [bP, b_v]
                    if pos > 0:
                        pairs += [(qd[:, c, blk], S_b[:, c, :]) for c in range(2)]
                        rd += [b_qd, b_Sb]
                    kb.mm(N_[:, 0:512], pairs, reads=rd, writes=[bN])
                    if d == 0:
                        kb.op(kb.act, lambda e: e.activation(out=hsum[:, i, :], in_=N_[:, 0:512], func=AF.Copy), reads=[bN], writes=[b_hs[i]])
                    else:
                        kb.op(kb.dve, lambda e: e.tensor_tensor(out=hsum[:, i, :], in0=N_[:, 0:512], in1=hsum[:, i, :], op=ALU.add),
                              reads=[bN, b_hs[i]], writes=[b_hs[i]])
                    if pos < NB - 1:
                        T_, bT = psT.next()
                        for c in range(2):
                            kb.op(kb.pe, lambda e: e.transpose(out=T_[:, c * 128:(c + 1) * 128], in_=ke[:, c, blk], identity=self.cmb[:, 0, :]),
                                  reads=[b_ke, self.b_cm], writes=[bT])
                        keT, bkeT = keTr.next()
                        kb.op(kb.act, lambda e: e.activation(out=keT[:], in_=T_[:, 0:256], func=AF.Copy), reads=[bT], writes=[bkeT])
                        for c in range(2):
                            U, bU = psZ.next()
                            kb.mm(U[:, 0:512], [(keT[:, c * 128:(c + 1) * 128], v[:, i, :])], reads=[bkeT, b_v], writes=[bU])
                            kb.op(kb.dve, lambda e: e.scalar_tensor_tensor(out=S_f[:, c, :], in0=S_f[:, c, :], scalar=gmm[:, c, i:i + 1], in1=U[:, 0:512],
                                                                            op0=ALU.mult, op1=ALU.add), reads=[bU, b_gmm, b_Sf], writes=[b_Sf])
                            kb.op(kb.act, lambda e: e.activation(out=S_b[:, c, :], in_=S_f[:, c, :], func=AF.Copy), reads=[b_Sf], writes=[b_Sb])
            kb.op(kb.dve, lambda e: e.memset(ss[:], 0.0), writes=[b_ss])
            for i in range(NB):
                kb.op(kb.act, lambda e: e.activation(out=junk[:], in_=hsum[:, i, :], func=AF.Square, accum_out=ss[:, i:i + 1]),
                      reads=[b_hs[i]], writes=[b_junk, b_ss])
            kb.op(kb.act, lambda e: e.activation(out=rs[:], in_=ss[:], func=AF.Sqrt, scale=1.0 / 512, bias=EPS), reads=[b_ss], writes=[b_rs])
            kb.op(kb.dve, lambda e: e.reciprocal(out=rs[:], in_=rs[:]), reads=[b_rs], writes=[b_rs])
            for i in range(NB):
                t_, bt = tmpo.next()
                o_, bo, so = ost.next()
                kb.op(kb.dve, lambda e: e.scalar_tensor_tensor(out=t_[:], in0=hsum[:, i, :], scalar=rs[:, i:i + 1], in1=gain[:], op0=ALU.mult, op1=ALU.mult),
                      reads=[b_hs[i], b_rs, b_gain], writes=[bt])
                kb.op(kb.pool, lambda e: e.tensor_tensor(out=o_[:], in0=t_[:], in1=rg[:, i, :], op=ALU.mult), reads=[bt, b_rg], writes=[bo])
                kb.dma(kb.sp, mixv[:, i, h * 512:(h + 1) * 512], o_[:], so, reads=[bo], writes=[b_mix])
        ph.close()

    def phase_outproj(self, l, skip_ctx=False):
        kb = self.kb
        w_dram = self.ab_w_out if l == 0 else self.gla_w_out
        b_mix, b_y = self.sbuf_of("mixTM"), self.sbuf_of("yscr")
        HALVES = [(0, 1152, [(0, 512), (512, 1024), (1024, 1152)]), (1152, 2304, [(1152, 1664), (1664, 2176), (2176, 2304)])]
        if skip_ctx:
            HALVES[0] = (0, 1152, [(256, 768), (768, 1152)])
        for hi_, (lo, hi, tiles) in enumerate(HALVES):
            nh = hi - lo
            tg = "%d_%d" % (l, hi_)
            ph = Phase(kb, "op" + tg)
            mixT = ph.sb("mixT", [128, NDC, nh], BF16)
            b_mT = [kb.buf("mixT%d" % i) for i in range(nh // 128)]
            rowr = Ring(kb, "oprow" + tg, [ph.sb("row%d" % i, [128, D], BF16) for i in range(2)], with_sem=True)
            pst = Ring(kb, "oppt" + tg, [ph.ps("pt%d" % i, [128, 512], BF16) for i in range(2)])
            for tb in range(nh // 128):
                if skip_ctx and lo + tb * 128 < LC:
                    continue
                row, brow_, srow = rowr.next()
                kb.dma(kb.sp, row[:], self.mixTM[lo + tb * 128:lo + (tb + 1) * 128, :], srow, reads=[b_mix], writes=[brow_])
                for c4 in range(8):
                    pt, bpt = pst.next()
                    for k in range(4):
                        c = c4 * 4 + k
                        kb.op(kb.pe, lambda e: e.transpose(out=pt[:, k * 128:(k + 1) * 128], in_=row[:, c * 128:(c + 1) * 128], identity=self.cmb[:, 0, :]),
                              reads=[brow_, self.b_cm], writes=[bpt])
                    dst = mixT[:, c4 * 4:(c4 + 1) * 4, tb * 128:(tb + 1) * 128]
                    src = pt[:, :].rearrange("p (a b) -> p a b", a=4)
                    if c4 % 2 == 0:
                        kb.op(kb.act, lambda e: e.activation(out=dst, in_=src, func=AF.Copy), reads=[bpt], writes=[b_mT[tb]])
                    else:
                        kb.op(kb.dve, lambda e: e.tensor_copy(out=dst, in_=src), reads=[bpt], writes=[b_mT[tb]])
            wr = Ring(kb, "opw" + tg, [ph.sb("w%d" % i, [128, NDC, 128], BF16) for i in range(2)], with_sem=True)
            psr = Ring(kb, "opps" + tg, [ph.ps("p%d" % i, [128, 512], F32) for i in range(6)])
            yst = Ring(kb, "opy" + tg, [ph.sb("y%d" % i, [128, nh], F32) for i in range(2)], with_sem=True, multi=True)
            for dc in range(NDC):
                w, bw, sw = wr.next()
                kb.dma(kb.pool, w[:], w_dram[dc], sw, writes=[bw])
                y, by, sy = yst.next()
                pbs = [psr.next() for _ in tiles]
                kb.mm_multi([pb[0][:, 0:t1 - t0] for pb, (t0, t1) in zip(pbs, tiles)], NDC,
                            lambda kc: w[:, kc, :], lambda kc, ti: mixT[:, kc, tiles[ti][0] - lo:tiles[ti][1] - lo],
                            reads=[bw] + b_mT, writes=[pb[1] for pb in pbs])
                for ti, (t0, t1) in enumerate(tiles):
                    n = t1 - t0
                    p, bp = pbs[ti]
                    if ti % 2 == 0:
                        kb.op(kb.act, lambda e: e.activation(out=y[:, t0 - lo:t1 - lo], in_=p[:, 0:n], func=AF.Copy), reads=[bp], writes=[by])
                    else:
                        kb.op(kb.dve, lambda e: e.tensor_copy(out=y[:, t0 - lo:t1 - lo], in_=p[:, 0:n]), reads=[bp], writes=[by])
                y0 = tiles[0][0]
                kb.dma(kb.sp, self.yscr[dc][:, y0:hi], y[:, y0 - lo:], sy, reads=[by], writes=[b_y])
            ph.close()

    def phase_resid(self, l, kgate, xsrc, b_xsrc, xdst, b_xdst, final=False):
        kb = self.kb
        ph = Phase(kb, "res%d_%d" % (l, kgate))
        b_y = self.sbuf_of("yscr")
        yr = Ring(kb, "ry", [ph.sb("y%d" % i, [128, T], F32) for i in range(2)], with_sem=True)
        xr = Ring(kb, "rx", [ph.sb("x%d" % i, [128, T], F32) for i in range(2)], with_sem=True)
        orr = Ring(kb, "ro", [ph.sb("o%d" % i, [128, T], F32) for i in range(2)], with_sem=True, multi=True)
        acc = ph.sb("acc", [128, T], F32); tmp = ph.sb("tmp", [128, T], F32); rstd = ph.sb("rstd", [128, T], F32)
        b_acc, b_tmp, b_rstd = kb.buf("racc"), kb.buf("rtmp"), kb.buf("rrstd")
        pss = [ph.ps("ps%d" % i, [128, 512], F32) for i in range(2)]
        b_pss = [kb.buf("rps%d" % i) for i in range(2)]
        for dc in range(NDC):
            y, by, sy = yr.next()
            kb.dma(kb.sp, y[:], self.yscr[dc], sy, reads=[b_y], writes=[by])
            if dc == 0:
                kb.op(kb.dve, lambda e: e.tensor_tensor(out=acc[:], in0=y[:], in1=y[:], op=ALU.mult), reads=[by], writes=[b_acc])
            else:
                kb.op(kb.act, lambda e: e.activation(out=tmp[:], in_=y[:], func=AF.Square), reads=[by], writes=[b_tmp])
                kb.op(kb.dve, lambda e: e.tensor_tensor(out=acc[:], in0=acc[:], in1=tmp[:], op=ALU.add), reads=[b_tmp], writes=[b_acc])
        for i, (t0, t1) in enumerate(TT):
            p = pss[i % 2]
            kb.mm(p[:, 0:t1 - t0], [(self.cm[:, 1, :], acc[:, t0:t1])], reads=[self.b_cm, b_acc], writes=[b_pss[i % 2]])
            kb.op(kb.act, lambda e: e.activation(out=tmp[:, t0:t1], in_=p[:, 0:t1 - t0], func=AF.Sqrt, scale=1.0 / D, bias=EPS),
                  reads=[b_pss[i % 2]], writes=[b_tmp])
        kb.op(kb.dve, lambda e: e.reciprocal(out=rstd[:], in_=tmp[:]), reads=[b_tmp], writes=[b_rstd])
        def _load(dcc):
            y_, by_, sy_ = yr.next()
            x_, bx_, sx_ = xr.next()
            kb.dma(kb.sp, y_[:], self.yscr[dcc], sy_, reads=[b_y], writes=[by_])
            kb.dma(kb.sp, x_[:], xsrc[dcc], sx_, reads=[b_xsrc], writes=[bx_])
            return (y_, by_, x_, bx_)
        nxt = _load(0)
        for dc in range(NDC):
            y, by, x, bx = nxt
            if dc + 1 < NDC:
                nxt = _load(dc + 1)
            o, bo, so = orr.next()
            kb.op(kb.dve, lambda e: e.tensor_tensor(out=y[:], in0=y[:], in1=rstd[:], op=ALU.mult), reads=[b_rstd, by], writes=[by])
            for cl, a0, a1 in cls_cols(0, T):
                kb.op(kb.dve, lambda e: e.scalar_tensor_tensor(out=o[:, a0:a1], in0=y[:, a0:a1], scalar=self.der[:, l, kgate, dc, cl:cl + 1],
                                                                in1=x[:, a0:a1], op0=ALU.mult, op1=ALU.add),
                      reads=[by, bx, self.b_der], writes=[bo])
            if final:
                kb.dma(kb.sp, self.out[dc], o[:, LC:T], so, reads=[bo])
            else:
                kb.dma(kb.sp, xdst[dc], o[:], so, reads=[bo], writes=[b_xdst])
        ph.close()

    def phase_ffn_up(self, l, xsrc, b_x, skip_ctx=False):
        kb = self.kb
        b_act = self.sbuf_of("actscr")
        HALVES = [
            (0, 1153, [(0, 256), (256, 768), (768, 1153)], lambda t: (t + 1) if t < LC else (t + 2), 1156, [(1, 257, 0), (258, 1154, 256)]),
            (1151, 2304, [(1151, 1663), (1663, 2175), (2175, 2304)], lambda t: t - 1151, 1154, [(1, 1153, 1152)]),
        ]
        if skip_ctx:
            h0 = HALVES[0]
            HALVES[0] = (h0[0], h0[1], h0[2][1:], h0[3], h0[4], h0[5][1:])
        for hi_, (lo, hi, tiles, colof, ncols, outs) in enumerate(HALVES):
            nh = hi - lo
            tg = "%d_%d" % (l, hi_)
            ph = Phase(kb, "fu" + tg)
            hT = ph.sb("hT", [128, NDC, nh], BF16)
            b_hT = [kb.buf("fhT%d" % i) for i in range(NDC)]
            ph2 = Phase(kb, "fun" + tg)
            self.norm_mod(ph2, xsrc, b_x, l, 3, 4, hT, b_hT, lo, hi)
            ph2.close()
            cw = ph.sb("cw", [128, 3, 2 * NFC], F32); cbt = ph.sb("cb", [128, 2 * NFC], F32)
            b_cw = kb.buf("cw")
            kb.dma(kb.sp, cw[:], self.ffn_cw[l], kb.dsem('m'), writes=[b_cw])
            kb.dma(kb.sp, cbt[:], self.ffn_cb[l], kb.dsem('m'), writes=[b_cw])
            wr = Ring(kb, "fuw" + tg, [ph.sb("w%d" % i, [128, NDC, 128], BF16) for i in range(4)], with_sem=True)
            psr = Ring(kb, "fups" + tg, [ph.ps("p%d" % i, [128, 512], F32) for i in range(6)])
            ur = Ring(kb, "fuu" + tg, [ph.sb("u%d" % i, [128, ncols], F32) for i in range(2)], multi=True)
            cgr = Ring(kb, "fucg" + tg, [ph.sb("cg%d" % i, [128, ncols], F32) for i in range(2)])
            sg = ph.sb("sg", [128, ncols], F32); b_sg = kb.buf("sg")
            ast = Ring(kb, "fua" + tg, [ph.sb("a%d" % i, [128, ncols], BF16) for i in range(2)], with_sem=True)
            for u in ur.tiles:
                kb.op(kb.pool, lambda e: e.memset(u[:], 0.0), writes=[ur.bufs[ur.tiles.index(u)]])
            W_ = ncols - 2
            for c in range(NFC):
                cgs = []
                for kind in range(2):
                    ch = c + kind * NFC
                    w, bw, sw = wr.next()
                    kb.dma(kb.pool, w[:], self.ffn_w_up[l][ch], sw, writes=[bw])
                    u, bu = ur.next()
                    pbs = [psr.next() for _ in tiles]
                    kb.mm_multi([pb[0][:, 0:t1 - t0] for pb, (t0, t1) in zip(pbs, tiles)], NDC,
                                lambda kc: w[:, kc, :], lambda kc, ti: hT[:, kc, tiles[ti][0] - lo:tiles[ti][1] - lo],
                                reads=[bw] + b_hT, writes=[pb[1] for pb in pbs])
                    for ti, (t0, t1) in enumerate(tiles):
                        n = t1 - t0
                        p, bp = pbs[ti]
                        c0 = colof(t0)
                        kb.op(kb.act, lambda e: e.activation(out=u[:, c0:c0 + n], in_=p[:, 0:n], func=AF.Copy), reads=[bp], writes=[bu])
                    cg, bcg = cgr.next()
                    kb.op(kb.dve, lambda e: e.tensor_scalar(out=cg[:, 0:W_], in0=u[:, 1:1 + W_], scalar1=cw[:, 1, ch:ch + 1], scalar2=cbt[:, ch:ch + 1],
                                                           op0=ALU.mult, op1=ALU.add), reads=[bu, b_cw], writes=[bcg])
                    kb.op(kb.dve, lambda e: e.scalar_tensor_tensor(out=cg[:, 0:W_], in0=u[:, 0:W_], scalar=cw[:, 0, ch:ch + 1], in1=cg[:, 0:W_],
                                                                    op0=ALU.mult, op1=ALU.add), reads=[bu, b_cw, bcg], writes=[bcg])
                    kb.op(kb.dve, lambda e: e.scalar_tensor_tensor(out=cg[:, 0:W_], in0=u[:, 2:2 + W_], scalar=cw[:, 2, ch:ch + 1], in1=cg[:, 0:W_],
                                                                    op0=ALU.mult, op1=ALU.add), reads=[bu, b_cw, bcg], writes=[bcg])
                    cgs.append((cg, bcg))
                (cgg, bcgg), (cgv, bcgv) = cgs
                kb.op(kb.act, lambda e: e.activation(out=sg[:, 0:W_], in_=cgg[:, 0:W_], func=AF.Silu), reads=[bcgg], writes=[b_sg])
                a, ba, sa = ast.next()
                kb.op(kb.dve, lambda e: e.tensor_tensor(out=a[:, 0:W_], in0=sg[:, 0:W_], in1=cgv[:, 0:W_], op=ALU.mult), reads=[b_sg, bcgv], writes=[ba])
                for (oc0, oc1, tk0) in outs:
                    kb.dma(kb.sp, self.actscr[c][:, tk0:tk0 + (oc1 - oc0)], a[:, oc0 - 1:oc1 - 1], sa, reads=[ba], writes=[b_act])
            ph.close()

    def phase_ffn_down(self, l, skip_ctx=False):
        kb = self.kb
        b_act, b_y = self.sbuf_of("actscr"), self.sbuf_of("yscr")
        ph = Phase(kb, "fd%d" % l)
        at = ph.sb("at", [128, NFC, 512], BF16); b_at = kb.buf("at", multi=True); s_at = kb.dsem("at")
        wr = Ring(kb, "fdw%d" % l, [ph.sb("w%d" % i, [128, NFC, 128], BF16) for i in range(2)], with_sem=True)
        psr = Ring(kb, "fdps%d" % l, [ph.ps("p%d" % i, [128, 512], F32) for i in range(4)])
        yst = Ring(kb, "fdy%d" % l, [ph.sb("y%d" % i, [128, 512], F32) for i in range(3)], with_sem=True)
        for (t0, t1) in (TT[1:] if skip_ctx else TT):
            n = t1 - t0
            for c0 in range(0, NFC, 8):
                c1 = min(NFC, c0 + 8)
                kb.dma(kb.sp, at[:, c0:c1, 0:n], self.actscr[c0:c1, :, t0:t1].rearrange("c p t -> p c t"), s_at, reads=[b_act], writes=[b_at])
            for dc in range(NDC):
                w, bw, sw = wr.next()
                kb.dma(kb.pool, w[:], self.ffn_w_down[l][dc], sw, writes=[bw])
                p, bp = psr.next()
                kb.mm(p[:, 0:n], [(w[:, kc, :], at[:, kc, 0:n]) for kc in range(NFC)], reads=[bw, b_at], writes=[bp])
                y, by, sy = yst.next()
                if dc % 2 == 0:
                    kb.op(kb.act, lambda e: e.activation(out=y[:, 0:n], in_=p[:, 0:n], func=AF.Copy), reads=[bp], writes=[by])
                else:
                    kb.op(kb.dve, lambda e: e.tensor_copy(out=y[:, 0:n], in_=p[:, 0:n]), reads=[bp], writes=[by])
                kb.dma(kb.sp, self.yscr[dc][:, t0:t1], y[:, 0:n], sy, reads=[by], writes=[b_y])
        ph.close()

    def finish(self):
        kb = self.kb
        kb.barrier()
        SRC = {"projFM": self.projFM, "projTM": self.projTM, "gatesTM": self.gatesTM, "mixTM": self.mixTM,
               "xA": self.xA, "xB": self.xB, "yscr": self.yscr, "actscr": self.actscr, "rlowFM": self.rlowFM}
        for name, shape, dtp in self.dbg:
            if name in SRC:
                kb.dma(kb.sp, self.dbgout[name], SRC[name], self.s_out)
            elif name == "mods":
                kb.dma(kb.sp, self.dbgout[name], self.mods[:], self.s_out)
            elif name == "der":
                kb.dma(kb.sp, self.dbgout[name], self.der[:], self.s_out)
        if self.s_out.v > 0:
            kb.sp.e.wait_ge(self.s_out.h, self.s_out.v)


def _rope_tables():
    rows = LL // 64
    row = np.repeat(np.arange(rows), 64).astype(np.float32)
    col = np.tile(np.arange(64), rows).astype(np.float32)
    inv = (10000.0 ** (-np.arange(32, dtype=np.float32) / 32)).astype(np.float32)
    ar = row[:, None] * inv
    ac = col[:, None] * inv
    C = np.concatenate([np.cos(ar), np.cos(ar), np.cos(ac), np.cos(ac)], axis=1).T
    S = np.concatenate([np.sin(ar), np.sin(ar), np.sin(ac), np.sin(ac)], axis=1).T
    return np.ascontiguousarray(C, np.float32), np.ascontiguousarray(S, np.float32)


def _cmat():
    m = np.zeros((6, 128, 128), np.float32)
    m[0] = np.eye(128)
    m[1] = 1.0
    idx = np.arange(128)
    m[2] = (idx[:, None] <= idx[None, :])
    m[3] = (idx[:, None] >= idx[None, :])
    R = np.zeros((128, 128), np.float32)
    for base in (0, 64):
        for i in range(32):
            R[base + i, base + 32 + i] = -1.0
            R[base + 32 + i, base + i] = 1.0
    m[4] = R.T
    return m


def _grp(w, ng):
    k, n = w.shape
    wp = np.zeros((k, ng * 256), np.float32)
    wp[:, :n] = w
    return np.ascontiguousarray(wp.reshape(NDC, 128, ng, 256).transpose(2, 1, 0, 3))


def _blk(w, kc, nc_):
    return np.ascontiguousarray(w.reshape(kc, 128, nc_, 128).transpose(2, 1, 0, 3))


def prep_shared(inp, names):
    sh = {}
    f = lambda a: np.asarray(a, np.float32)
    if "ada_w" in names: sh["ada_w"] = f(inp["ada_w"])
    if "ada_b" in names: sh["ada_b"] = np.ascontiguousarray(f(inp["ada_b"]).reshape(2, 192, 128).transpose(0, 2, 1))
    if "norm_w" in names: sh["norm_w"] = np.ascontiguousarray(f(inp["norm_w"]).reshape(2, 4, NDC, 128).transpose(0, 3, 1, 2))
    if "ab_w_in" in names: sh["ab_w_in"] = _grp(f(inp["ab_w_in"])[0], AB_G)
    if "ab_gate_b" in names: sh["ab_gate_b"] = f(inp["ab_gate_b"]).reshape(1, 16)
    if "ab_sink" in names: sh["ab_sink"] = f(inp["ab_sink"]).reshape(1, 16)
    if "ab_head_norm" in names: sh["ab_head_norm"] = f(inp["ab_head_norm"]).reshape(1, 2048)
    if "ab_w_out" in names: sh["ab_w_out"] = _blk(f(inp["ab_w_out"])[0], NDC, NDC)
    if "gla_w_in" in names: sh["gla_w_in"] = _grp(f(inp["gla_w_in"])[0], GLA_G)
    if "gla_gw2" in names:
        sh["gla_gw2"] = np.ascontiguousarray(np.concatenate([f(inp["gla_gate_w2"])[0], f(inp["gla_gate_b"])[0][:, None, :]], axis=1))
    if "gla_head_norm" in names: sh["gla_head_norm"] = f(inp["gla_head_norm"]).reshape(1, 4096)
    if "gla_w_out" in names: sh["gla_w_out"] = _blk(f(inp["gla_w_out"])[0], NDC, NDC)
    if "ffn_w_up" in names:
        sh["ffn_w_up"] = np.stack([_blk(f(inp["ffn_w_up"])[l], NDC, 2 * NFC) for l in range(2)])
    if "ffn_cw" in names:
        sh["ffn_cw"] = np.ascontiguousarray(f(inp["ffn_conv_w"]).reshape(2, 3, 2 * NFC, 128).transpose(0, 3, 1, 2))
    if "ffn_cb" in names:
        sh["ffn_cb"] = np.ascontiguousarray(f(inp["ffn_conv_b"]).reshape(2, 2 * NFC, 128).transpose(0, 2, 1))
    if "ffn_w_down" in names:
        sh["ffn_w_down"] = np.stack([_blk(f(inp["ffn_w_down"])[l], NFC, NDC) for l in range(2)])
    C, S = _rope_tables()
    sh["ropeC"], sh["ropeS"], sh["cmat"] = C, S, _cmat()
    return sh


def prep_core(inp, b, sh):
    m = dict(sh)
    xt = np.concatenate([np.asarray(inp["ctx"][b], np.float32), np.asarray(inp["x"][b], np.float32)], axis=0)
    m["xT"] = np.ascontiguousarray(xt.T.reshape(NDC, 128, T))
    cs = np.stack([np.asarray(inp["c"][b], np.float32), np.asarray(inp["c_ctx"], np.float32)], axis=-1)
    m["csT"] = np.ascontiguousarray(cs.reshape(NDC, 128, 2).transpose(1, 0, 2))
    return m


def build_program(level=5, dbg=()):
    nc = bass.Bass("TRN2", target_bir_lowering=False)
    P = Prog(nc, level, dbg)
    kb = P.kb
    with nc.Block():
        P.phase_consts_ada()
        b_x0 = kb.buf("xT")
        b_xA, b_xB = P.sbuf_of("xA"), P.sbuf_of("xB")
        P.phase_inproj(0, P.xT, b_x0)
        if level >= 2:
            P.phase_mix_ab()
        if level >= 3:
            P.phase_outproj(0)
            P.phase_resid(0, 2, P.xT, b_x0, P.xA, b_xA)
        if level >= 4:
            P.phase_ffn_up(0, P.xA, b_xA)
            P.phase_ffn_down(0)
            P.phase_resid(0, 5, P.xA, b_xA, P.xB, b_xB)
        if level >= 5:
            P.phase_inproj(1, P.xB, b_xB)
            P.phase_mix_gla()
            P.phase_outproj(1, skip_ctx=True)
            P.phase_resid(1, 2, P.xB, b_xB, P.xA, b_xA)
            P.phase_ffn_up(1, P.xA, b_xA, skip_ctx=True)
            P.phase_ffn_down(1, skip_ctx=True)
            P.phase_resid(1, 5, P.xA, b_xA, None, None, final=True)
        P.finish()
    return nc, P


_CACHE = {}


def kernel(**inputs):
    ncores = 4
    if "prog" not in _CACHE:
        _CACHE["prog"] = build_program(level=5, dbg=())
    nc, P = _CACHE["prog"]
    names = set(P.in_names)
    sh = prep_shared(inputs, names)
    in_maps = []
    for b in range(ncores):
        m = prep_core(inputs, b, sh)
        in_maps.append({k: m[k] for k in P.in_names})
    res = run_bass_kernel_spmd(nc, in_maps, core_ids=list(range(ncores)))
    outs = []
    for b in range(ncores):
        o = np.asarray(res.results[b]["out"], np.float32).reshape(D, LL)
        outs.append(np.ascontiguousarray(o.T))
    return np.stack(outs, axis=0)
```

```python
import numpy as np
import ml_dtypes
import concourse.bass as bass
import concourse.mybir as mybir
from concourse.bass_utils import run_bass_kernel_spmd

F32 = mybir.dt.float32
BF16 = mybir.dt.bfloat16
AF = mybir.ActivationFunctionType
ALU = mybir.AluOpType
AX = mybir.AxisListType

D = 4096
NDC = 32
LC = 256
LL = 2048
T = LC + LL
NB = T // 128
DFF = 11008
NFC = DFF // 128
EPS = 1e-6
TT = [(0, 256), (256, 768), (768, 1280), (1280, 1792), (1792, 2304)]
AB_IN = 9232
GLA_IN = 12320
AB_G = 37
GLA_G = 49


class Sem:
    def __init__(self, nc, name):
        self.h = nc.alloc_semaphore(name)
        self.v = 0
        self.name = name


class Buf:
    __slots__ = ("name", "w", "r", "multi")

    def __init__(self, name, multi=False):
        self.name = name
        self.w = {}
        self.r = {}
        self.multi = multi


class Eng:
    def __init__(self, kb, name, e):
        self.kb = kb
        self.name = name
        self.e = e
        self.sem = Sem(kb.nc, "p_" + name)
        self.seen = {}
        self.nsem = 0

    def tick(self):
        if self.sem.v >= 60000:
            self.nsem += 1
            self.sem = Sem(self.kb.nc, "p_%s%d" % (self.name, self.nsem))
        self.sem.v += 1
        return (self.sem, self.sem.v)


class KB:
    def __init__(self, nc):
        self.nc = nc
        self.pe = Eng(self, "pe", nc.tensor)
        self.act = Eng(self, "act", nc.scalar)
        self.dve = Eng(self, "dve", nc.vector)
        self.pool = Eng(self, "pool", nc.gpsimd)
        self.sp = Eng(self, "sp", nc.sync)
        self.engs = [self.pe, self.act, self.dve, self.pool, self.sp]
        self.dsems = []
        self.free_sems = []
        self.cur_phase = None
        self.phase_stack = []
        self.nbuf = 0

    def buf(self, name=None, multi=False):
        self.nbuf += 1
        return Buf(name or ("b%d" % self.nbuf), multi)

    def dsem(self, name):
        if self.free_sems:
            s = self.free_sems.pop()
        else:
            s = Sem(self.nc, "d_%d" % len(self.dsems))
            self.dsems.append(s)
        if self.cur_phase is not None:
            self.cur_phase.sems.append(s)
        return s

    def _waits(self, eng, reads, writes):
        need = {}
        for b in reads:
            for s, v in b.w.items():
                if need.get(s, 0) < v:
                    need[s] = v
        for b in writes:
            if not b.multi:
                for s, v in b.w.items():
                    if need.get(s, 0) < v:
                        need[s] = v
            for s, v in b.r.items():
                if need.get(s, 0) < v:
                    need[s] = v
        for s, v in need.items():
            if eng.seen.get(s, 0) < v:
                eng.e.wait_ge(s.h, v)
                eng.seen[s] = v

    def _mark(self, tok, reads, writes):
        s, v = tok
        for b in writes:
            if b.multi:
                if b.w.get(s, 0) < v:
                    b.w[s] = v
            else:
                b.w = {s: v}
                b.r = {}
        for b in reads:
            if b.r.get(s, 0) < v:
                b.r[s] = v

    def op(self, eng, fn, reads=(), writes=()):
        self._waits(eng, reads, writes)
        ins = fn(eng.e)
        tok = eng.tick()
        ins.then_inc(tok[0].h, 1)
        self._mark(tok, reads, writes)
        return ins

    def mm(self, out_ap, pairs, reads, writes, fp32=False):
        eng = self.pe
        self._waits(eng, reads, writes)
        n = len(pairs)
        ins = None
        for i, (l, r) in enumerate(pairs):
            ins = eng.e.matmul(out_ap, lhsT=l, rhs=r, start=(i == 0), stop=(i == n - 1))
        tok = eng.tick()
        ins.then_inc(tok[0].h, 1)
        self._mark(tok, reads, writes)

    def mm_multi(self, outs, nk, lhs_fn, rhs_fn, reads, writes):
        eng = self.pe
        self._waits(eng, reads, writes)
        ins = None
        for kc in range(nk):
            l = lhs_fn(kc)
            for ti, o in enumerate(outs):
                ins = eng.e.matmul(o, lhsT=l, rhs=rhs_fn(kc, ti), start=(kc == 0), stop=(kc == nk - 1))
        tok = eng.tick()
        ins.then_inc(tok[0].h, 1)
        self._mark(tok, reads, writes)

    def dma(self, q, out_ap, in_ap, sem, reads=(), writes=()):
        self._waits(q, reads, writes)
        ins = q.e.dma_start(out=out_ap, in_=in_ap)
        sem.v += 16
        ins.then_inc(sem.h, 16)
        self._mark((sem, sem.v), reads, writes)

    def barrier(self):
        toks = {}
        for e in self.engs:
            if e.sem.v > 0:
                toks[e.sem] = e.sem.v
        for s in self.dsems:
            if s.v > 0:
                toks[s] = s.v
        for e in self.engs:
            for s, v in toks.items():
                if s is e.sem:
                    continue
                if e.seen.get(s, 0) < v:
                    e.e.wait_ge(s.h, v)
                    e.seen[s] = v


class Ring:
    def __init__(self, kb, name, tiles, with_sem=False, multi=False):
        self.tiles = tiles
        self.bufs = [kb.buf("%s%d" % (name, i), multi) for i in range(len(tiles))]
        self.sems = [kb.dsem("%s%d" % (name, i)) for i in range(len(tiles))] if with_sem else None
        self.i = -1

    def next(self):
        self.i = (self.i + 1) % len(self.tiles)
        if self.sems:
            return self.tiles[self.i], self.bufs[self.i], self.sems[self.i]
        return self.tiles[self.i], self.bufs[self.i]


class Phase:
    def __init__(self, kb, name):
        self.kb = kb
        self.name = name
        self.guards = []
        self.sems = []
        kb.phase_stack.append(self)
        kb.cur_phase = self

    def sb(self, name, shape, dt):
        g = self.kb.nc.sbuf_tensor(self.name + "_" + name, shape, dt)
        t = g.__enter__()
        self.guards.append(g)
        return t

    def ps(self, name, shape, dt=F32):
        g = self.kb.nc.psum_tensor(self.name + "_" + name, shape, dt)
        t = g.__enter__()
        self.guards.append(g)
        return t

    def close(self):
        self.kb.barrier()
        for g in reversed(self.guards):
            g.__exit__(None, None, None)
        self.guards = []
        self.kb.free_sems.extend(self.sems)
        self.sems = []
        assert self.kb.phase_stack[-1] is self
        self.kb.phase_stack.pop()
        self.kb.cur_phase = self.kb.phase_stack[-1] if self.kb.phase_stack else None


def cls_cols(t0, t1):
    out = []
    if t0 < LC:
        out.append((1, t0, min(t1, LC)))
    if t1 > LC:
        out.append((0, max(t0, LC), t1))
    return out


class Prog:
    LEVELS = {"ab_gate_b": 2, "ab_sink": 2, "ab_head_norm": 2, "ab_w_out": 3, "ffn_w_up": 4, "ffn_cw": 4,
              "ffn_cb": 4, "ffn_w_down": 4, "gla_w_in": 5, "gla_gw2": 5, "gla_head_norm": 5, "gla_w_out": 5}

    def __init__(self, nc, level=5, dbg=()):
        self.nc = nc
        self.kb = KB(nc)
        self.level = level
        self.dbg = dbg
        self.in_names = []
        kb = self.kb
        dt = nc.dram_tensor

        def din(name, shape, dtp=F32):
            if self.LEVELS.get(name, 1) > level:
                return None
            self.in_names.append(name)
            return dt(name, list(shape), dtp, kind="ExternalInput").ap()

        def dscr(name, shape, dtp):
            return dt(name, list(shape), dtp).ap()

        self.xT = din("xT", [NDC, 128, T])
        self.csT = din("csT", [128, NDC, 2])
        self.ada_w = din("ada_w", [2, D, 6 * D])
        self.ada_b = din("ada_b", [2, 128, 192])
        self.norm_w = din("norm_w", [2, 128, 4, NDC])
        self.ab_w_in = din("ab_w_in", [AB_G, 128, NDC, 256])
        self.ab_gate_b = din("ab_gate_b", [1, 16])
        self.ab_sink = din("ab_sink", [1, 16])
        self.ab_head_norm = din("ab_head_norm", [1, 2048])
        self.ab_w_out = din("ab_w_out", [NDC, 128, NDC, 128])
        self.gla_w_in = din("gla_w_in", [GLA_G, 128, NDC, 256])
        self.gla_gw2 = din("gla_gw2", [2, 17, 2048])
        self.gla_head_norm = din("gla_head_norm", [1, 4096])
        self.gla_w_out = din("gla_w_out", [NDC, 128, NDC, 128])
        self.ffn_w_up = din("ffn_w_up", [2, 2 * NFC, 128, NDC, 128])
        self.ffn_cw = din("ffn_cw", [2, 128, 3, 2 * NFC])
        self.ffn_cb = din("ffn_cb", [2, 128, 2 * NFC])
        self.ffn_w_down = din("ffn_w_down", [2, NDC, 128, NFC, 128])
        self.ropeC = din("ropeC", [128, LL])
        self.ropeS = din("ropeS", [128, LL])
        self.cmat = din("cmat", [6, 128, 128])
        self.out = dt("out", [NDC, 128, LL], F32, kind="ExternalOutput").ap()
        self.xA = dscr("xA", [NDC, 128, T], F32)
        self.xB = dscr("xB", [NDC, 128, T], F32)
        self.yscr = dscr("yscr", [NDC, 128, T], F32)
        self.projFM = dscr("projFM", [40, 128, T], BF16)
        self.projTM = dscr("projTM", [32, T, 256], BF16)
        self.gatesTM = dscr("gatesTM", [T, 16], F32)
        self.rlowFM = dscr("rlowFM", [32, T], F32)
        self.mixTM = dscr("mixTM", [T, D], BF16)
        self.actscr = dscr("actscr", [NFC, 128, T], BF16)
        self.rstdy = dscr("rstdy", [1, T], F32)
        self.dbgout = {}
        for name, shape, dtp in dbg:
            self.dbgout[name] = dt("dbg_" + name, list(shape), dtp, kind="ExternalOutput").ap()

        a = nc.alloc_sbuf_tensor
        self.cm = a("cm", [128, 6, 128], F32)
        self.cmb = a("cmb", [128, 6, 128], BF16)
        self.mods = a("mods", [128, 2, 192, 2], F32)
        self.nw = a("nw", [128, 2, 4, NDC], F32)
        self.der = a("der", [128, 2, 6, NDC, 2], F32)
        self.sc = a("sc", [128, NDC, 2], F32)
        self.scb = a("scb", [128, NDC, 2], BF16)
        self.adab = a("adab", [128, 2, 192], F32)
        self.b_cs = kb.buf("cs")
        self.b_adab = kb.buf("adab")
        self.b_nw = kb.buf("nw")
        self.b_cm = kb.buf("cm")
        self.b_mods = kb.buf("mods", multi=True)
        self.b_der = kb.buf("der")
        self.s_misc = kb.dsem("misc")
        self.s_out = kb.dsem("out")
        self.b_scr = {}

    def sbuf_of(self, name):
        if name not in self.b_scr:
            self.b_scr[name] = self.kb.buf("scr_" + name, multi=True)
        return self.b_scr[name]

    def ada_group(self, l, g, wr, psr, ptr, rowr):
        kb = self.kb
        w, bw, sw = wr.next()
        kb.dma(kb.pool, w[:], self.ada_w[l].rearrange("(kc p) n -> p kc n", p=128)[:, :, g * 512:(g + 1) * 512], sw, writes=[bw])
        p, bp = psr.next()
        kb.mm(p[0:2, :], [(self.scb[:, kc, :], w[:, kc, :]) for kc in range(NDC)], reads=[bw, self.b_cs], writes=[bp])
        row, brow_ = rowr.next()
        kb.op(kb.act, lambda e: e.activation(out=row[:], in_=p[0:2, :], func=AF.Copy), reads=[bp], writes=[brow_])
        pt, bpt = ptr.next()
        for m in range(4):
            kb.mm(pt[:, 2 * m:2 * m + 2], [(row[0:2, m * 128:(m + 1) * 128], self.cm[0:2, 0, 0:2])], reads=[brow_, self.b_cm], writes=[bpt])
        for cl in range(2):
            kb.op(kb.dve, lambda e, cl=cl: e.tensor_tensor(
                out=self.mods[:, l, g * 4:(g + 1) * 4, cl], in0=pt[:, 0:8].rearrange("p (j c) -> p j c", c=2)[:, :, cl],
                in1=self.adab[:, l, g * 4:(g + 1) * 4], op=ALU.add), reads=[bpt, self.b_adab], writes=[self.b_mods])

    def ada_derived(self, l):
        kb = self.kb
        for cl in range(2):
            md = lambda w: self.mods[:, l, w * 32:(w + 1) * 32, cl]
            dr = lambda k: self.der[:, l, k, :, cl]
            ops = [
                lambda e: e.scalar_tensor_tensor(out=dr(0), in0=md(1), scalar=1.0, in1=self.nw[:, l, 0, :], op0=ALU.add, op1=ALU.mult),
                lambda e: e.tensor_copy(out=dr(1), in_=md(0)),
                lambda e: e.tensor_tensor(out=dr(2), in0=md(2), in1=self.nw[:, l, 1, :], op=ALU.mult),
                lambda e: e.scalar_tensor_tensor(out=dr(3), in0=md(4), scalar=1.0, in1=self.nw[:, l, 2, :], op0=ALU.add, op1=ALU.mult),
                lambda e: e.tensor_copy(out=dr(4), in_=md(3)),
                lambda e: e.tensor_tensor(out=dr(5), in0=md(5), in1=self.nw[:, l, 3, :], op=ALU.mult),
            ]
            for f in ops:
                kb.op(kb.dve, f, reads=[self.b_mods, self.b_nw], writes=[self.b_der])

    def phase_consts_ada(self):
        kb, nc = self.kb, self.nc
        ph = Phase(kb, "ada")
        kb.dma(kb.sp, self.cm[:], self.cmat.rearrange("k p n -> p k n"), kb.dsem('m'), writes=[self.b_cm])
        kb.op(kb.dve, lambda e: e.tensor_copy(out=self.cmb[:], in_=self.cm[:]), reads=[self.b_cm], writes=[self.b_cm])
        kb.dma(kb.sp, self.nw[:], self.norm_w.rearrange("l p f d -> p l f d"), kb.dsem('m'), writes=[self.b_nw])
        kb.dma(kb.sp, self.sc[:], self.csT, kb.dsem('m'), writes=[self.b_cs])
        kb.dma(kb.sp, self.adab[:], self.ada_b.rearrange("l p j -> p l j"), kb.dsem('m'), writes=[self.b_adab])
        kb.op(kb.act, lambda e: e.activation(out=self.sc[:], in_=self.sc[:], func=AF.Silu), reads=[self.b_cs], writes=[self.b_cs])
        kb.op(kb.dve, lambda e: e.tensor_copy(out=self.scb[:], in_=self.sc[:]), reads=[self.b_cs], writes=[self.b_cs])
        wr = Ring(kb, "adaw", [ph.sb("w%d" % i, [128, NDC, 512], BF16) for i in range(3)], with_sem=True)
        psr = Ring(kb, "adap", [ph.ps("p%d" % i, [128, 512], F32) for i in range(2)])
        ptr = Ring(kb, "adat", [ph.ps("t%d" % i, [128, 512], F32) for i in range(2)])
        rowr = Ring(kb, "adar", [ph.sb("row%d" % i, [2, 512], F32) for i in range(2)])
        for g in range(48):
            self.ada_group(0, g, wr, psr, ptr, rowr)
        self.ada_derived(0)
        ph.close()

    def norm_mod(self, ph, xsrc, b_x, l, kg, ksh, hT, b_hT, lo, hi):
        kb = self.kb
        n = hi - lo
        xr = Ring(kb, "nx", [ph.sb("nx%d" % i, [128, n], F32) for i in range(2)], with_sem=True)
        acc = ph.sb("nacc", [128, n], F32)
        tmp = ph.sb("ntmp", [128, n], F32)
        rstd = ph.sb("nrstd", [128, n], F32)
        b_acc, b_tmp, b_rstd = kb.buf("nacc"), kb.buf("ntmp"), kb.buf("nrstd")
        pss = [ph.ps("nps%d" % i, [128, 512], F32) for i in range(2)]
        b_pss = [kb.buf("nps%d" % i) for i in range(2)]
        for dc in range(NDC):
            x, bx, sx = xr.next()
            kb.dma(kb.sp, x[:], xsrc[dc][:, lo:hi], sx, reads=[b_x], writes=[bx])
            if dc == 0:
                kb.op(kb.dve, lambda e: e.tensor_tensor(out=acc[:], in0=x[:], in1=x[:], op=ALU.mult), reads=[bx], writes=[b_acc])
            else:
                kb.op(kb.act, lambda e: e.activation(out=tmp[:], in_=x[:], func=AF.Square), reads=[bx], writes=[b_tmp])
                kb.op(kb.dve, lambda e: e.tensor_tensor(out=acc[:], in0=acc[:], in1=tmp[:], op=ALU.add), reads=[b_tmp], writes=[b_acc])
        i = 0
        for c0 in range(0, n, 512):
            c1 = min(n, c0 + 512)
            p = pss[i % 2]
            kb.mm(p[:, 0:c1 - c0], [(self.cm[:, 1, :], acc[:, c0:c1])], reads=[self.b_cm, b_acc], writes=[b_pss[i % 2]])
            kb.op(kb.act, lambda e: e.activation(out=tmp[:, c0:c1], in_=p[:, 0:c1 - c0], func=AF.Sqrt, scale=1.0 / D, bias=EPS),
                  reads=[b_pss[i % 2]], writes=[b_tmp])
            i += 1
        kb.op(kb.dve, lambda e: e.reciprocal(out=rstd[:], in_=tmp[:]), reads=[b_tmp], writes=[b_rstd])
        for dc in range(NDC):
            x, bx, sx = xr.next()
            kb.dma(kb.sp, x[:], xsrc[dc][:, lo:hi], sx, reads=[b_x], writes=[bx])
            kb.op(kb.dve, lambda e: e.tensor_tensor(out=tmp[:], in0=x[:], in1=rstd[:], op=ALU.mult), reads=[bx, b_rstd], writes=[b_tmp])
            for cl, a0, a1 in cls_cols(lo, hi):
                kb.op(kb.act, lambda e: e.activation(out=hT[:, dc, a0 - lo:a1 - lo], in_=tmp[:, a0 - lo:a1 - lo], func=AF.Identity,
                                                     scale=self.der[:, l, kg, dc, cl:cl + 1], bias=self.der[:, l, ksh, dc, cl:cl + 1]),
                      reads=[b_tmp, self.b_der], writes=[b_hT[dc]])

    def phase_inproj(self, l, xsrc, b_x):
        kb = self.kb
        w_dram = self.ab_w_in if l == 0 else self.gla_w_in
        ngroups = AB_G if l == 0 else GLA_G
        if l == 0:
            def plan(g):
                if g < 8: return ("FM", 2 * g, 128 ** -0.5, True)
                if g < 10: return ("FM", 16 + 2 * (g - 8), 1.0, True)
                if g < 12: return ("TM", g - 10, 1.0, False)
                if g < 16: return ("FM", 20 + 2 * (g - 12), 1.0, False)
                if g < 20: return ("FM", 28 + 2 * (g - 16), 1.0 / 16, False)
                if g < 36: return ("TM", 2 + (g - 20), 1.0, False)
                return ("GT", 0, 1.0, False)
        else:
            def plan(g):
                if g < 8: return ("FM", 2 * g, 1.0 / 16, False)
                if g < 16: return ("FM", 16 + 2 * (g - 8), 1.0, False)
                if g < 48: return ("TM", g - 16, 1.0, False)
                return ("GF", 0, 1.0, False)
        b_pFM = self.sbuf_of("projFM")
        b_pTM = self.sbuf_of("projTM")
        b_gT = self.sbuf_of("gatesTM")
        b_rl = self.sbuf_of("rlowFM")
        HALVES = [(0, 1152, [(0, 256), (256, 768), (768, 1152)]), (1152, 2304, [(1152, 1664), (1664, 2176), (2176, 2304)])]
        for hi_, (lo, hi, tiles) in enumerate(HALVES):
            nh = hi - lo
            nbh = nh // 128
            ph = Phase(kb, "inp%d_%d" % (l, hi_))
            hT = ph.sb("hT", [128, NDC, nh], BF16)
            b_hT = [kb.buf("hT%d" % i) for i in range(NDC)]
            ph2 = Phase(kb, "inpn%d_%d" % (l, hi_))
            self.norm_mod(ph2, xsrc, b_x, l, 0, 1, hT, b_hT, lo, hi)
            ph2.close()
            tg = "%d_%d" % (l, hi_)
            wr = Ring(kb, "inw" + tg, [ph.sb("w%d" % i, [128, NDC, 256], BF16) for i in range(2)], with_sem=True)
            psr = Ring(kb, "inps" + tg, [ph.ps("p%d" % i, [128, 512], F32) for i in range(5)])
            psq = Ring(kb, "inpq" + tg, [ph.ps("q%d" % i, [128, 512], F32) for i in range(1)])
            psT = Ring(kb, "inpT" + tg, [ph.ps("T%d" % i, [128, 512], BF16) for i in range(2)])
            fmr = Ring(kb, "infm" + tg, [ph.sb("fm%d" % i, [128, nh], BF16) for i in range(4)], multi=True)
            stF = Ring(kb, "stF" + tg, [ph.sb("stF%d" % i, [128, nh], BF16) for i in range(2)], with_sem=True, multi=True)
            stT = Ring(kb, "stT" + tg, [ph.sb("stT%d" % i, [128, nbh, 256], BF16) for i in range(2)], with_sem=True, multi=True)
            stG = ph.sb("stG", [128, nbh, 64], F32)
            b_stG = kb.buf("stG", multi=True)
            s_stG = kb.dsem("stG" + tg)
            rC = Ring(kb, "rC" + tg, [ph.sb("rC%d" % i, [128, 512], F32) for i in range(2)], with_sem=True)
            rS = Ring(kb, "rS" + tg, [ph.sb("rS%d" % i, [128, 512], F32) for i in range(2)], with_sem=True)
            qb_r = Ring(kb, "qb" + tg, [ph.sb("qb%d" % i, [128, 512], BF16) for i in range(2)])
            t1_r = Ring(kb, "t1" + tg, [ph.sb("t1%d" % i, [128, 512], F32) for i in range(2)])
            t2_r = Ring(kb, "t2" + tg, [ph.sb("t2%d" % i, [128, 512], F32) for i in range(2)])
            stGf = stG[0:32, :, :].rearrange("p a b -> p (a b)")
            for g in range(ngroups):
                form, dst, scale, rope = plan(g)
                w, bw, sw = wr.next()
                kb.dma(kb.pool, w[:], w_dram[g], sw, writes=[bw])
                if form in ("FM", "GF"):
                    for m in range(2 if form == "FM" else 1):
                        if form == "FM":
                            st, bst, sst = stF.next()
                        pbs = [psr.next() for _ in tiles]
                        kb.mm_multi([pb[0][:, 0:t1 - t0] for pb, (t0, t1) in zip(pbs, tiles)], NDC,
                                    lambda kc: w[:, kc, m * 128:(m + 1) * 128],
                                    lambda kc, ti: hT[:, kc, tiles[ti][0] - lo:tiles[ti][1] - lo],
                                    reads=[bw] + b_hT, writes=[pb[1] for pb in pbs])
                        for ti, (t0, t1) in enumerate(tiles):
                            n = t1 - t0
                            p, bp = pbs[ti]
                            if form == "GF":
                                kb.op(kb.act, lambda e: e.activation(out=stGf[:, 0:n], in_=p[0:32, 0:n], func=AF.Copy),
                                      reads=[bp], writes=[b_stG])
                                kb.dma(kb.sp, self.rlowFM[:, t0:t1], stGf[:, 0:n], s_stG, reads=[b_stG], writes=[b_rl])
                                continue
                            if rope and t0 >= LC:
                                qb, bqb = qb_r.next()
                                kb.op(kb.act, lambda e: e.activation(out=qb[:, 0:n], in_=p[:, 0:n], func=AF.Copy, scale=scale),
                                      reads=[bp], writes=[bqb])
                                c_, bc_, sc_ = rC.next()
                                s_, bs_, ss_ = rS.next()
                                kb.dma(kb.sp, c_[:, 0:n], self.ropeC[:, t0 - LC:t1 - LC], sc_, writes=[bc_])
                                kb.dma(kb.sp, s_[:, 0:n], self.ropeS[:, t0 - LC:t1 - LC], ss_, writes=[bs_])
                                q2, bq2 = psq.next()
                                kb.mm(q2[:, 0:n], [(self.cmb[:, 4, :], qb[:, 0:n])], reads=[bqb, self.b_cm], writes=[bq2])
                                a1, ba1 = t1_r.next()
                                a2, ba2 = t2_r.next()
                                kb.op(kb.dve, lambda e: e.tensor_tensor(out=a1[:, 0:n], in0=qb[:, 0:n], in1=c_[:, 0:n], op=ALU.mult),
                                      reads=[bqb, bc_], writes=[ba1])
                                kb.op(kb.dve, lambda e: e.tensor_tensor(out=a2[:, 0:n], in0=q2[:, 0:n], in1=s_[:, 0:n], op=ALU.mult),
                                      reads=[bq2, bs_], writes=[ba2])
                                kb.op(kb.dve, lambda e: e.tensor_tensor(out=st[:, t0 - lo:t1 - lo], in0=a1[:, 0:n], in1=a2[:, 0:n], op=ALU.add),
                                      reads=[ba1, ba2], writes=[bst])
                            else:
                                kb.op(kb.act, lambda e: e.activation(out=st[:, t0 - lo:t1 - lo], in_=p[:, 0:n], func=AF.Copy, scale=scale),
                                      reads=[bp], writes=[bst])
                        if form == "FM":
                            kb.dma(kb.sp, self.projFM[dst + m][:, lo:hi], st[:], sst, reads=[bst], writes=[b_pFM])
                elif form == "TM":
                    st, bst, sst = stT.next()
                    fms = []
                    for m in range(2):
                        pbs = [psr.next() for _ in tiles]
                        kb.mm_multi([pb[0][:, 0:t1 - t0] for pb, (t0, t1) in zip(pbs, tiles)], NDC,
                                    lambda kc: w[:, kc, m * 128:(m + 1) * 128],
                                    lambda kc, ti: hT[:, kc, tiles[ti][0] - lo:tiles[ti][1] - lo],
                                    reads=[bw] + b_hT, writes=[pb[1] for pb in pbs])
                        fm, bfm = fmr.next()
                        for ti, (t0, t1) in enumerate(tiles):
                            p, bp = pbs[ti]
                            if ti % 2 == 0:
                                kb.op(kb.act, lambda e: e.activation(out=fm[:, t0 - lo:t1 - lo], in_=p[:, 0:t1 - t0], func=AF.Copy), reads=[bp], writes=[bfm])
                            else:
                                kb.op(kb.dve, lambda e: e.tensor_copy(out=fm[:, t0 - lo:t1 - lo], in_=p[:, 0:t1 - t0]), reads=[bp], writes=[bfm])
                        fms.append((fm, bfm))
                    for tb in range(nbh):
                        T_, bT = psT.next()
                        for m in range(2):
                            fm, bfm = fms[m]
                            kb.op(kb.pe, lambda e: e.transpose(out=T_[:, m * 128:(m + 1) * 128], in_=fm[:, tb * 128:(tb + 1) * 128], identity=self.cmb[:, 0, :]),
                                  reads=[bfm, self.b_cm], writes=[bT])
                        if tb % 2 == 0:
                            kb.op(kb.act, lambda e: e.activation(out=st[:, tb, :], in_=T_[:, 0:256], func=AF.Copy), reads=[bT], writes=[bst])
                        else:
                            kb.op(kb.dve, lambda e: e.tensor_copy(out=st[:, tb, :], in_=T_[:, 0:256]), reads=[bT], writes=[bst])
                    kb.dma(kb.sp, self.projTM[dst][lo:hi].rearrange("(tb p) c -> p tb c", p=128), st[:], sst, reads=[bst], writes=[b_pTM])
                else:
                    for tb in range(nbh):
                        p, bp = psr.next()
                        kb.mm(p[:, 0:256], [(hT[:, kc, tb * 128:(tb + 1) * 128], w[:, kc, :]) for kc in range(NDC)],
                              reads=[bw] + b_hT, writes=[bp])
                        if form == "TM":
                            if tb % 2 == 0:
                                kb.op(kb.act, lambda e: e.activation(out=st[:, tb, :], in_=p[:, 0:256], func=AF.Copy), reads=[bp], writes=[bst])
                            else:
                                kb.op(kb.dve, lambda e: e.tensor_copy(out=st[:, tb, :], in_=p[:, 0:256]), reads=[bp], writes=[bst])
                        else:
                            kb.op(kb.act, lambda e: e.activation(out=stG[:, tb, 0:16], in_=p[:, 0:16], func=AF.Copy), reads=[bp], writes=[b_stG])
                    if form == "TM":
                        kb.dma(kb.sp, self.projTM[dst][lo:hi].rearrange("(tb p) c -> p tb c", p=128), st[:], sst, reads=[bst], writes=[b_pTM])
                    else:
                        kb.dma(kb.sp, self.gatesTM[lo:hi].rearrange("(tb p) c -> p tb c", p=128), stG[:, :, 0:16], s_stG, reads=[b_stG], writes=[b_gT])
            ph.close()

    def phase_mix_ab(self):
        kb = self.kb
        b_pFM, b_pTM, b_gT, b_mix = (self.sbuf_of(n) for n in ("projFM", "projTM", "gatesTM", "mixTM"))
        ph = Phase(kb, "att")
        kT = ph.sb("kT", [128, T], BF16); b_kT = kb.buf("kT"); s_kT = kb.dsem("kT")
        va = ph.sb("va", [128, NB, 132], BF16); b_va = kb.buf("va"); s_va = kb.dsem("va")
        qr = Ring(kb, "aq", [ph.sb("q%d" % i, [128, T], BF16) for i in range(2)], with_sem=True)
        ost = Ring(kb, "aost", [ph.sb("ost%d" % i, [128, NB, 512], BF16) for i in range(2)], with_sem=True, multi=True)
        esk = ph.sb("esk", [128, 16], F32); b_esk = kb.buf("esk")
        kb.dma(kb.sp, esk[:], self.ab_sink.partition_broadcast(128), kb.dsem('m'), writes=[b_esk])
        kb.op(kb.act, lambda e: e.activation(out=esk[:], in_=esk[:], func=AF.Exp), reads=[b_esk], writes=[b_esk])
        psS = Ring(kb, "aS", [ph.ps("S%d" % i, [128, 512], F32) for i in range(4)])
        psN = Ring(kb, "aN", [ph.ps("N%d" % i, [128, 512], F32) for i in range(2)])
        PT = Ring(kb, "aPT", [ph.sb("PT%d" % i, [128, 5, 128], BF16) for i in range(3)])
        rr = Ring(kb, "arr", [ph.sb("rr%d" % i, [128, 2], F32) for i in range(3)])
        a_wr = Ring(kb, "adaw1", [ph.sb("aw%d" % i, [128, NDC, 512], BF16) for i in range(2)], with_sem=True)
        a_psr = Ring(kb, "adap1", [ph.ps("ap%d" % i, [128, 512], F32) for i in range(1)])
        a_ptr = Ring(kb, "adat1", [ph.ps("at%d" % i, [128, 512], F32) for i in range(1)])
        a_rowr = Ring(kb, "adar1", [ph.sb("arow%d" % i, [2, 512], F32) for i in range(2)])
        ada_g = 0
        for kvh in range(4):
            kb.dma(kb.sp, kT[:], self.projFM[16 + kvh], s_kT, reads=[b_pFM], writes=[b_kT])
            kb.op(kb.pool, lambda e: e.memset(va[:, :, 128:129], 1.0), writes=[b_va])
            kb.dma(kb.sp, va[:, :, 0:128], self.projTM[kvh // 2].rearrange("(tb p) c -> p tb c", p=128)[:, :, (kvh % 2) * 128:(kvh % 2 + 1) * 128],
                   s_va, reads=[b_pTM], writes=[b_va])
            o_t, b_o, s_o = ost.next()
            for hq in range(4):
                head = kvh * 4 + hq
                q, bq, sq = qr.next()
                kb.dma(kb.sp, q[:], self.projFM[head], sq, reads=[b_pFM], writes=[bq])
                for i in range(NB):
                    if i < 2:
                        js = [(0, None), (1, None)]
                    else:
                        js = [(0, None), (1, None)]
                        if i - 1 >= 2: js.append((i - 1, 3))
                        js.append((i, None))
                        if i + 1 < NB: js.append((i + 1, 2))
                    pt, bpt = PT.next()
                    for c0 in range(0, len(js), 4):
                        part = js[c0:c0 + 4]
                        S, bS = psS.next()
                        for jj, (j, mk) in enumerate(part):
                            kb.mm(S[:, jj * 128:(jj + 1) * 128], [(kT[:, j * 128:(j + 1) * 128], q[:, i * 128:(i + 1) * 128])],
                                  reads=[b_kT, bq], writes=[bS])
                        npc = len(part)
                        kb.op(kb.act, lambda e: e.activation(out=pt[:, c0:c0 + npc, :].rearrange("p a b -> p (a b)"), in_=S[:, 0:npc * 128], func=AF.Exp),
                              reads=[bS], writes=[bpt])
                    for jj, (j, mk) in enumerate(js):
                        if mk is not None:
                            kb.op(kb.dve, lambda e: e.tensor_tensor(out=pt[:, jj, :], in0=pt[:, jj, :], in1=self.cmb[:, mk, :], op=ALU.mult),
                                  reads=[bpt, self.b_cm], writes=[bpt])
                    N_, bN = psN.next()
                    kb.mm(N_[:, 0:129], [(pt[:, jj, :], va[:, j, 0:129]) for jj, (j, mk) in enumerate(js)], reads=[bpt, b_va], writes=[bN])
                    r_, br = rr.next()
                    kb.op(kb.dve, lambda e: e.tensor_scalar(out=r_[:, 0:1], in0=N_[:, 128:129], scalar1=esk[:, head:head + 1], scalar2=None, op0=ALU.add),
                          reads=[bN, b_esk], writes=[br])
                    kb.op(kb.dve, lambda e: e.reciprocal(out=r_[:, 1:2], in_=r_[:, 0:1]), reads=[br], writes=[br])
                    kb.op(kb.act, lambda e: e.activation(out=o_t[:, i, hq * 128:(hq + 1) * 128], in_=N_[:, 0:128], func=AF.Copy, scale=r_[:, 1:2]),
                          reads=[bN, br], writes=[b_o])
                    if i % 6 == 5 and ada_g < 48:
                        self.ada_group(1, ada_g, a_wr, a_psr, a_ptr, a_rowr)
                        ada_g += 1
            kb.dma(kb.sp, self.mixTM.rearrange("(tb p) c -> p tb c", p=128)[:, :, kvh * 512:(kvh + 1) * 512], o_t[:], s_o, reads=[b_o], writes=[b_mix])
        while ada_g < 48:
            self.ada_group(1, ada_g, a_wr, a_psr, a_ptr, a_rowr)
            ada_g += 1
        self.ada_derived(1)
        ph.close()
        ph = Phase(kb, "mls")
        G = ph.sb("G", [128, NB, 16], F32); b_G = kb.buf("G")
        gb = ph.sb("gb", [128, 16], F32); b_gb = kb.buf("gb")
        sp_ = ph.sb("sp", [128, NB, 16], F32); b_sp = kb.buf("sp")
        csum = ph.sb("csum", [128, NB, 8], F32); b_cs = kb.buf("csum")
        cb = ph.sb("cb", [128, NB, 8], F32); b_cb = kb.buf("cb")
        gain = ph.sb("gain", [128, 2048], F32); b_gain = kb.buf("gain")
        kb.dma(kb.sp, G[:], self.gatesTM.rearrange("(tb p) c -> p tb c", p=128), kb.dsem('m'), reads=[b_gT], writes=[b_G])
        kb.dma(kb.sp, gb[:], self.ab_gate_b.partition_broadcast(128), kb.dsem('m'), writes=[b_gb])
        kb.dma(kb.sp, gain[:], self.ab_head_norm.partition_broadcast(128), kb.dsem('m'), writes=[b_gain])
        for tb in range(NB):
            kb.op(kb.dve, lambda e: e.tensor_tensor(out=G[:, tb, :], in0=G[:, tb, :], in1=gb[:], op=ALU.add), reads=[b_gb, b_G], writes=[b_G])
        kb.op(kb.act, lambda e: e.activation(out=sp_[:], in_=G[:], func=AF.Exp, scale=-1.0), reads=[b_G], writes=[b_sp])
        kb.op(kb.act, lambda e: e.activation(out=sp_[:], in_=sp_[:], func=AF.Ln, bias=1.0), reads=[b_sp], writes=[b_sp])
        ORD = [list(range(NB)), [1, 0] + list(range(NB - 1, 1, -1))]
        MK = [2, 3]
        psC = ph.ps("C", [128, 512], F32); b_psC = kb.buf("psC")
        for d in range(2):
            for pos, i in enumerate(ORD[d]):
                pairs = [(self.cm[:, 1, :], sp_[:, j, 4 + 8 * d:8 + 8 * d]) for j in ORD[d][:pos]]
                pairs.append((self.cm[:, MK[d], :], sp_[:, i, 4 + 8 * d:8 + 8 * d]))
                kb.mm(psC[:, 0:4], pairs, reads=[b_sp, self.b_cm], writes=[b_psC])
                kb.op(kb.dve, lambda e: e.tensor_copy(out=csum[:, i, 4 * d:4 * d + 4], in_=psC[:, 0:4]), reads=[b_psC], writes=[b_cs])
                kb.op(kb.dve, lambda e: e.tensor_tensor(out=cb[:, i, 4 * d:4 * d + 4], in0=psC[:, 0:4], in1=G[:, i, 8 * d:8 * d + 4], op=ALU.add),
                      reads=[b_psC, b_G], writes=[b_cb])
        qT = ph.sb("qT", [128, 2, T], BF16); b_qT = kb.buf("qT"); s_qT = kb.dsem("qT")
        kT = ph.sb("kT", [128, 2, T], BF16); b_kT = kb.buf("kT"); s_kT = kb.dsem("kT")
        v = ph.sb("v", [128, NB, 512], BF16); b_v = kb.buf("v"); s_v = kb.dsem("v")
        og = ph.sb("og", [128, NB, 512], BF16); b_og = kb.buf("og"); s_og = kb.dsem("og")
        hsum = ph.sb("hsum", [128, NB, 512], F32); b_hs = [kb.buf("hs%d" % i) for i in range(NB)]
        brow = ph.sb("brow", [128, NB, 128], F32); b_brow = [kb.buf("brow%d" % i) for i in range(NB)]
        dg = Ring(kb, "dg", [ph.sb("dg%d" % i, [128, 128], F32) for i in range(2)])
        ost = ph.sb("ost", [128, NB, 512], BF16); b_ost = kb.buf("most", multi=True); s_ost = kb.dsem("most")
        ss = ph.sb("ss", [128, NB], F32); b_ss = kb.buf("ss")
        rs = ph.sb("rs", [128, NB], F32); b_rs = kb.buf("rs")
        junk = ph.sb("junk", [128, 512], F32); b_junk = kb.buf("junk")
        tmpo = Ring(kb, "tmpo", [ph.sb("tmpo%d" % i, [128, 512], F32) for i in range(2)])
        DT = Ring(kb, "DT", [ph.sb("DT%d" % i, [128, 4, 128], F32) for i in range(3)])
        PT = Ring(kb, "mPT", [ph.sb("PT%d" % i, [128, 4, 128], BF16) for i in range(3)])
        psS = Ring(kb, "mS", [ph.ps("S%d" % i, [128, 512], F32) for i in range(3)])
        psN = Ring(kb, "mN", [ph.ps("N%d" % i, [128, 512], F32) for i in range(2)])
        psD = Ring(kb, "mD", [ph.ps("D%d" % i, [128, 512], F32) for i in range(2)])
        rr = Ring(kb, "mrr", [ph.sb("rr%d" % i, [128, 2], F32) for i in range(3)])
        onesb = self.cmb[:, 1, 0:1]
        for h in range(4):
            for c in range(2):
                kb.dma(kb.sp, qT[:, c, :], self.projFM[20 + 2 * h + c], s_qT, reads=[b_pFM], writes=[b_qT])
                kb.dma(kb.sp, kT[:, c, :], self.projFM[28 + 2 * h + c], s_kT, reads=[b_pFM], writes=[b_kT])
                kb.dma(kb.sp, v[:, :, c * 256:(c + 1) * 256], self.projTM[2 + 2 * h + c].rearrange("(tb p) c -> p tb c", p=128), s_v,
                       reads=[b_pTM], writes=[b_v])
                kb.dma(kb.sp, og[:, :, c * 256:(c + 1) * 256], self.projTM[10 + 2 * h + c].rearrange("(tb p) c -> p tb c", p=128), s_og,
                       reads=[b_pTM], writes=[b_og])
            kb.op(kb.act, lambda e: e.activation(out=og[:], in_=og[:], func=AF.Sigmoid), reads=[b_og], writes=[b_og])
            for d in range(2):
                col = 4 * d + h
                for i in range(NB):
                    dt_, bdt = dg.next()
                    kb.op(kb.dve, lambda e: e.tensor_scalar(out=dt_[:], in0=self.cm[:, 0, :], scalar1=csum[:, i, col:col + 1], scalar2=None, op0=ALU.mult),
                          reads=[b_cs, self.b_cm], writes=[bdt])
                    kb.mm(psC[:, 0:128], [(self.cm[:, 1, :], dt_[:])], reads=[bdt, self.b_cm], writes=[b_psC])
                    kb.op(kb.act, lambda e: e.activation(out=brow[:, i, :], in_=psC[:, 0:128], func=AF.Copy), reads=[b_psC], writes=[b_brow[i]])
                for pos, i in enumerate(ORD[d]):
                    js = ORD[d][:pos + 1]
                    N_, bN = psN.next()
                    Dn, bDn = psD.next()
                    ngr = (len(js) + 3) // 4
                    for gi in range(ngr):
                        part = js[gi * 4:gi * 4 + 4]
                        npc = len(part)
                        S, bS = psS.next()
                        D_, bD = DT.next()
                        P_, bP = PT.next()
                        for jj, j in enumerate(part):
                            kb.mm(S[:, jj * 128:(jj + 1) * 128],
                                  [(kT[:, c, j * 128:(j + 1) * 128], qT[:, c, i * 128:(i + 1) * 128]) for c in range(2)],
                                  reads=[b_kT, b_qT], writes=[bS])
                            kb.op(kb.act, lambda e: e.activation(out=D_[:, jj, :], in_=brow[:, i, :], func=AF.Exp, scale=-1.0, bias=cb[:, j, col:col + 1]),
                                  reads=[b_brow[i], b_cb], writes=[bD])
                            if j == i:
                                kb.op(kb.pool, lambda e: e.tensor_tensor(out=D_[:, jj, :], in0=D_[:, jj, :], in1=self.cm[:, MK[d], :], op=ALU.mult),
                                      reads=[bD, self.b_cm], writes=[bD])
                        kb.op(kb.dve, lambda e: e.tensor_tensor(out=P_[:, 0:npc, :].rearrange("p a b -> p (a b)"), in0=S[:, 0:npc * 128],
                                                               in1=D_[:, 0:npc, :].rearrange("p a b -> p (a b)"), op=ALU.mult),
                              reads=[bS, bD], writes=[bP])
                        first = (gi == 0)
                        last = (gi == ngr - 1)
                        kb._waits(kb.pe, [bP, b_v], [bN, bDn] if first else [])
                        ins = None
                        for jj, j in enumerate(part):
                            kb.pe.e.matmul(N_[:, 0:512], lhsT=P_[:, jj, :], rhs=v[:, j, :], start=(first and jj == 0), stop=(last and jj == npc - 1))
                            ins = kb.pe.e.matmul(Dn[:, 0:1], lhsT=P_[:, jj, :], rhs=onesb, start=(first and jj == 0), stop=(last and jj == npc - 1))
                        tok = kb.pe.tick()
                        ins.then_inc(tok[0].h, 1)
                        kb._mark(tok, [bP, b_v], [bN, bDn])
                    r_, br = rr.next()
                    kb.op(kb.act, lambda e: e.activation(out=r_[:, 0:1], in_=Dn[:, 0:1], func=AF.Abs), reads=[bDn], writes=[br])
                    kb.op(kb.dve, lambda e: e.tensor_scalar_max(out=r_[:, 0:1], in0=r_[:, 0:1], scalar1=1.0), reads=[br], writes=[br])
                    kb.op(kb.dve, lambda e: e.reciprocal(out=r_[:, 1:2], in_=r_[:, 0:1]), reads=[br], writes=[br])
                    if d == 0:
                        kb.op(kb.act, lambda e: e.activation(out=hsum[:, i, :], in_=N_[:, 0:512], func=AF.Copy, scale=r_[:, 1:2]),
                              reads=[bN, br], writes=[b_hs[i]])
                    else:
                        kb.op(kb.dve, lambda e: e.scalar_tensor_tensor(out=hsum[:, i, :], in0=N_[:, 0:512], scalar=r_[:, 1:2], in1=hsum[:, i, :],
                                                                        op0=ALU.mult, op1=ALU.add), reads=[bN, br, b_hs[i]], writes=[b_hs[i]])
            kb.op(kb.dve, lambda e: e.memset(ss[:], 0.0), writes=[b_ss])
            for i in range(NB):
                kb.op(kb.act, lambda e: e.activation(out=junk[:], in_=hsum[:, i, :], func=AF.Square, accum_out=ss[:, i:i + 1]),
                      reads=[b_hs[i]], writes=[b_junk, b_ss])
            kb.op(kb.act, lambda e: e.activation(out=rs[:], in_=ss[:], func=AF.Sqrt, scale=1.0 / 512, bias=EPS), reads=[b_ss], writes=[b_rs])
            kb.op(kb.dve, lambda e: e.reciprocal(out=rs[:], in_=rs[:]), reads=[b_rs], writes=[b_rs])
            for i in range(NB):
                t_, bt = tmpo.next()
                kb.op(kb.dve, lambda e: e.scalar_tensor_tensor(out=t_[:], in0=hsum[:, i, :], scalar=rs[:, i:i + 1], in1=gain[:, h * 512:(h + 1) * 512],
                                                                op0=ALU.mult, op1=ALU.mult), reads=[b_hs[i], b_rs, b_gain], writes=[bt])
                kb.op(kb.pool, lambda e: e.tensor_tensor(out=ost[:, i, :], in0=t_[:], in1=og[:, i, :], op=ALU.mult), reads=[bt, b_og], writes=[b_ost])
            kb.dma(kb.sp, self.mixTM.rearrange("(tb p) c -> p tb c", p=128)[:, :, 2048 + h * 512:2048 + (h + 1) * 512], ost[:], s_ost,
                   reads=[b_ost], writes=[b_mix])
        ph.close()

    def phase_mix_gla(self):
        kb = self.kb
        b_pFM, b_pTM, b_rl, b_mix = (self.sbuf_of(n) for n in ("projFM", "projTM", "rlowFM", "mixTM"))
        ph = Phase(kb, "gla")
        ORD = [list(range(NB)), [1, 0] + list(range(NB - 1, 1, -1))]
        MK = [2, 3]
        raug = ph.sb("raug", [32, T], F32); b_raug = kb.buf("raug"); s_raug = kb.dsem("raug")
        gw = ph.sb("gw", [32, 256], F32); b_gw = kb.buf("gw"); s_gw = kb.dsem("gw")
        sp_ = ph.sb("sp", [128, NB, 256], F32); b_sp = [kb.buf("gsp%d" % i) for i in range(NB)]
        Gp = ph.sb("Gp", [128, 2, T], F32); b_Gp = kb.buf("Gp", multi=True)
        qT = ph.sb("qT", [128, 2, T], BF16); b_qT = kb.buf("gqT", multi=True); s_qT = kb.dsem("gqT")
        kT = ph.sb("kT", [128, 2, T], BF16); b_kT = kb.buf("gkT", multi=True); s_kT = kb.dsem("gkT")
        qd = ph.sb("qd", [128, 2, T], BF16); b_qd = kb.buf("qd", multi=True)
        ke = ph.sb("ke", [128, 2, T], BF16); b_ke = kb.buf("ke", multi=True)
        ki = ph.sb("ki", [128, 2, T], BF16); b_ki = kb.buf("ki", multi=True)
        v = ph.sb("v", [128, NB, 512], BF16); b_v = kb.buf("gv", multi=True); s_v = kb.dsem("gv")
        rg = ph.sb("rg", [128, NB, 512], BF16); b_rg = kb.buf("grg", multi=True); s_rg = kb.dsem("grg")
        hsum = ph.sb("hsum", [128, NB, 512], F32); b_hs = [kb.buf("ghs%d" % i) for i in range(NB)]
        gain = ph.sb("gain", [128, 512], F32); b_gain = kb.buf("ggain"); s_gain = kb.dsem("ggain")
        g16 = ph.sb("g16", [128, 4, 2, NB], F32); b_g16 = kb.buf("g16")
        gam = ph.sb("gam", [128, 2, NB], F32); b_gam = kb.buf("gam")
        gtm = ph.sb("gtm", [128, 2, NB], F32); b_gtm = kb.buf("gtm")
        ss = ph.sb("ss", [128, NB], F32); b_ss = kb.buf("gss")
        rs = ph.sb("rs", [128, NB], F32); b_rs = kb.buf("grs")
        junk = ph.sb("junk", [128, 512], F32); b_junk = kb.buf("gjunk")
        Er = Ring(kb, "gE", [ph.sb("E%d" % i, [128, 128], F32) for i in range(3)])
        PT = Ring(kb, "gPT", [ph.sb("PT%d" % i, [128, 128], BF16) for i in range(3)])
        keTr = Ring(kb, "gkeT", [ph.sb("keT%d" % i, [128, 256], BF16) for i in range(2)])
        S_f = ph.sb("S_f", [128, 2, 512], F32); S_b = ph.sb("S_b", [128, 2, 512], BF16)
        b_Sf = kb.buf("S_f"); b_Sb = kb.buf("S_b")
        gmm = ph.sb("gmm", [128, 2, NB], F32); b_gmm = kb.buf("gmm")
        tot = ph.sb("tot", [128, 2, NB], F32); gsr = ph.sb("gsr", [128, 2, NB], F32); b_tot = kb.buf("tot")
        tmpo = Ring(kb, "gtmpo", [ph.sb("tmpo%d" % i, [128, 512], F32) for i in range(2)])
        ost = Ring(kb, "gost", [ph.sb("ost%d" % i, [128, 512], BF16) for i in range(2)], with_sem=True)
        psZ = Ring(kb, "gZ", [ph.ps("Z%d" % i, [128, 512], F32) for i in range(2)])
        psS = Ring(kb, "gS", [ph.ps("S%d" % i, [128, 512], F32) for i in range(2)])
        psT = Ring(kb, "gT", [ph.ps("T%d" % i, [128, 512], BF16) for i in range(1)])
        psN = Ring(kb, "gN", [ph.ps("N%d" % i, [128, 512], F32) for i in range(2)])
        mixv = self.mixTM.rearrange("(tb p) c -> p tb c", p=128)
        for h in range(8):
            for c in range(2):
                kb.dma(kb.sp, v[:, :, c * 256:(c + 1) * 256], self.projTM[2 * h + c].rearrange("(tb p) c -> p tb c", p=128), s_v, reads=[b_pTM], writes=[b_v])
                kb.dma(kb.sp, rg[:, :, c * 256:(c + 1) * 256], self.projTM[16 + 2 * h + c].rearrange("(tb p) c -> p tb c", p=128), s_rg, reads=[b_pTM], writes=[b_rg])
            kb.op(kb.act, lambda e: e.activation(out=rg[:], in_=rg[:], func=AF.Silu), reads=[b_rg], writes=[b_rg])
            kb.dma(kb.sp, gain[:], self.gla_head_norm[:, h * 512:(h + 1) * 512].partition_broadcast(128), s_gain, writes=[b_gain])
            for d in range(2):
                kb.op(kb.dve, lambda e: e.memset(raug[:], 1.0), writes=[b_raug])
                kb.dma(kb.sp, raug[0:16, :], self.rlowFM[16 * d:16 * d + 16, :], s_raug, reads=[b_rl], writes=[b_raug])
                kb.dma(kb.sp, gw[0:17, :], self.gla_gw2[d][:, h * 256:(h + 1) * 256], s_gw, writes=[b_gw])
                for i in range(NB):
                    Z, bZ = psZ.next()
                    kb.mm(Z[:, 0:256], [(raug[0:17, i * 128:(i + 1) * 128], gw[0:17, :])], reads=[b_raug, b_gw], writes=[bZ])
                    kb.op(kb.act, lambda e: e.activation(out=sp_[:, i, :], in_=Z[:, 0:256], func=AF.Exp, scale=-1.0), reads=[bZ], writes=[b_sp[i]])
                    kb.op(kb.act, lambda e: e.activation(out=sp_[:, i, :], in_=sp_[:, i, :], func=AF.Ln, bias=1.0), reads=[b_sp[i]], writes=[b_sp[i]])
                for c in range(2):
                    Z, bZ = psZ.next()
                    for i in range(NB):
                        kb.mm(Z[:, i:i + 1], [(sp_[:, i, c * 128:(c + 1) * 128], self.cm[:, 1, 0:1])], reads=[b_sp[i], self.b_cm], writes=[bZ])
                    kb.op(kb.dve, lambda e: e.tensor_copy(out=tot[:, c, :], in_=Z[:, 0:NB]), reads=[bZ], writes=[b_tot])
                    prev = None
                    for i in ORD[d]:
                        if prev is None:
                            kb.op(kb.dve, lambda e: e.memset(gsr[:, c, i:i + 1], 0.0), writes=[b_tot])
                        else:
                            kb.op(kb.dve, lambda e: e.tensor_tensor(out=gsr[:, c, i:i + 1], in0=gsr[:, c, prev:prev + 1], in1=tot[:, c, prev:prev + 1], op=ALU.add),
                                  reads=[b_tot], writes=[b_tot])
                        prev = i
                kb.op(kb.dve, lambda e: e.tensor_scalar(out=g16[:, 0, :, :], in0=gsr[:], scalar1=1.0 / 16, scalar2=None, op0=ALU.mult), reads=[b_tot], writes=[b_g16])
                kb.op(kb.dve, lambda e: e.tensor_tensor(out=tot[:], in0=tot[:], in1=gsr[:], op=ALU.add), reads=[b_tot], writes=[b_tot])
                kb.op(kb.dve, lambda e: e.tensor_scalar(out=g16[:, 1, :, :], in0=tot[:], scalar1=1.0 / 16, scalar2=None, op0=ALU.mult), reads=[b_tot], writes=[b_g16])
                for c in range(2):
                    for i in range(NB):
                        Z, bZ = psZ.next()
                        kb.mm(Z[:, 0:128], [(sp_[:, i, c * 128:(c + 1) * 128], self.cm[:, MK[d], :])], reads=[b_sp[i], self.b_cm], writes=[bZ])
                        dst = Gp[:, c, i * 128:(i + 1) * 128]
                        if i % 2 == 0:
                            kb.op(kb.dve, lambda e: e.tensor_scalar(out=dst, in0=Z[:, 0:128], scalar1=gsr[:, c, i:i + 1], scalar2=None, op0=ALU.add),
                                  reads=[bZ, b_tot], writes=[b_Gp])
                        else:
                            kb.op(kb.act, lambda e: e.activation(out=dst, in_=Z[:, 0:128], func=AF.Identity, bias=gsr[:, c, i:i + 1]),
                                  reads=[bZ, b_tot], writes=[b_Gp])
                kb.op(kb.dve, lambda e: e.tensor_scalar(out=g16[:, 2:4, :, :], in0=g16[:, 0:2, :, :], scalar1=-1.0, scalar2=None, op0=ALU.mult),
                      reads=[b_g16], writes=[b_g16])
                kb.op(kb.dve, lambda e: e.tensor_tensor(out=gmm[:], in0=g16[:, 0, :, :], in1=g16[:, 1, :, :], op=ALU.subtract), reads=[b_g16], writes=[b_gmm])
                kb.op(kb.act, lambda e: e.activation(out=gmm[:], in_=gmm[:], func=AF.Exp), reads=[b_gmm], writes=[b_gmm])
                for c in range(2):
                    kb.dma(kb.sp, qT[:, c, :], self.projFM[2 * h + c], s_qT, reads=[b_pFM], writes=[b_qT])
                    kb.dma(kb.sp, kT[:, c, :], self.projFM[16 + 2 * h + c], s_kT, reads=[b_pFM], writes=[b_kT])
                for c in range(2):
                    for i in range(NB):
                        blk = slice(i * 128, (i + 1) * 128)
                        for which, src, dstt, bd, sc_, bias in ((0, qT, qd, b_qd, -1.0 / 16, g16[:, 0, c, i:i + 1]),
                                                               (1, kT, ke, b_ke, 1.0 / 16, g16[:, 3, c, i:i + 1]),
                                                               (2, kT, ki, b_ki, 1.0 / 16, g16[:, 2, c, i:i + 1])):
                            E, bE = Er.next()
                            kb.op(kb.act, lambda e: e.activation(out=E[:], in_=Gp[:, c, blk], func=AF.Exp, scale=sc_, bias=bias),
                                  reads=[b_Gp, b_g16], writes=[bE])
                            kb.op(kb.dve, lambda e: e.tensor_tensor(out=dstt[:, c, blk], in0=src[:, c, blk], in1=E[:], op=ALU.mult),
                                  reads=[bE, b_qT if which == 0 else b_kT], writes=[bd])
                kb.op(kb.dve, lambda e: e.memset(S_f[:], 0.0), writes=[b_Sf])
                for pos, i in enumerate(ORD[d]):
                    blk = slice(i * 128, (i + 1) * 128)
                    S, bS = psS.next()
                    kb.mm(S[:, 0:128], [(ki[:, c, blk], qd[:, c, blk]) for c in range(2)], reads=[b_ki, b_qd], writes=[bS])
                    P_, bP = PT.next()
                    kb.op(kb.dve, lambda e: e.tensor_tensor(out=P_[:], in0=S[:, 0:128], in1=self.cm[:, MK[d], :], op=ALU.mult),
                          reads=[bS, self.b_cm], writes=[bP])
                    N_, bN = psN.next()
                    pairs = [(P_[:], v[:, i, :])]
                    rd = [bP, b_v]
                    if pos > 0:
                        pairs += [(qd[:, c, blk], S_b[:, c, :]) for c in range(2)]
                        rd += [b_qd, b_Sb]
                    kb.mm(N_[:, 0:512], pairs, reads=rd, writes=[bN])
                    if d == 0:
                        kb.op(kb.act, lambda e: e.activation(out=hsum[:, i, :], in_=N_[:, 0:512], func=AF.Copy), reads=[bN], writes=[b_hs[i]])
                    else:
                        kb.op(kb.dve, lambda e: e.tensor_tensor(out=hsum[:, i, :], in0=N_[:, 0:512], in1=hsum[:, i, :], op=ALU.add),
                              reads=[bN, b_hs[i]], writes=[b_hs[i]])
                    if pos < NB - 1:
                        T_, bT = psT.next()
                        for c in range(2):
                            kb.op(kb.pe, lambda e: e.transpose(out=T_[:, c * 128:(c + 1) * 128], in_=ke[:, c, blk], identity=self.cmb[:, 0, :]),
                                  reads=[b_ke, self.b_cm], writes=[bT])
                        keT, bkeT = keTr.next()
                        kb.op(kb.act, lambda e: e.activation(out=keT[:], in_=T_[:, 0:256], func=AF.Copy), reads=[bT], writes=[bkeT])
                        for c in range(2):
                            U, bU = psZ.next()
                            kb.mm(U[:, 0:512], [(keT[:, c * 128:(c + 1) * 128], v[:, i, :])], reads=[bkeT, b_v], writes=[bU])
                            kb.op(kb.dve, lambda e: e.scalar_tensor_tensor(out=S_f[:, c, :], in0=S_f[:, c, :], scalar=gmm[:, c, i:i + 1], in1=U[:, 0:512],
                                                                            op0=ALU.mult, op1=ALU.add), reads=[bU, b_gmm, b_Sf], writes=[b_Sf])
                            kb.op(kb.act, lambda e: e.activation(out=S_b[:, c, :], in_=S_f[:, c, :], func=AF.Copy), reads=[b_Sf], writes=[b_Sb])
            kb.op(kb.dve, lambda e: e.memset(ss[:], 0.0), writes=[b_ss])
            for i in range(NB):
                kb.op(kb.act, lambda e: e.activation(out=junk[:], in_=hsum[:, i, :], func=AF.Square, accum_out=ss[:, i:i + 1]),
                      reads=[b_hs[i]], writes=[b_junk, b_ss])
            kb.op(kb.act, lambda e: e.activation(out=rs[:], in_=ss[:], func=AF.Sqrt, scale=1.0 / 512, bias=EPS), reads=[b_ss], writes=[b_rs])
            kb.op(kb.dve, lambda e: e.reciprocal(out=rs[:], in_=rs[:]), reads=[b_rs], writes=[b_rs])
            for i in range(NB):
                t_, bt = tmpo.next()
                o_, bo, so = ost.next()
                kb.op(kb.dve, lambda e: e.scalar_tensor_tensor(out=t_[:], in0=hsum[:, i, :], scalar=rs[:, i:i + 1], in1=gain[:], op0=ALU.mult, op1=ALU.mult),
                      reads=[b_hs[i], b_rs, b_gain], writes=[bt])
                kb.op(kb.pool, lambda e: e.tensor_tensor(out=o_[:], in0=t_[:], in1=rg[:, i, :], op=ALU.mult), reads=[bt, b_rg], writes=[bo])
                kb.dma(kb.sp, mixv[:, i, h * 512:(h + 1) * 512], o_[:], so, reads=[bo], writes=[b_mix])
        ph.close()

    def phase_outproj(self, l, skip_ctx=False):
        kb = self.kb
        w_dram = self.ab_w_out if l == 0 else self.gla_w_out
        b_mix, b_y = self.sbuf_of("mixTM"), self.sbuf_of("yscr")
        HALVES = [(0, 1152, [(0, 512), (512, 1024), (1024, 1152)]), (1152, 2304, [(1152, 1664), (1664, 2176), (2176, 2304)])]
        if skip_ctx:
            HALVES[0] = (0, 1152, [(256, 768), (768, 1152)])
        for hi_, (lo, hi, tiles) in enumerate(HALVES):
            nh = hi - lo
            tg = "%d_%d" % (l, hi_)
            ph = Phase(kb, "op" + tg)
            mixT = ph.sb("mixT", [128, NDC, nh], BF16)
            b_mT = [kb.buf("mixT%d" % i) for i in range(nh // 128)]
            rowr = Ring(kb, "oprow" + tg, [ph.sb("row%d" % i, [128, D], BF16) for i in range(2)], with_sem=True)
            pst = Ring(kb, "oppt" + tg, [ph.ps("pt%d" % i, [128, 512], BF16) for i in range(2)])
            for tb in range(nh // 128):
                if skip_ctx and lo + tb * 128 < LC:
                    continue
                row, brow_, srow = rowr.next()
                kb.dma(kb.sp, row[:], self.mixTM[lo + tb * 128:lo + (tb + 1) * 128, :], srow, reads=[b_mix], writes=[brow_])
                for c4 in range(8):
                    pt, bpt = pst.next()
                    for k in range(4):
                        c = c4 * 4 + k
                        kb.op(kb.pe, lambda e: e.transpose(out=pt[:, k * 128:(k + 1) * 128], in_=row[:, c * 128:(c + 1) * 128], identity=self.cmb[:, 0, :]),
                              reads=[brow_, self.b_cm], writes=[bpt])
                    dst = mixT[:, c4 * 4:(c4 + 1) * 4, tb * 128:(tb + 1) * 128]
                    src = pt[:, :].rearrange("p (a b) -> p a b", a=4)
                    if c4 % 2 == 0:
                        kb.op(kb.act, lambda e: e.activation(out=dst, in_=src, func=AF.Copy), reads=[bpt], writes=[b_mT[tb]])
                    else:
                        kb.op(kb.dve, lambda e: e.tensor_copy(out=dst, in_=src), reads=[bpt], writes=[b_mT[tb]])
            wr = Ring(kb, "opw" + tg, [ph.sb("w%d" % i, [128, NDC, 128], BF16) for i in range(2)], with_sem=True)
            psr = Ring(kb, "opps" + tg, [ph.ps("p%d" % i, [128, 512], F32) for i in range(6)])
            yst = Ring(kb, "opy" + tg, [ph.sb("y%d" % i, [128, nh], F32) for i in range(2)], with_sem=True, multi=True)
            oacc = ph.sb("oacc", [128, nh], F32); osq = ph.sb("osq", [128, nh], F32)
            b_oacc, b_osq = kb.buf("oacc"), kb.buf("osq")
            for dc in range(NDC):
                w, bw, sw = wr.next()
                kb.dma(kb.pool, w[:], w_dram[dc], sw, writes=[bw])
                y, by, sy = yst.next()
                pbs = [psr.next() for _ in tiles]
                kb.mm_multi([pb[0][:, 0:t1 - t0] for pb, (t0, t1) in zip(pbs, tiles)], NDC,
                            lambda kc: w[:, kc, :], lambda kc, ti: mixT[:, kc, tiles[ti][0] - lo:tiles[ti][1] - lo],
                            reads=[bw] + b_mT, writes=[pb[1] for pb in pbs])
                for ti, (t0, t1) in enumerate(tiles):
                    n = t1 - t0
                    p, bp = pbs[ti]
                    if ti % 2 == 0:
                        kb.op(kb.act, lambda e: e.activation(out=y[:, t0 - lo:t1 - lo], in_=p[:, 0:n], func=AF.Copy), reads=[bp], writes=[by])
                    else:
                        kb.op(kb.dve, lambda e: e.tensor_copy(out=y[:, t0 - lo:t1 - lo], in_=p[:, 0:n]), reads=[bp], writes=[by])
                y0 = tiles[0][0]
                kb.dma(kb.sp, self.yscr[dc][:, y0:hi], y[:, y0 - lo:], sy, reads=[by], writes=[b_y])
                if dc == 0:
                    kb.op(kb.act, lambda e: e.activation(out=oacc[:, y0 - lo:], in_=y[:, y0 - lo:], func=AF.Square), reads=[by], writes=[b_oacc])
                else:
                    kb.op(kb.act, lambda e: e.activation(out=osq[:, y0 - lo:], in_=y[:, y0 - lo:], func=AF.Square), reads=[by], writes=[b_osq])
                    kb.op(kb.dve, lambda e: e.tensor_tensor(out=oacc[:, y0 - lo:], in0=oacc[:, y0 - lo:], in1=osq[:, y0 - lo:], op=ALU.add),
                          reads=[b_osq], writes=[b_oacc])
            self.rstd_row(psr, oacc[:, y0 - lo:], b_oacc, hi - y0, osq, b_osq, y0)
            ph.close()

    def rstd_row(self, ph_ring, acc, b_acc, n, tmp, b_tmp, c_lo):
        kb = self.kb
        for c0 in range(0, n, 512):
            c1 = min(n, c0 + 512)
            p, bp = ph_ring.next()
            kb.mm(p[:, 0:c1 - c0], [(self.cm[:, 1, :], acc[:, c0:c1])], reads=[self.b_cm, b_acc], writes=[bp])
            kb.op(kb.act, lambda e: e.activation(out=tmp[0:1, c0:c1], in_=p[0:1, 0:c1 - c0], func=AF.Sqrt, scale=1.0 / D, bias=EPS),
                  reads=[bp], writes=[b_tmp])
        kb.op(kb.dve, lambda e: e.reciprocal(out=tmp[0:1, 0:n], in_=tmp[0:1, 0:n]), reads=[b_tmp], writes=[b_tmp])
        kb.dma(kb.sp, self.rstdy[:, c_lo:c_lo + n], tmp[0:1, 0:n], kb.dsem("ry"), reads=[b_tmp], writes=[self.sbuf_of("rstdy")])

    def phase_resid(self, l, kgate, xsrc, b_xsrc, xdst, b_xdst, final=False):
        kb = self.kb
        ph = Phase(kb, "res%d_%d" % (l, kgate))
        b_y = self.sbuf_of("yscr")
        yr = Ring(kb, "ry", [ph.sb("y%d" % i, [128, T], F32) for i in range(2)], with_sem=True)
        xr = Ring(kb, "rx", [ph.sb("x%d" % i, [128, T], F32) for i in range(2)], with_sem=True)
        orr = Ring(kb, "ro", [ph.sb("o%d" % i, [128, T], F32) for i in range(2)], with_sem=True, multi=True)
        acc = ph.sb("acc", [128, T], F32); tmp = ph.sb("tmp", [128, T], F32); rstd = ph.sb("rstd", [128, T], F32)
        b_acc, b_tmp, b_rstd = kb.buf("racc"), kb.buf("rtmp"), kb.buf("rrstd")
        pss = [ph.ps("ps%d" % i, [128, 512], F32) for i in range(2)]
        b_pss = [kb.buf("rps%d" % i) for i in range(2)]
        kb.dma(kb.sp, rstd[:], self.rstdy.partition_broadcast(128), kb.dsem("ryl"), reads=[self.sbuf_of("rstdy")], writes=[b_rstd])
        def _load(dcc):
            y_, by_, sy_ = yr.next()
            x_, bx_, sx_ = xr.next()
            kb.dma(kb.sp, y_[:], self.yscr[dcc], sy_, reads=[b_y], writes=[by_])
            kb.dma(kb.sp, x_[:], xsrc[dcc], sx_, reads=[b_xsrc], writes=[bx_])
            return (y_, by_, x_, bx_)
        nxt = _load(0)
        for dc in range(NDC):
            y, by, x, bx = nxt
            if dc + 1 < NDC:
                nxt = _load(dc + 1)
            o, bo, so = orr.next()
            kb.op(kb.dve, lambda e: e.tensor_tensor(out=y[:], in0=y[:], in1=rstd[:], op=ALU.mult), reads=[b_rstd, by], writes=[by])
            for cl, a0, a1 in cls_cols(0, T):
                kb.op(kb.dve, lambda e: e.scalar_tensor_tensor(out=o[:, a0:a1], in0=y[:, a0:a1], scalar=self.der[:, l, kgate, dc, cl:cl + 1],
                                                                in1=x[:, a0:a1], op0=ALU.mult, op1=ALU.add),
                      reads=[by, bx, self.b_der], writes=[bo])
            if final:
                kb.dma(kb.sp, self.out[dc], o[:, LC:T], so, reads=[bo])
            else:
                kb.dma(kb.sp, xdst[dc], o[:], so, reads=[bo], writes=[b_xdst])
        ph.close()

    def phase_ffn_up(self, l, xsrc, b_x, skip_ctx=False):
        kb = self.kb
        b_act = self.sbuf_of("actscr")
        HALVES = [
            (0, 1153, [(0, 256), (256, 768), (768, 1153)], lambda t: (t + 1) if t < LC else (t + 2), 1156, [(1, 257, 0), (258, 1154, 256)]),
            (1151, 2304, [(1151, 1663), (1663, 2175), (2175, 2304)], lambda t: t - 1151, 1154, [(1, 1153, 1152)]),
        ]
        if skip_ctx:
            h0 = HALVES[0]
            HALVES[0] = (h0[0], h0[1], h0[2][1:], h0[3], h0[4], h0[5][1:])
        for hi_, (lo, hi, tiles, colof, ncols, outs) in enumerate(HALVES):
            nh = hi - lo
            tg = "%d_%d" % (l, hi_)
            ph = Phase(kb, "fu" + tg)
            hT = ph.sb("hT", [128, NDC, nh], BF16)
            b_hT = [kb.buf("fhT%d" % i) for i in range(NDC)]
            ph2 = Phase(kb, "fun" + tg)
            self.norm_mod(ph2, xsrc, b_x, l, 3, 4, hT, b_hT, lo, hi)
            ph2.close()
            cw = ph.sb("cw", [128, 3, 2 * NFC], F32); cbt = ph.sb("cb", [128, 2 * NFC], F32)
            b_cw = kb.buf("cw")
            kb.dma(kb.sp, cw[:], self.ffn_cw[l], kb.dsem('m'), writes=[b_cw])
            kb.dma(kb.sp, cbt[:], self.ffn_cb[l], kb.dsem('m'), writes=[b_cw])
            wr = Ring(kb, "fuw" + tg, [ph.sb("w%d" % i, [128, NDC, 128], BF16) for i in range(4)], with_sem=True)
            psr = Ring(kb, "fups" + tg, [ph.ps("p%d" % i, [128, 512], F32) for i in range(6)])
            ur = Ring(kb, "fuu" + tg, [ph.sb("u%d" % i, [128, ncols], F32) for i in range(2)], multi=True)
            cgr = Ring(kb, "fucg" + tg, [ph.sb("cg%d" % i, [128, ncols], F32) for i in range(2)])
            sg = ph.sb("sg", [128, ncols], F32); b_sg = kb.buf("sg")
            ast = Ring(kb, "fua" + tg, [ph.sb("a%d" % i, [128, ncols], BF16) for i in range(2)], with_sem=True)
            for u in ur.tiles:
                kb.op(kb.pool, lambda e: e.memset(u[:], 0.0), writes=[ur.bufs[ur.tiles.index(u)]])
            W_ = ncols - 2
            for c in range(NFC):
                cgs = []
                for kind in range(2):
                    ch = c + kind * NFC
                    w, bw, sw = wr.next()
                    kb.dma(kb.pool, w[:], self.ffn_w_up[l][ch], sw, writes=[bw])
                    u, bu = ur.next()
                    pbs = [psr.next() for _ in tiles]
                    kb.mm_multi([pb[0][:, 0:t1 - t0] for pb, (t0, t1) in zip(pbs, tiles)], NDC,
                                lambda kc: w[:, kc, :], lambda kc, ti: hT[:, kc, tiles[ti][0] - lo:tiles[ti][1] - lo],
                                reads=[bw] + b_hT, writes=[pb[1] for pb in pbs])
                    for ti, (t0, t1) in enumerate(tiles):
                        n = t1 - t0
                        p, bp = pbs[ti]
                        c0 = colof(t0)
                        kb.op(kb.act, lambda e: e.activation(out=u[:, c0:c0 + n], in_=p[:, 0:n], func=AF.Copy), reads=[bp], writes=[bu])
                    cg, bcg = cgr.next()
                    kb.op(kb.dve, lambda e: e.tensor_scalar(out=cg[:, 0:W_], in0=u[:, 1:1 + W_], scalar1=cw[:, 1, ch:ch + 1], scalar2=cbt[:, ch:ch + 1],
                                                           op0=ALU.mult, op1=ALU.add), reads=[bu, b_cw], writes=[bcg])
                    kb.op(kb.dve, lambda e: e.scalar_tensor_tensor(out=cg[:, 0:W_], in0=u[:, 0:W_], scalar=cw[:, 0, ch:ch + 1], in1=cg[:, 0:W_],
                                                                    op0=ALU.mult, op1=ALU.add), reads=[bu, b_cw, bcg], writes=[bcg])
                    kb.op(kb.dve, lambda e: e.scalar_tensor_tensor(out=cg[:, 0:W_], in0=u[:, 2:2 + W_], scalar=cw[:, 2, ch:ch + 1], in1=cg[:, 0:W_],
                                                                    op0=ALU.mult, op1=ALU.add), reads=[bu, b_cw, bcg], writes=[bcg])
                    cgs.append((cg, bcg))
                (cgg, bcgg), (cgv, bcgv) = cgs
                kb.op(kb.act, lambda e: e.activation(out=sg[:, 0:W_], in_=cgg[:, 0:W_], func=AF.Silu), reads=[bcgg], writes=[b_sg])
                a, ba, sa = ast.next()
                kb.op(kb.dve, lambda e: e.tensor_tensor(out=a[:, 0:W_], in0=sg[:, 0:W_], in1=cgv[:, 0:W_], op=ALU.mult), reads=[b_sg, bcgv], writes=[ba])
                for (oc0, oc1, tk0) in outs:
                    kb.dma(kb.sp, self.actscr[c][:, tk0:tk0 + (oc1 - oc0)], a[:, oc0 - 1:oc1 - 1], sa, reads=[ba], writes=[b_act])
            ph.close()

    def phase_ffn_down(self, l, skip_ctx=False):
        kb = self.kb
        b_act, b_y = self.sbuf_of("actscr"), self.sbuf_of("yscr")
        ph = Phase(kb, "fd%d" % l)
        at = ph.sb("at", [128, NFC, 512], BF16); b_at = kb.buf("at", multi=True); s_at = kb.dsem("at")
        wr = Ring(kb, "fdw%d" % l, [ph.sb("w%d" % i, [128, NFC, 128], BF16) for i in range(2)], with_sem=True)
        psr = Ring(kb, "fdps%d" % l, [ph.ps("p%d" % i, [128, 512], F32) for i in range(4)])
        yst = Ring(kb, "fdy%d" % l, [ph.sb("y%d" % i, [128, 512], F32) for i in range(3)], with_sem=True)
        facc = ph.sb("facc", [128, 512], F32); fsq = ph.sb("fsq", [128, 512], F32)
        b_facc, b_fsq = kb.buf("facc"), kb.buf("fsq")
        for (t0, t1) in (TT[1:] if skip_ctx else TT):
            n = t1 - t0
            for c0 in range(0, NFC, 8):
                c1 = min(NFC, c0 + 8)
                kb.dma(kb.sp, at[:, c0:c1, 0:n], self.actscr[c0:c1, :, t0:t1].rearrange("c p t -> p c t"), s_at, reads=[b_act], writes=[b_at])
            for dc in range(NDC):
                w, bw, sw = wr.next()
                kb.dma(kb.pool, w[:], self.ffn_w_down[l][dc], sw, writes=[bw])
                p, bp = psr.next()
                kb.mm(p[:, 0:n], [(w[:, kc, :], at[:, kc, 0:n]) for kc in range(NFC)], reads=[bw, b_at], writes=[bp])
                y, by, sy = yst.next()
                if dc % 2 == 0:
                    kb.op(kb.act, lambda e: e.activation(out=y[:, 0:n], in_=p[:, 0:n], func=AF.Copy), reads=[bp], writes=[by])
                else:
                    kb.op(kb.dve, lambda e: e.tensor_copy(out=y[:, 0:n], in_=p[:, 0:n]), reads=[bp], writes=[by])
                kb.dma(kb.sp, self.yscr[dc][:, t0:t1], y[:, 0:n], sy, reads=[by], writes=[b_y])
                if dc == 0:
                    kb.op(kb.act, lambda e: e.activation(out=facc[:, 0:n], in_=y[:, 0:n], func=AF.Square), reads=[by], writes=[b_facc])
                else:
                    kb.op(kb.act, lambda e: e.activation(out=fsq[:, 0:n], in_=y[:, 0:n], func=AF.Square), reads=[by], writes=[b_fsq])
                    kb.op(kb.dve, lambda e: e.tensor_tensor(out=facc[:, 0:n], in0=facc[:, 0:n], in1=fsq[:, 0:n], op=ALU.add),
                          reads=[b_fsq], writes=[b_facc])
            self.rstd_row(psr, facc, b_facc, n, fsq, b_fsq, t0)
        ph.close()

    def finish(self):
        kb = self.kb
        kb.barrier()
        SRC = {"projFM": self.projFM, "projTM": self.projTM, "gatesTM": self.gatesTM, "mixTM": self.mixTM,
               "xA": self.xA, "xB": self.xB, "yscr": self.yscr, "actscr": self.actscr, "rlowFM": self.rlowFM}
        for name, shape, dtp in self.dbg:
            if name in SRC:
                kb.dma(kb.sp, self.dbgout[name], SRC[name], self.s_out)
            elif name == "mods":
                kb.dma(kb.sp, self.dbgout[name], self.mods[:], self.s_out)
            elif name == "der":
                kb.dma(kb.sp, self.dbgout[name], self.der[:], self.s_out)
        if self.s_out.v > 0:
            kb.sp.e.wait_ge(self.s_out.h, self.s_out.v)


def _rope_tables():
    rows = LL // 64
    row = np.repeat(np.arange(rows), 64).astype(np.float32)
    col = np.tile(np.arange(64), rows).astype(np.float32)
    inv = (10000.0 ** (-np.arange(32, dtype=np.float32) / 32)).astype(np.float32)
    ar = row[:, None] * inv
    ac = col[:, None] * inv
    C = np.concatenate([np.cos(ar), np.cos(ar), np.cos(ac), np.cos(ac)], axis=1).T
    S = np.concatenate([np.sin(ar), np.sin(ar), np.sin(ac), np.sin(ac)], axis=1).T
    return np.ascontiguousarray(C, np.float32), np.ascontiguousarray(S, np.float32)


def _cmat():
    m = np.zeros((6, 128, 128), np.float32)
    m[0] = np.eye(128)
    m[1] = 1.0
    idx = np.arange(128)
    m[2] = (idx[:, None] <= idx[None, :])
    m[3] = (idx[:, None] >= idx[None, :])
    R = np.zeros((128, 128), np.float32)
    for base in (0, 64):
        for i in range(32):
            R[base + i, base + 32 + i] = -1.0
            R[base + 32 + i, base + i] = 1.0
    m[4] = R.T
    return m


def _grp(w, ng):
    k, n = w.shape
    wp = np.zeros((k, ng * 256), np.float32)
    wp[:, :n] = w
    return np.ascontiguousarray(wp.reshape(NDC, 128, ng, 256).transpose(2, 1, 0, 3))


def _blk(w, kc, nc_):
    return np.ascontiguousarray(w.reshape(kc, 128, nc_, 128).transpose(2, 1, 0, 3))


def prep_shared(inp, names):
    sh = {}
    f = lambda a: np.asarray(a, np.float32)
    if "ada_w" in names: sh["ada_w"] = f(inp["ada_w"])
    if "ada_b" in names: sh["ada_b"] = np.ascontiguousarray(f(inp["ada_b"]).reshape(2, 192, 128).transpose(0, 2, 1))
    if "norm_w" in names: sh["norm_w"] = np.ascontiguousarray(f(inp["norm_w"]).reshape(2, 4, NDC, 128).transpose(0, 3, 1, 2))
    if "ab_w_in" in names: sh["ab_w_in"] = _grp(f(inp["ab_w_in"])[0], AB_G)
    if "ab_gate_b" in names: sh["ab_gate_b"] = f(inp["ab_gate_b"]).reshape(1, 16)
    if "ab_sink" in names: sh["ab_sink"] = f(inp["ab_sink"]).reshape(1, 16)
    if "ab_head_norm" in names: sh["ab_head_norm"] = f(inp["ab_head_norm"]).reshape(1, 2048)
    if "ab_w_out" in names: sh["ab_w_out"] = _blk(f(inp["ab_w_out"])[0], NDC, NDC)
    if "gla_w_in" in names: sh["gla_w_in"] = _grp(f(inp["gla_w_in"])[0], GLA_G)
    if "gla_gw2" in names:
        sh["gla_gw2"] = np.ascontiguousarray(np.concatenate([f(inp["gla_gate_w2"])[0], f(inp["gla_gate_b"])[0][:, None, :]], axis=1))
    if "gla_head_norm" in names: sh["gla_head_norm"] = f(inp["gla_head_norm"]).reshape(1, 4096)
    if "gla_w_out" in names: sh["gla_w_out"] = _blk(f(inp["gla_w_out"])[0], NDC, NDC)
    if "ffn_w_up" in names:
        sh["ffn_w_up"] = np.stack([_blk(f(inp["ffn_w_up"])[l], NDC, 2 * NFC) for l in range(2)])
    if "ffn_cw" in names:
        sh["ffn_cw"] = np.ascontiguousarray(f(inp["ffn_conv_w"]).reshape(2, 3, 2 * NFC, 128).transpose(0, 3, 1, 2))
    if "ffn_cb" in names:
        sh["ffn_cb"] = np.ascontiguousarray(f(inp["ffn_conv_b"]).reshape(2, 2 * NFC, 128).transpose(0, 2, 1))
    if "ffn_w_down" in names:
        sh["ffn_w_down"] = np.stack([_blk(f(inp["ffn_w_down"])[l], NFC, NDC) for l in range(2)])
    C, S = _rope_tables()
    sh["ropeC"], sh["ropeS"], sh["cmat"] = C, S, _cmat()
    return sh


def prep_core(inp, b, sh):
    m = dict(sh)
    xt = np.concatenate([np.asarray(inp["ctx"][b], np.float32), np.asarray(inp["x"][b], np.float32)], axis=0)
    m["xT"] = np.ascontiguousarray(xt.T.reshape(NDC, 128, T))
    cs = np.stack([np.asarray(inp["c"][b], np.float32), np.asarray(inp["c_ctx"], np.float32)], axis=-1)
    m["csT"] = np.ascontiguousarray(cs.reshape(NDC, 128, 2).transpose(1, 0, 2))
    return m


def build_program(level=5, dbg=()):
    nc = bass.Bass("TRN2", target_bir_lowering=False)
    P = Prog(nc, level, dbg)
    kb = P.kb
    with nc.Block():
        P.phase_consts_ada()
        b_x0 = kb.buf("xT")
        b_xA, b_xB = P.sbuf_of("xA"), P.sbuf_of("xB")
        P.phase_inproj(0, P.xT, b_x0)
        if level >= 2:
            P.phase_mix_ab()
        if level >= 3:
            P.phase_outproj(0)
            P.phase_resid(0, 2, P.xT, b_x0, P.xA, b_xA)
        if level >= 4:
            P.phase_ffn_up(0, P.xA, b_xA)
            P.phase_ffn_down(0)
            P.phase_resid(0, 5, P.xA, b_xA, P.xB, b_xB)
        if level >= 5:
            P.phase_inproj(1, P.xB, b_xB)
            P.phase_mix_gla()
            P.phase_outproj(1, skip_ctx=True)
            P.phase_resid(1, 2, P.xB, b_xB, P.xA, b_xA)
            P.phase_ffn_up(1, P.xA, b_xA, skip_ctx=True)
            P.phase_ffn_down(1, skip_ctx=True)
            P.phase_resid(1, 5, P.xA, b_xA, None, None, final=True)
        P.finish()
    return nc, P


_CACHE = {}


def kernel(**inputs):
    ncores = 4
    if "prog" not in _CACHE:
        _CACHE["prog"] = build_program(level=5, dbg=())
    nc, P = _CACHE["prog"]
    names = set(P.in_names)
    sh = prep_shared(inputs, names)
    in_maps = []
    for b in range(ncores):
        m = prep_core(inputs, b, sh)
        in_maps.append({k: m[k] for k in P.in_names})
    res = run_bass_kernel_spmd(nc, in_maps, core_ids=list(range(ncores)))
    outs = []
    for b in range(ncores):
        o = np.asarray(res.results[b]["out"], np.float32).reshape(D, LL)
        outs.append(np.ascontiguousarray(o.T))
    return np.stack(outs, axis=0)
```

```python
import numpy as np
import ml_dtypes
import concourse.bass as bass
import concourse.mybir as mybir
from concourse.bass_utils import run_bass_kernel_spmd

F32 = mybir.dt.float32
BF16 = mybir.dt.bfloat16
AF = mybir.ActivationFunctionType
ALU = mybir.AluOpType
AX = mybir.AxisListType

D = 4096
NDC = 32
LC = 256
LL = 2048
T = LC + LL
NB = T // 128
DFF = 11008
NFC = DFF // 128
EPS = 1e-6
TT = [(0, 256), (256, 768), (768, 1280), (1280, 1792), (1792, 2304)]
AB_IN = 9232
GLA_IN = 12320
AB_G = 37
GLA_G = 49


class Sem:
    def __init__(self, nc, name):
        self.h = nc.alloc_semaphore(name)
        self.v = 0
        self.name = name


class Buf:
    __slots__ = ("name", "w", "r", "multi")

    def __init__(self, name, multi=False):
        self.name = name
        self.w = {}
        self.r = {}
        self.multi = multi


class Eng:
    def __init__(self, kb, name, e):
        self.kb = kb
        self.name = name
        self.e = e
        self.sem = Sem(kb.nc, "p_" + name)
        self.seen = {}
        self.nsem = 0

    def tick(self):
        if self.sem.v >= 60000:
            self.nsem += 1
            self.sem = Sem(self.kb.nc, "p_%s%d" % (self.name, self.nsem))
        self.sem.v += 1
        return (self.sem, self.sem.v)


class KB:
    def __init__(self, nc):
        self.nc = nc
        self.pe = Eng(self, "pe", nc.tensor)
        self.act = Eng(self, "act", nc.scalar)
        self.dve = Eng(self, "dve", nc.vector)
        self.pool = Eng(self, "pool", nc.gpsimd)
        self.sp = Eng(self, "sp", nc.sync)
        self.engs = [self.pe, self.act, self.dve, self.pool, self.sp]
        self.dsems = []
        self.free_sems = []
        self.cur_phase = None
        self.phase_stack = []
        self.nbuf = 0

    def buf(self, name=None, multi=False):
        self.nbuf += 1
        return Buf(name or ("b%d" % self.nbuf), multi)

    def dsem(self, name):
        if self.free_sems:
            s = self.free_sems.pop()
        else:
            s = Sem(self.nc, "d_%d" % len(self.dsems))
            self.dsems.append(s)
        if self.cur_phase is not None:
            self.cur_phase.sems.append(s)
        return s

    def _waits(self, eng, reads, writes):
        need = {}
        for b in reads:
            for s, v in b.w.items():
                if need.get(s, 0) < v:
                    need[s] = v
        for b in writes:
            if not b.multi:
                for s, v in b.w.items():
                    if need.get(s, 0) < v:
                        need[s] = v
            for s, v in b.r.items():
                if need.get(s, 0) < v:
                    need[s] = v
        for s, v in need.items():
            if eng.seen.get(s, 0) < v:
                eng.e.wait_ge(s.h, v)
                eng.seen[s] = v

    def _mark(self, tok, reads, writes):
        s, v = tok
        for b in writes:
            if b.multi:
                if b.w.get(s, 0) < v:
                    b.w[s] = v
            else:
                b.w = {s: v}
                b.r = {}
        for b in reads:
            if b.r.get(s, 0) < v:
                b.r[s] = v

    def op(self, eng, fn, reads=(), writes=()):
        self._waits(eng, reads, writes)
        ins = fn(eng.e)
        tok = eng.tick()
        ins.then_inc(tok[0].h, 1)
        self._mark(tok, reads, writes)
        return ins

    def mm(self, out_ap, pairs, reads, writes, fp32=False):
        eng = self.pe
        self._waits(eng, reads, writes)
        n = len(pairs)
        ins = None
        for i, (l, r) in enumerate(pairs):
            ins = eng.e.matmul(out_ap, lhsT=l, rhs=r, start=(i == 0), stop=(i == n - 1))
        tok = eng.tick()
        ins.then_inc(tok[0].h, 1)
        self._mark(tok, reads, writes)

    def mm_multi(self, outs, nk, lhs_fn, rhs_fn, reads, writes):
        eng = self.pe
        self._waits(eng, reads, writes)
        ins = None
        for kc in range(nk):
            l = lhs_fn(kc)
            for ti, o in enumerate(outs):
                ins = eng.e.matmul(o, lhsT=l, rhs=rhs_fn(kc, ti), start=(kc == 0), stop=(kc == nk - 1))
        tok = eng.tick()
        ins.then_inc(tok[0].h, 1)
        self._mark(tok, reads, writes)

    def dma(self, q, out_ap, in_ap, sem, reads=(), writes=()):
        self._waits(q, reads, writes)
        ins = q.e.dma_start(out=out_ap, in_=in_ap)
        sem.v += 16
        ins.then_inc(sem.h, 16)
        self._mark((sem, sem.v), reads, writes)

    def barrier(self):
        toks = {}
        for e in self.engs:
            if e.sem.v > 0:
                toks[e.sem] = e.sem.v
        for s in self.dsems:
            if s.v > 0:
                toks[s] = s.v
        for e in self.engs:
            for s, v in toks.items():
                if s is e.sem:
                    continue
                if e.seen.get(s, 0) < v:
                    e.e.wait_ge(s.h, v)
                    e.seen[s] = v


class Ring:
    def __init__(self, kb, name, tiles, with_sem=False, multi=False):
        self.tiles = tiles
        self.bufs = [kb.buf("%s%d" % (name, i), multi) for i in range(len(tiles))]
        self.sems = [kb.dsem("%s%d" % (name, i)) for i in range(len(tiles))] if with_sem else None
        self.i = -1

    def next(self):
        self.i = (self.i + 1) % len(self.tiles)
        if self.sems:
            return self.tiles[self.i], self.bufs[self.i], self.sems[self.i]
        return self.tiles[self.i], self.bufs[self.i]


class Phase:
    def __init__(self, kb, name):
        self.kb = kb
        self.name = name
        self.guards = []
        self.sems = []
        kb.phase_stack.append(self)
        kb.cur_phase = self

    def sb(self, name, shape, dt):
        g = self.kb.nc.sbuf_tensor(self.name + "_" + name, shape, dt)
        t = g.__enter__()
        self.guards.append(g)
        return t

    def ps(self, name, shape, dt=F32):
        g = self.kb.nc.psum_tensor(self.name + "_" + name, shape, dt)
        t = g.__enter__()
        self.guards.append(g)
        return t

    def close(self):
        self.kb.barrier()
        for g in reversed(self.guards):
            g.__exit__(None, None, None)
        self.guards = []
        self.kb.free_sems.extend(self.sems)
        self.sems = []
        assert self.kb.phase_stack[-1] is self
        self.kb.phase_stack.pop()
        self.kb.cur_phase = self.kb.phase_stack[-1] if self.kb.phase_stack else None


def cls_cols(t0, t1):
    out = []
    if t0 < LC:
        out.append((1, t0, min(t1, LC)))
    if t1 > LC:
        out.append((0, max(t0, LC), t1))
    return out


class Prog:
    LEVELS = {"ab_gate_b": 2, "ab_sink": 2, "ab_head_norm": 2, "ab_w_out": 3, "ffn_w_up": 4, "ffn_cw": 4,
              "ffn_cb": 4, "ffn_w_down": 4, "gla_w_in": 5, "gla_gw2": 5, "gla_head_norm": 5, "gla_w_out": 5}

    def __init__(self, nc, level=5, dbg=()):
        self.nc = nc
        self.kb = KB(nc)
        self.level = level
        self.dbg = dbg
        self.in_names = []
        kb = self.kb
        dt = nc.dram_tensor

        def din(name, shape, dtp=F32):
            if self.LEVELS.get(name, 1) > level:
                return None
            self.in_names.append(name)
            return dt(name, list(shape), dtp, kind="ExternalInput").ap()

        def dscr(name, shape, dtp):
            return dt(name, list(shape), dtp).ap()

        self.xT = din("xT", [NDC, 128, T])
        self.csT = din("csT", [128, NDC, 2])
        self.ada_w = din("ada_w", [2, D, 6 * D])
        self.ada_b = din("ada_b", [2, 128, 192])
        self.norm_w = din("norm_w", [2, 128, 4, NDC])
        self.ab_w_in = din("ab_w_in", [AB_G, 128, NDC, 256])
        self.ab_gate_b = din("ab_gate_b", [1, 16])
        self.ab_sink = din("ab_sink", [1, 16])
        self.ab_head_norm = din("ab_head_norm", [1, 2048])
        self.ab_w_out = din("ab_w_out", [NDC, 128, NDC, 128])
        self.gla_w_in = din("gla_w_in", [GLA_G, 128, NDC, 256])
        self.gla_gw2 = din("gla_gw2", [2, 17, 2048])
        self.gla_head_norm = din("gla_head_norm", [1, 4096])
        self.gla_w_out = din("gla_w_out", [NDC, 128, NDC, 128])
        self.ffn_w_up = din("ffn_w_up", [2, 2 * NFC, 128, NDC, 128])
        self.ffn_cw = din("ffn_cw", [2, 128, 3, 2 * NFC])
        self.ffn_cb = din("ffn_cb", [2, 128, 2 * NFC])
        self.ffn_w_down = din("ffn_w_down", [2, NDC, 128, NFC, 128])
        self.ropeC = din("ropeC", [128, LL])
        self.ropeS = din("ropeS", [128, LL])
        self.cmat = din("cmat", [6, 128, 128])
        self.out = dt("out", [NDC, 128, LL], F32, kind="ExternalOutput").ap()
        self.xA = dscr("xA", [NDC, 128, T], F32)
        self.xB = dscr("xB", [NDC, 128, T], F32)
        self.yscr = dscr("yscr", [NDC, 128, T], F32)
        self.projFM = dscr("projFM", [40, 128, T], BF16)
        self.projTM = dscr("projTM", [32, T, 256], BF16)
        self.gatesTM = dscr("gatesTM", [T, 16], F32)
        self.rlowFM = dscr("rlowFM", [32, T], F32)
        self.mixTM = dscr("mixTM", [T, D], BF16)
        self.actscr = dscr("actscr", [NFC, 128, T], BF16)
        self.dbgout = {}
        for name, shape, dtp in dbg:
            self.dbgout[name] = dt("dbg_" + name, list(shape), dtp, kind="ExternalOutput").ap()

        a = nc.alloc_sbuf_tensor
        self.cm = a("cm", [128, 6, 128], F32)
        self.cmb = a("cmb", [128, 6, 128], BF16)
        self.mods = a("mods", [128, 2, 192, 2], F32)
        self.nw = a("nw", [128, 2, 4, NDC], F32)
        self.der = a("der", [128, 2, 6, NDC, 2], F32)
        self.sc = a("sc", [128, NDC, 2], F32)
        self.scb = a("scb", [128, NDC, 2], BF16)
        self.adab = a("adab", [128, 2, 192], F32)
        self.b_cs = kb.buf("cs")
        self.b_adab = kb.buf("adab")
        self.b_nw = kb.buf("nw")
        self.b_cm = kb.buf("cm")
        self.b_mods = kb.buf("mods", multi=True)
        self.b_der = kb.buf("der")
        self.s_misc = kb.dsem("misc")
        self.s_out = kb.dsem("out")
        self.b_scr = {}

    def sbuf_of(self, name):
        if name not in self.b_scr:
            self.b_scr[name] = self.kb.buf("scr_" + name, multi=True)
        return self.b_scr[name]

    def ada_group(self, l, g, wr, psr, ptr, rowr):
        kb = self.kb
        w, bw, sw = wr.next()
        kb.dma(kb.pool, w[:], self.ada_w[l].rearrange("(kc p) n -> p kc n", p=128)[:, :, g * 512:(g + 1) * 512], sw, writes=[bw])
        p, bp = psr.next()
        kb.mm(p[0:2, :], [(self.scb[:, kc, :], w[:, kc, :]) for kc in range(NDC)], reads=[bw, self.b_cs], writes=[bp])
        row, brow_ = rowr.next()
        kb.op(kb.act, lambda e: e.activation(out=row[:], in_=p[0:2, :], func=AF.Copy), reads=[bp], writes=[brow_])
        pt, bpt = ptr.next()
        for m in range(4):
            kb.mm(pt[:, 2 * m:2 * m + 2], [(row[0:2, m * 128:(m + 1) * 128], self.cm[0:2, 0, 0:2])], reads=[brow_, self.b_cm], writes=[bpt])
        for cl in range(2):
            kb.op(kb.dve, lambda e, cl=cl: e.tensor_tensor(
                out=self.mods[:, l, g * 4:(g + 1) * 4, cl], in0=pt[:, 0:8].rearrange("p (j c) -> p j c", c=2)[:, :, cl],
                in1=self.adab[:, l, g * 4:(g + 1) * 4], op=ALU.add), reads=[bpt, self.b_adab], writes=[self.b_mods])

    def ada_derived(self, l):
        kb = self.kb
        for cl in range(2):
            md = lambda w: self.mods[:, l, w * 32:(w + 1) * 32, cl]
            dr = lambda k: self.der[:, l, k, :, cl]
            ops = [
                lambda e: e.scalar_tensor_tensor(out=dr(0), in0=md(1), scalar=1.0, in1=self.nw[:, l, 0, :], op0=ALU.add, op1=ALU.mult),
                lambda e: e.tensor_copy(out=dr(1), in_=md(0)),
                lambda e: e.tensor_tensor(out=dr(2), in0=md(2), in1=self.nw[:, l, 1, :], op=ALU.mult),
                lambda e: e.scalar_tensor_tensor(out=dr(3), in0=md(4), scalar=1.0, in1=self.nw[:, l, 2, :], op0=ALU.add, op1=ALU.mult),
                lambda e: e.tensor_copy(out=dr(4), in_=md(3)),
                lambda e: e.tensor_tensor(out=dr(5), in0=md(5), in1=self.nw[:, l, 3, :], op=ALU.mult),
            ]
            for f in ops:
                kb.op(kb.dve, f, reads=[self.b_mods, self.b_nw], writes=[self.b_der])

    def phase_consts_ada(self):
        kb, nc = self.kb, self.nc
        ph = Phase(kb, "ada")
        kb.dma(kb.sp, self.cm[:], self.cmat.rearrange("k p n -> p k n"), kb.dsem('m'), writes=[self.b_cm])
        kb.op(kb.dve, lambda e: e.tensor_copy(out=self.cmb[:], in_=self.cm[:]), reads=[self.b_cm], writes=[self.b_cm])
        kb.dma(kb.sp, self.nw[:], self.norm_w.rearrange("l p f d -> p l f d"), kb.dsem('m'), writes=[self.b_nw])
        kb.dma(kb.sp, self.sc[:], self.csT, kb.dsem('m'), writes=[self.b_cs])
        kb.dma(kb.sp, self.adab[:], self.ada_b.rearrange("l p j -> p l j"), kb.dsem('m'), writes=[self.b_adab])
        kb.op(kb.act, lambda e: e.activation(out=self.sc[:], in_=self.sc[:], func=AF.Silu), reads=[self.b_cs], writes=[self.b_cs])
        kb.op(kb.dve, lambda e: e.tensor_copy(out=self.scb[:], in_=self.sc[:]), reads=[self.b_cs], writes=[self.b_cs])
        wr = Ring(kb, "adaw", [ph.sb("w%d" % i, [128, NDC, 512], BF16) for i in range(3)], with_sem=True)
        psr = Ring(kb, "adap", [ph.ps("p%d" % i, [128, 512], F32) for i in range(2)])
        ptr = Ring(kb, "adat", [ph.ps("t%d" % i, [128, 512], F32) for i in range(2)])
        rowr = Ring(kb, "adar", [ph.sb("row%d" % i, [2, 512], F32) for i in range(2)])
        for g in range(48):
            self.ada_group(0, g, wr, psr, ptr, rowr)
        self.ada_derived(0)
        ph.close()

    def norm_mod(self, ph, xsrc, b_x, l, kg, ksh, hT, b_hT, lo, hi):
        kb = self.kb
        n = hi - lo
        xr = Ring(kb, "nx", [ph.sb("nx%d" % i, [128, n], F32) for i in range(2)], with_sem=True)
        acc = ph.sb("nacc", [128, n], F32)
        tmp = ph.sb("ntmp", [128, n], F32)
        rstd = ph.sb("nrstd", [128, n], F32)
        b_acc, b_tmp, b_rstd = kb.buf("nacc"), kb.buf("ntmp"), kb.buf("nrstd")
        pss = [ph.ps("nps%d" % i, [128, 512], F32) for i in range(2)]
        b_pss = [kb.buf("nps%d" % i) for i in range(2)]
        for dc in range(NDC):
            x, bx, sx = xr.next()
            kb.dma(kb.sp, x[:], xsrc[dc][:, lo:hi], sx, reads=[b_x], writes=[bx])
            if dc == 0:
                kb.op(kb.dve, lambda e: e.tensor_tensor(out=acc[:], in0=x[:], in1=x[:], op=ALU.mult), reads=[bx], writes=[b_acc])
            else:
                kb.op(kb.act, lambda e: e.activation(out=tmp[:], in_=x[:], func=AF.Square), reads=[bx], writes=[b_tmp])
                kb.op(kb.dve, lambda e: e.tensor_tensor(out=acc[:], in0=acc[:], in1=tmp[:], op=ALU.add), reads=[b_tmp], writes=[b_acc])
        i = 0
        for c0 in range(0, n, 512):
            c1 = min(n, c0 + 512)
            p = pss[i % 2]
            kb.mm(p[:, 0:c1 - c0], [(self.cm[:, 1, :], acc[:, c0:c1])], reads=[self.b_cm, b_acc], writes=[b_pss[i % 2]])
            kb.op(kb.act, lambda e: e.activation(out=tmp[:, c0:c1], in_=p[:, 0:c1 - c0], func=AF.Sqrt, scale=1.0 / D, bias=EPS),
                  reads=[b_pss[i % 2]], writes=[b_tmp])
            i += 1
        kb.op(kb.dve, lambda e: e.reciprocal(out=rstd[:], in_=tmp[:]), reads=[b_tmp], writes=[b_rstd])
        for dc in range(NDC):
            x, bx, sx = xr.next()
            kb.dma(kb.sp, x[:], xsrc[dc][:, lo:hi], sx, reads=[b_x], writes=[bx])
            kb.op(kb.dve, lambda e: e.tensor_tensor(out=tmp[:], in0=x[:], in1=rstd[:], op=ALU.mult), reads=[bx, b_rstd], writes=[b_tmp])
            for cl, a0, a1 in cls_cols(lo, hi):
                kb.op(kb.act, lambda e: e.activation(out=hT[:, dc, a0 - lo:a1 - lo], in_=tmp[:, a0 - lo:a1 - lo], func=AF.Identity,
                                                     scale=self.der[:, l, kg, dc, cl:cl + 1], bias=self.der[:, l, ksh, dc, cl:cl + 1]),
                      reads=[b_tmp, self.b_der], writes=[b_hT[dc]])

    def phase_inproj(self, l, xsrc, b_x):
        kb = self.kb
        w_dram = self.ab_w_in if l == 0 else self.gla_w_in
        ngroups = AB_G if l == 0 else GLA_G
        if l == 0:
            def plan(g):
                if g < 8: return ("FM", 2 * g, 128 ** -0.5, True)
                if g < 10: return ("FM", 16 + 2 * (g - 8), 1.0, True)
                if g < 12: return ("TM", g - 10, 1.0, False)
                if g < 16: return ("FM", 20 + 2 * (g - 12), 1.0, False)
                if g < 20: return ("FM", 28 + 2 * (g - 16), 1.0 / 16, False)
                if g < 36: return ("TM", 2 + (g - 20), 1.0, False)
                return ("GT", 0, 1.0, False)
        else:
            def plan(g):
                if g < 8: return ("FM", 2 * g, 1.0 / 16, False)
                if g < 16: return ("FM", 16 + 2 * (g - 8), 1.0, False)
                if g < 48: return ("TM", g - 16, 1.0, False)
                return ("GF", 0, 1.0, False)
        b_pFM = self.sbuf_of("projFM")
        b_pTM = self.sbuf_of("projTM")
        b_gT = self.sbuf_of("gatesTM")
        b_rl = self.sbuf_of("rlowFM")
        HALVES = [(0, 1152, [(0, 256), (256, 768), (768, 1152)]), (1152, 2304, [(1152, 1664), (1664, 2176), (2176, 2304)])]
        for hi_, (lo, hi, tiles) in enumerate(HALVES):
            nh = hi - lo
            nbh = nh // 128
            ph = Phase(kb, "inp%d_%d" % (l, hi_))
            hT = ph.sb("hT", [128, NDC, nh], BF16)
            b_hT = [kb.buf("hT%d" % i) for i in range(NDC)]
            ph2 = Phase(kb, "inpn%d_%d" % (l, hi_))
            self.norm_mod(ph2, xsrc, b_x, l, 0, 1, hT, b_hT, lo, hi)
            ph2.close()
            tg = "%d_%d" % (l, hi_)
            wr = Ring(kb, "inw" + tg, [ph.sb("w%d" % i, [128, NDC, 256], BF16) for i in range(2)], with_sem=True)
            psr = Ring(kb, "inps" + tg, [ph.ps("p%d" % i, [128, 512], F32) for i in range(5)])
            psq = Ring(kb, "inpq" + tg, [ph.ps("q%d" % i, [128, 512], F32) for i in range(1)])
            psT = Ring(kb, "inpT" + tg, [ph.ps("T%d" % i, [128, 512], BF16) for i in range(2)])
            fmr = Ring(kb, "infm" + tg, [ph.sb("fm%d" % i, [128, nh], BF16) for i in range(4)], multi=True)
            stF = Ring(kb, "stF" + tg, [ph.sb("stF%d" % i, [128, nh], BF16) for i in range(2)], with_sem=True, multi=True)
            stT = Ring(kb, "stT" + tg, [ph.sb("stT%d" % i, [128, nbh, 256], BF16) for i in range(2)], with_sem=True, multi=True)
            stG = ph.sb("stG", [128, nbh, 64], F32)
            b_stG = kb.buf("stG", multi=True)
            s_stG = kb.dsem("stG" + tg)
            rC = Ring(kb, "rC" + tg, [ph.sb("rC%d" % i, [128, 512], F32) for i in range(2)], with_sem=True)
            rS = Ring(kb, "rS" + tg, [ph.sb("rS%d" % i, [128, 512], F32) for i in range(2)], with_sem=True)
            qb_r = Ring(kb, "qb" + tg, [ph.sb("qb%d" % i, [128, 512], BF16) for i in range(2)])
            t1_r = Ring(kb, "t1" + tg, [ph.sb("t1%d" % i, [128, 512], F32) for i in range(2)])
            t2_r = Ring(kb, "t2" + tg, [ph.sb("t2%d" % i, [128, 512], F32) for i in range(2)])
            stGf = stG[0:32, :, :].rearrange("p a b -> p (a b)")
            for g in range(ngroups):
                form, dst, scale, rope = plan(g)
                w, bw, sw = wr.next()
                kb.dma(kb.pool, w[:], w_dram[g], sw, writes=[bw])
                if form in ("FM", "GF"):
                    for m in range(2 if form == "FM" else 1):
                        if form == "FM":
                            st, bst, sst = stF.next()
                        pbs = [psr.next() for _ in tiles]
                        kb.mm_multi([pb[0][:, 0:t1 - t0] for pb, (t0, t1) in zip(pbs, tiles)], NDC,
                                    lambda kc: w[:, kc, m * 128:(m + 1) * 128],
                                    lambda kc, ti: hT[:, kc, tiles[ti][0] - lo:tiles[ti][1] - lo],
                                    reads=[bw] + b_hT, writes=[pb[1] for pb in pbs])
                        for ti, (t0, t1) in enumerate(tiles):
                            n = t1 - t0
                            p, bp = pbs[ti]
                            if form == "GF":
                                kb.op(kb.act, lambda e: e.activation(out=stGf[:, 0:n], in_=p[0:32, 0:n], func=AF.Copy),
                                      reads=[bp], writes=[b_stG])
                                kb.dma(kb.sp, self.rlowFM[:, t0:t1], stGf[:, 0:n], s_stG, reads=[b_stG], writes=[b_rl])
                                continue
                            if rope and t0 >= LC:
                                qb, bqb = qb_r.next()
                                kb.op(kb.act, lambda e: e.activation(out=qb[:, 0:n], in_=p[:, 0:n], func=AF.Copy, scale=scale),
                                      reads=[bp], writes=[bqb])
                                c_, bc_, sc_ = rC.next()
                                s_, bs_, ss_ = rS.next()
                                kb.dma(kb.sp, c_[:, 0:n], self.ropeC[:, t0 - LC:t1 - LC], sc_, writes=[bc_])
                                kb.dma(kb.sp, s_[:, 0:n], self.ropeS[:, t0 - LC:t1 - LC], ss_, writes=[bs_])
                                q2, bq2 = psq.next()
                                kb.mm(q2[:, 0:n], [(self.cmb[:, 4, :], qb[:, 0:n])], reads=[bqb, self.b_cm], writes=[bq2])
                                a1, ba1 = t1_r.next()
                                a2, ba2 = t2_r.next()
                                kb.op(kb.dve, lambda e: e.tensor_tensor(out=a1[:, 0:n], in0=qb[:, 0:n], in1=c_[:, 0:n], op=ALU.mult),
                                      reads=[bqb, bc_], writes=[ba1])
                                kb.op(kb.dve, lambda e: e.tensor_tensor(out=a2[:, 0:n], in0=q2[:, 0:n], in1=s_[:, 0:n], op=ALU.mult),
                                      reads=[bq2, bs_], writes=[ba2])
                                kb.op(kb.dve, lambda e: e.tensor_tensor(out=st[:, t0 - lo:t1 - lo], in0=a1[:, 0:n], in1=a2[:, 0:n], op=ALU.add),
                                      reads=[ba1, ba2], writes=[bst])
                            else:
                                kb.op(kb.act, lambda e: e.activation(out=st[:, t0 - lo:t1 - lo], in_=p[:, 0:n], func=AF.Copy, scale=scale),
                                      reads=[bp], writes=[bst])
                        if form == "FM":
                            kb.dma(kb.sp, self.projFM[dst + m][:, lo:hi], st[:], sst, reads=[bst], writes=[b_pFM])
                elif form == "TM":
                    st, bst, sst = stT.next()
                    fms = []
                    for m in range(2):
                        pbs = [psr.next() for _ in tiles]
                        kb.mm_multi([pb[0][:, 0:t1 - t0] for pb, (t0, t1) in zip(pbs, tiles)], NDC,
                                    lambda kc: w[:, kc, m * 128:(m + 1) * 128],
                                    lambda kc, ti: hT[:, kc, tiles[ti][0] - lo:tiles[ti][1] - lo],
                                    reads=[bw] + b_hT, writes=[pb[1] for pb in pbs])
                        fm, bfm = fmr.next()
                        for ti, (t0, t1) in enumerate(tiles):
                            p, bp = pbs[ti]
                            if ti % 2 == 0:
                                kb.op(kb.act, lambda e: e.activation(out=fm[:, t0 - lo:t1 - lo], in_=p[:, 0:t1 - t0], func=AF.Copy), reads=[bp], writes=[bfm])
                            else:
                                kb.op(kb.dve, lambda e: e.tensor_copy(out=fm[:, t0 - lo:t1 - lo], in_=p[:, 0:t1 - t0]), reads=[bp], writes=[bfm])
                        fms.append((fm, bfm))
                    for tb in range(nbh):
                        T_, bT = psT.next()
                        for m in range(2):
                            fm, bfm = fms[m]
                            kb.op(kb.pe, lambda e: e.transpose(out=T_[:, m * 128:(m + 1) * 128], in_=fm[:, tb * 128:(tb + 1) * 128], identity=self.cmb[:, 0, :]),
                                  reads=[bfm, self.b_cm], writes=[bT])
                        if tb % 2 == 0:
                            kb.op(kb.act, lambda e: e.activation(out=st[:, tb, :], in_=T_[:, 0:256], func=AF.Copy), reads=[bT], writes=[bst])
                        else:
                            kb.op(kb.dve, lambda e: e.tensor_copy(out=st[:, tb, :], in_=T_[:, 0:256]), reads=[bT], writes=[bst])
                    kb.dma(kb.sp, self.projTM[dst][lo:hi].rearrange("(tb p) c -> p tb c", p=128), st[:], sst, reads=[bst], writes=[b_pTM])
                else:
                    for tb in range(nbh):
                        p, bp = psr.next()
                        kb.mm(p[:, 0:256], [(hT[:, kc, tb * 128:(tb + 1) * 128], w[:, kc, :]) for kc in range(NDC)],
                              reads=[bw] + b_hT, writes=[bp])
                        if form == "TM":
                            if tb % 2 == 0:
                                kb.op(kb.act, lambda e: e.activation(out=st[:, tb, :], in_=p[:, 0:256], func=AF.Copy), reads=[bp], writes=[bst])
                            else:
                                kb.op(kb.dve, lambda e: e.tensor_copy(out=st[:, tb, :], in_=p[:, 0:256]), reads=[bp], writes=[bst])
                        else:
                            kb.op(kb.act, lambda e: e.activation(out=stG[:, tb, 0:16], in_=p[:, 0:16], func=AF.Copy), reads=[bp], writes=[b_stG])
                    if form == "TM":
                        kb.dma(kb.sp, self.projTM[dst][lo:hi].rearrange("(tb p) c -> p tb c", p=128), st[:], sst, reads=[bst], writes=[b_pTM])
                    else:
                        kb.dma(kb.sp, self.gatesTM[lo:hi].rearrange("(tb p) c -> p tb c", p=128), stG[:, :, 0:16], s_stG, reads=[b_stG], writes=[b_gT])
            ph.close()

    def phase_mix_ab(self):
        kb = self.kb
        b_pFM, b_pTM, b_gT, b_mix = (self.sbuf_of(n) for n in ("projFM", "projTM", "gatesTM", "mixTM"))
        ph = Phase(kb, "att")
        kT = ph.sb("kT", [128, T], BF16); b_kT = kb.buf("kT"); s_kT = kb.dsem("kT")
        va = ph.sb("va", [128, NB, 132], BF16); b_va = kb.buf("va"); s_va = kb.dsem("va")
        qr = Ring(kb, "aq", [ph.sb("q%d" % i, [128, T], BF16) for i in range(2)], with_sem=True)
        ost = Ring(kb, "aost", [ph.sb("ost%d" % i, [128, NB, 512], BF16) for i in range(2)], with_sem=True, multi=True)
        esk = ph.sb("esk", [128, 16], F32); b_esk = kb.buf("esk")
        kb.dma(kb.sp, esk[:], self.ab_sink.partition_broadcast(128), kb.dsem('m'), writes=[b_esk])
        kb.op(kb.act, lambda e: e.activation(out=esk[:], in_=esk[:], func=AF.Exp), reads=[b_esk], writes=[b_esk])
        psS = Ring(kb, "aS", [ph.ps("S%d" % i, [128, 512], F32) for i in range(4)])
        psN = Ring(kb, "aN", [ph.ps("N%d" % i, [128, 512], F32) for i in range(2)])
        PT = Ring(kb, "aPT", [ph.sb("PT%d" % i, [128, 5, 128], BF16) for i in range(3)])
        rr = Ring(kb, "arr", [ph.sb("rr%d" % i, [128, 2], F32) for i in range(3)])
        a_wr = Ring(kb, "adaw1", [ph.sb("aw%d" % i, [128, NDC, 512], BF16) for i in range(2)], with_sem=True)
        a_psr = Ring(kb, "adap1", [ph.ps("ap%d" % i, [128, 512], F32) for i in range(1)])
        a_ptr = Ring(kb, "adat1", [ph.ps("at%d" % i, [128, 512], F32) for i in range(1)])
        a_rowr = Ring(kb, "adar1", [ph.sb("arow%d" % i, [2, 512], F32) for i in range(2)])
        ada_g = 0
        for kvh in range(4):
            kb.dma(kb.sp, kT[:], self.projFM[16 + kvh], s_kT, reads=[b_pFM], writes=[b_kT])
            kb.op(kb.pool, lambda e: e.memset(va[:, :, 128:129], 1.0), writes=[b_va])
            kb.dma(kb.sp, va[:, :, 0:128], self.projTM[kvh // 2].rearrange("(tb p) c -> p tb c", p=128)[:, :, (kvh % 2) * 128:(kvh % 2 + 1) * 128],
                   s_va, reads=[b_pTM], writes=[b_va])
            o_t, b_o, s_o = ost.next()
            for hq in range(4):
                head = kvh * 4 + hq
                q, bq, sq = qr.next()
                kb.dma(kb.sp, q[:], self.projFM[head], sq, reads=[b_pFM], writes=[bq])
                for i in range(NB):
                    if i < 2:
                        js = [(0, None), (1, None)]
                    else:
                        js = [(0, None), (1, None)]
                        if i - 1 >= 2: js.append((i - 1, 3))
                        js.append((i, None))
                        if i + 1 < NB: js.append((i + 1, 2))
                    pt, bpt = PT.next()
                    for c0 in range(0, len(js), 4):
                        part = js[c0:c0 + 4]
                        S, bS = psS.next()
                        for jj, (j, mk) in enumerate(part):
                            kb.mm(S[:, jj * 128:(jj + 1) * 128], [(kT[:, j * 128:(j + 1) * 128], q[:, i * 128:(i + 1) * 128])],
                                  reads=[b_kT, bq], writes=[bS])
                        npc = len(part)
                        kb.op(kb.act, lambda e: e.activation(out=pt[:, c0:c0 + npc, :].rearrange("p a b -> p (a b)"), in_=S[:, 0:npc * 128], func=AF.Exp),
                              reads=[bS], writes=[bpt])
                    for jj, (j, mk) in enumerate(js):
                        if mk is not None:
                            kb.op(kb.dve, lambda e: e.tensor_tensor(out=pt[:, jj, :], in0=pt[:, jj, :], in1=self.cmb[:, mk, :], op=ALU.mult),
                                  reads=[bpt, self.b_cm], writes=[bpt])
                    N_, bN = psN.next()
                    kb.mm(N_[:, 0:129], [(pt[:, jj, :], va[:, j, 0:129]) for jj, (j, mk) in enumerate(js)], reads=[bpt, b_va], writes=[bN])
                    r_, br = rr.next()
                    kb.op(kb.dve, lambda e: e.tensor_scalar(out=r_[:, 0:1], in0=N_[:, 128:129], scalar1=esk[:, head:head + 1], scalar2=None, op0=ALU.add),
                          reads=[bN, b_esk], writes=[br])
                    kb.op(kb.dve, lambda e: e.reciprocal(out=r_[:, 1:2], in_=r_[:, 0:1]), reads=[br], writes=[br])
                    kb.op(kb.act, lambda e: e.activation(out=o_t[:, i, hq * 128:(hq + 1) * 128], in_=N_[:, 0:128], func=AF.Copy, scale=r_[:, 1:2]),
                          reads=[bN, br], writes=[b_o])
                    if i % 6 == 5 and ada_g < 48:
                        self.ada_group(1, ada_g, a_wr, a_psr, a_ptr, a_rowr)
                        ada_g += 1
            kb.dma(kb.sp, self.mixTM.rearrange("(tb p) c -> p tb c", p=128)[:, :, kvh * 512:(kvh + 1) * 512], o_t[:], s_o, reads=[b_o], writes=[b_mix])
        while ada_g < 48:
            self.ada_group(1, ada_g, a_wr, a_psr, a_ptr, a_rowr)
            ada_g += 1
        self.ada_derived(1)
        ph.close()
        ph = Phase(kb, "mls")
        G = ph.sb("G", [128, NB, 16], F32); b_G = kb.buf("G")
        gb = ph.sb("gb", [128, 16], F32); b_gb = kb.buf("gb")
        sp_ = ph.sb("sp", [128, NB, 16], F32); b_sp = kb.buf("sp")
        csum = ph.sb("csum", [128, NB, 8], F32); b_cs = kb.buf("csum")
        cb = ph.sb("cb", [128, NB, 8], F32); b_cb = kb.buf("cb")
        gain = ph.sb("gain", [128, 2048], F32); b_gain = kb.buf("gain")
        kb.dma(kb.sp, G[:], self.gatesTM.rearrange("(tb p) c -> p tb c", p=128), kb.dsem('m'), reads=[b_gT], writes=[b_G])
        kb.dma(kb.sp, gb[:], self.ab_gate_b.partition_broadcast(128), kb.dsem('m'), writes=[b_gb])
        kb.dma(kb.sp, gain[:], self.ab_head_norm.partition_broadcast(128), kb.dsem('m'), writes=[b_gain])
        for tb in range(NB):
            kb.op(kb.dve, lambda e: e.tensor_tensor(out=G[:, tb, :], in0=G[:, tb, :], in1=gb[:], op=ALU.add), reads=[b_gb, b_G], writes=[b_G])
        kb.op(kb.act, lambda e: e.activation(out=sp_[:], in_=G[:], func=AF.Exp, scale=-1.0), reads=[b_G], writes=[b_sp])
        kb.op(kb.act, lambda e: e.activation(out=sp_[:], in_=sp_[:], func=AF.Ln, bias=1.0), reads=[b_sp], writes=[b_sp])
        ORD = [list(range(NB)), [1, 0] + list(range(NB - 1, 1, -1))]
        MK = [2, 3]
        psC = ph.ps("C", [128, 512], F32); b_psC = kb.buf("psC")
        for d in range(2):
            for pos, i in enumerate(ORD[d]):
                pairs = [(self.cm[:, 1, :], sp_[:, j, 4 + 8 * d:8 + 8 * d]) for j in ORD[d][:pos]]
                pairs.append((self.cm[:, MK[d], :], sp_[:, i, 4 + 8 * d:8 + 8 * d]))
                kb.mm(psC[:, 0:4], pairs, reads=[b_sp, self.b_cm], writes=[b_psC])
                kb.op(kb.dve, lambda e: e.tensor_copy(out=csum[:, i, 4 * d:4 * d + 4], in_=psC[:, 0:4]), reads=[b_psC], writes=[b_cs])
                kb.op(kb.dve, lambda e: e.tensor_tensor(out=cb[:, i, 4 * d:4 * d + 4], in0=psC[:, 0:4], in1=G[:, i, 8 * d:8 * d + 4], op=ALU.add),
                      reads=[b_psC, b_G], writes=[b_cb])
        qT = ph.sb("qT", [128, 2, T], BF16); b_qT = kb.buf("qT"); s_qT = kb.dsem("qT")
        kT = ph.sb("kT", [128, 2, T], BF16); b_kT = kb.buf("kT"); s_kT = kb.dsem("kT")
        v = ph.sb("v", [128, NB, 512], BF16); b_v = kb.buf("v"); s_v = kb.dsem("v")
        og = ph.sb("og", [128, NB, 512], BF16); b_og = kb.buf("og"); s_og = kb.dsem("og")
        hsum = ph.sb("hsum", [128, NB, 512], F32); b_hs = [kb.buf("hs%d" % i) for i in range(NB)]
        brow = ph.sb("brow", [128, NB, 128], F32); b_brow = [kb.buf("brow%d" % i) for i in range(NB)]
        dg = Ring(kb, "dg", [ph.sb("dg%d" % i, [128, 128], F32) for i in range(2)])
        ost = ph.sb("ost", [128, NB, 512], BF16); b_ost = kb.buf("most", multi=True); s_ost = kb.dsem("most")
        ss = ph.sb("ss", [128, NB], F32); b_ss = kb.buf("ss")
        rs = ph.sb("rs", [128, NB], F32); b_rs = kb.buf("rs")
        junk = ph.sb("junk", [128, 512], F32); b_junk = kb.buf("junk")
        tmpo = Ring(kb, "tmpo", [ph.sb("tmpo%d" % i, [128, 512], F32) for i in range(2)])
        DT = Ring(kb, "DT", [ph.sb("DT%d" % i, [128, 4, 128], F32) for i in range(3)])
        PT = Ring(kb, "mPT", [ph.sb("PT%d" % i, [128, 4, 128], BF16) for i in range(3)])
        psS = Ring(kb, "mS", [ph.ps("S%d" % i, [128, 512], F32) for i in range(3)])
        psN = Ring(kb, "mN", [ph.ps("N%d" % i, [128, 512], F32) for i in range(2)])
        psD = Ring(kb, "mD", [ph.ps("D%d" % i, [128, 512], F32) for i in range(2)])
        rr = Ring(kb, "mrr", [ph.sb("rr%d" % i, [128, 2], F32) for i in range(3)])
        onesb = self.cmb[:, 1, 0:1]
        for h in range(4):
            for c in range(2):
                kb.dma(kb.sp, qT[:, c, :], self.projFM[20 + 2 * h + c], s_qT, reads=[b_pFM], writes=[b_qT])
                kb.dma(kb.sp, kT[:, c, :], self.projFM[28 + 2 * h + c], s_kT, reads=[b_pFM], writes=[b_kT])
                kb.dma(kb.sp, v[:, :, c * 256:(c + 1) * 256], self.projTM[2 + 2 * h + c].rearrange("(tb p) c -> p tb c", p=128), s_v,
                       reads=[b_pTM], writes=[b_v])
                kb.dma(kb.sp, og[:, :, c * 256:(c + 1) * 256], self.projTM[10 + 2 * h + c].rearrange("(tb p) c -> p tb c", p=128), s_og,
                       reads=[b_pTM], writes=[b_og])
            kb.op(kb.act, lambda e: e.activation(out=og[:], in_=og[:], func=AF.Sigmoid), reads=[b_og], writes=[b_og])
            for d in range(2):
                col = 4 * d + h
                for i in range(NB):
                    dt_, bdt = dg.next()
                    kb.op(kb.dve, lambda e: e.tensor_scalar(out=dt_[:], in0=self.cm[:, 0, :], scalar1=csum[:, i, col:col + 1], scalar2=None, op0=ALU.mult),
                          reads=[b_cs, self.b_cm], writes=[bdt])
                    kb.mm(psC[:, 0:128], [(self.cm[:, 1, :], dt_[:])], reads=[bdt, self.b_cm], writes=[b_psC])
                    kb.op(kb.act, lambda e: e.activation(out=brow[:, i, :], in_=psC[:, 0:128], func=AF.Copy), reads=[b_psC], writes=[b_brow[i]])
                for pos, i in enumerate(ORD[d]):
                    js = ORD[d][:pos + 1]
                    N_, bN = psN.next()
                    Dn, bDn = psD.next()
                    ngr = (len(js) + 3) // 4
                    for gi in range(ngr):
                        part = js[gi * 4:gi * 4 + 4]
                        npc = len(part)
                        S, bS = psS.next()
                        D_, bD = DT.next()
                        P_, bP = PT.next()
                        for jj, j in enumerate(part):
                            kb.mm(S[:, jj * 128:(jj + 1) * 128],
                                  [(kT[:, c, j * 128:(j + 1) * 128], qT[:, c, i * 128:(i + 1) * 128]) for c in range(2)],
                                  reads=[b_kT, b_qT], writes=[bS])
                            kb.op(kb.act, lambda e: e.activation(out=D_[:, jj, :], in_=brow[:, i, :], func=AF.Exp, scale=-1.0, bias=cb[:, j, col:col + 1]),
                                  reads=[b_brow[i], b_cb], writes=[bD])
                            if j == i:
                                kb.op(kb.pool, lambda e: e.tensor_tensor(out=D_[:, jj, :], in0=D_[:, jj, :], in1=self.cm[:, MK[d], :], op=ALU.mult),
                                      reads=[bD, self.b_cm], writes=[bD])
                        kb.op(kb.dve, lambda e: e.tensor_tensor(out=P_[:, 0:npc, :].rearrange("p a b -> p (a b)"), in0=S[:, 0:npc * 128],
                                                               in1=D_[:, 0:npc, :].rearrange("p a b -> p (a b)"), op=ALU.mult),
                              reads=[bS, bD], writes=[bP])
                        first = (gi == 0)
                        last = (gi == ngr - 1)
                        kb._waits(kb.pe, [bP, b_v], [bN, bDn] if first else [])
                        ins = None
                        for jj, j in enumerate(part):
                            kb.pe.e.matmul(N_[:, 0:512], lhsT=P_[:, jj, :], rhs=v[:, j, :], start=(first and jj == 0), stop=(last and jj == npc - 1))
                            ins = kb.pe.e.matmul(Dn[:, 0:1], lhsT=P_[:, jj, :], rhs=onesb, start=(first and jj == 0), stop=(last and jj == npc - 1))
                        tok = kb.pe.tick()
                        ins.then_inc(tok[0].h, 1)
                        kb._mark(tok, [bP, b_v], [bN, bDn])
                    r_, br = rr.next()
                    kb.op(kb.act, lambda e: e.activation(out=r_[:, 0:1], in_=Dn[:, 0:1], func=AF.Abs), reads=[bDn], writes=[br])
                    kb.op(kb.dve, lambda e: e.tensor_scalar_max(out=r_[:, 0:1], in0=r_[:, 0:1], scalar1=1.0), reads=[br], writes=[br])
                    kb.op(kb.dve, lambda e: e.reciprocal(out=r_[:, 1:2], in_=r_[:, 0:1]), reads=[br], writes=[br])
                    if d == 0:
                        kb.op(kb.act, lambda e: e.activation(out=hsum[:, i, :], in_=N_[:, 0:512], func=AF.Copy, scale=r_[:, 1:2]),
                              reads=[bN, br], writes=[b_hs[i]])
                    else:
                        kb.op(kb.dve, lambda e: e.scalar_tensor_tensor(out=hsum[:, i, :], in0=N_[:, 0:512], scalar=r_[:, 1:2], in1=hsum[:, i, :],
                                                                        op0=ALU.mult, op1=ALU.add), reads=[bN, br, b_hs[i]], writes=[b_hs[i]])
            kb.op(kb.dve, lambda e: e.memset(ss[:], 0.0), writes=[b_ss])
            for i in range(NB):
                kb.op(kb.act, lambda e: e.activation(out=junk[:], in_=hsum[:, i, :], func=AF.Square, accum_out=ss[:, i:i + 1]),
                      reads=[b_hs[i]], writes=[b_junk, b_ss])
            kb.op(kb.act, lambda e: e.activation(out=rs[:], in_=ss[:], func=AF.Sqrt, scale=1.0 / 512, bias=EPS), reads=[b_ss], writes=[b_rs])
            kb.op(kb.dve, lambda e: e.reciprocal(out=rs[:], in_=rs[:]), reads=[b_rs], writes=[b_rs])
            for i in range(NB):
                t_, bt = tmpo.next()
                kb.op(kb.dve, lambda e: e.scalar_tensor_tensor(out=t_[:], in0=hsum[:, i, :], scalar=rs[:, i:i + 1], in1=gain[:, h * 512:(h + 1) * 512],
                                                                op0=ALU.mult, op1=ALU.mult), reads=[b_hs[i], b_rs, b_gain], writes=[bt])
                kb.op(kb.pool, lambda e: e.tensor_tensor(out=ost[:, i, :], in0=t_[:], in1=og[:, i, :], op=ALU.mult), reads=[bt, b_og], writes=[b_ost])
            kb.dma(kb.sp, self.mixTM.rearrange("(tb p) c -> p tb c", p=128)[:, :, 2048 + h * 512:2048 + (h + 1) * 512], ost[:], s_ost,
                   reads=[b_ost], writes=[b_mix])
        ph.close()

    def phase_mix_gla(self):
        kb = self.kb
        b_pFM, b_pTM, b_rl, b_mix = (self.sbuf_of(n) for n in ("projFM", "projTM", "rlowFM", "mixTM"))
        ph = Phase(kb, "gla")
        ORD = [list(range(NB)), [1, 0] + list(range(NB - 1, 1, -1))]
        MK = [2, 3]
        raug = ph.sb("raug", [32, T], F32); b_raug = kb.buf("raug"); s_raug = kb.dsem("raug")
        gw = ph.sb("gw", [32, 256], F32); b_gw = kb.buf("gw"); s_gw = kb.dsem("gw")
        sp_ = ph.sb("sp", [128, NB, 256], F32); b_sp = [kb.buf("gsp%d" % i) for i in range(NB)]
        Gp = ph.sb("Gp", [128, 2, T], F32); b_Gp = kb.buf("Gp", multi=True)
        qT = ph.sb("qT", [128, 2, T], BF16); b_qT = kb.buf("gqT", multi=True); s_qT = kb.dsem("gqT")
        kT = ph.sb("kT", [128, 2, T], BF16); b_kT = kb.buf("gkT", multi=True); s_kT = kb.dsem("gkT")
        qd = ph.sb("qd", [128, 2, T], BF16); b_qd = kb.buf("qd", multi=True)
        ke = ph.sb("ke", [128, 2, T], BF16); b_ke = kb.buf("ke", multi=True)
        ki = ph.sb("ki", [128, 2, T], BF16); b_ki = kb.buf("ki", multi=True)
        v = ph.sb("v", [128, NB, 512], BF16); b_v = kb.buf("gv", multi=True); s_v = kb.dsem("gv")
        rg = ph.sb("rg", [128, NB, 512], BF16); b_rg = kb.buf("grg", multi=True); s_rg = kb.dsem("grg")
        hsum = ph.sb("hsum", [128, NB, 512], F32); b_hs = [kb.buf("ghs%d" % i) for i in range(NB)]
        gain = ph.sb("gain", [128, 512], F32); b_gain = kb.buf("ggain"); s_gain = kb.dsem("ggain")
        g16 = ph.sb("g16", [128, 4, 2, NB], F32); b_g16 = kb.buf("g16")
        gam = ph.sb("gam", [128, 2, NB], F32); b_gam = kb.buf("gam")
        gtm = ph.sb("gtm", [128, 2, NB], F32); b_gtm = kb.buf("gtm")
        ss = ph.sb("ss", [128, NB], F32); b_ss = kb.buf("gss")
        rs = ph.sb("rs", [128, NB], F32); b_rs = kb.buf("grs")
        junk = ph.sb("junk", [128, 512], F32); b_junk = kb.buf("gjunk")
        Er = Ring(kb, "gE", [ph.sb("E%d" % i, [128, 128], F32) for i in range(3)])
        PT = Ring(kb, "gPT", [ph.sb("PT%d" % i, [128, 128], BF16) for i in range(3)])
        keTr = Ring(kb, "gkeT", [ph.sb("keT%d" % i, [128, 256], BF16) for i in range(2)])
        S_f = ph.sb("S_f", [128, 2, 512], F32); S_b = ph.sb("S_b", [128, 2, 512], BF16)
        b_Sf = kb.buf("S_f"); b_Sb = kb.buf("S_b")
        gmm = ph.sb("gmm", [128, 2, NB], F32); b_gmm = kb.buf("gmm")
        tot = ph.sb("tot", [128, 2, NB], F32); gsr = ph.sb("gsr", [128, 2, NB], F32); b_tot = kb.buf("tot")
        tmpo = Ring(kb, "gtmpo", [ph.sb("tmpo%d" % i, [128, 512], F32) for i in range(2)])
        ost = Ring(kb, "gost", [ph.sb("ost%d" % i, [128, 512], BF16) for i in range(2)], with_sem=True)
        psZ = Ring(kb, "gZ", [ph.ps("Z%d" % i, [128, 512], F32) for i in range(2)])
        psS = Ring(kb, "gS", [ph.ps("S%d" % i, [128, 512], F32) for i in range(2)])
        psT = Ring(kb, "gT", [ph.ps("T%d" % i, [128, 512], BF16) for i in range(1)])
        psN = Ring(kb, "gN", [ph.ps("N%d" % i, [128, 512], F32) for i in range(2)])
        mixv = self.mixTM.rearrange("(tb p) c -> p tb c", p=128)
        for h in range(8):
            for c in range(2):
                kb.dma(kb.sp, v[:, :, c * 256:(c + 1) * 256], self.projTM[2 * h + c].rearrange("(tb p) c -> p tb c", p=128), s_v, reads=[b_pTM], writes=[b_v])
                kb.dma(kb.sp, rg[:, :, c * 256:(c + 1) * 256], self.projTM[16 + 2 * h + c].rearrange("(tb p) c -> p tb c", p=128), s_rg, reads=[b_pTM], writes=[b_rg])
            kb.op(kb.act, lambda e: e.activation(out=rg[:], in_=rg[:], func=AF.Silu), reads=[b_rg], writes=[b_rg])
            kb.dma(kb.sp, gain[:], self.gla_head_norm[:, h * 512:(h + 1) * 512].partition_broadcast(128), s_gain, writes=[b_gain])
            for d in range(2):
                kb.op(kb.dve, lambda e: e.memset(raug[:], 1.0), writes=[b_raug])
                kb.dma(kb.sp, raug[0:16, :], self.rlowFM[16 * d:16 * d + 16, :], s_raug, reads=[b_rl], writes=[b_raug])
                kb.dma(kb.sp, gw[0:17, :], self.gla_gw2[d][:, h * 256:(h + 1) * 256], s_gw, writes=[b_gw])
                for i in range(NB):
                    Z, bZ = psZ.next()
                    kb.mm(Z[:, 0:256], [(raug[0:17, i * 128:(i + 1) * 128], gw[0:17, :])], reads=[b_raug, b_gw], writes=[bZ])
                    kb.op(kb.act, lambda e: e.activation(out=sp_[:, i, :], in_=Z[:, 0:256], func=AF.Exp, scale=-1.0), reads=[bZ], writes=[b_sp[i]])
                    kb.op(kb.act, lambda e: e.activation(out=sp_[:, i, :], in_=sp_[:, i, :], func=AF.Ln, bias=1.0), reads=[b_sp[i]], writes=[b_sp[i]])
                for c in range(2):
                    Z, bZ = psZ.next()
                    for i in range(NB):
                        kb.mm(Z[:, i:i + 1], [(sp_[:, i, c * 128:(c + 1) * 128], self.cm[:, 1, 0:1])], reads=[b_sp[i], self.b_cm], writes=[bZ])
                    kb.op(kb.dve, lambda e: e.tensor_copy(out=tot[:, c, :], in_=Z[:, 0:NB]), reads=[bZ], writes=[b_tot])
                    prev = None
                    for i in ORD[d]:
                        if prev is None:
                            kb.op(kb.dve, lambda e: e.memset(gsr[:, c, i:i + 1], 0.0), writes=[b_tot])
                        else:
                            kb.op(kb.dve, lambda e: e.tensor_tensor(out=gsr[:, c, i:i + 1], in0=gsr[:, c, prev:prev + 1], in1=tot[:, c, prev:prev + 1], op=ALU.add),
                                  reads=[b_tot], writes=[b_tot])
                        prev = i
                kb.op(kb.dve, lambda e: e.tensor_scalar(out=g16[:, 0, :, :], in0=gsr[:], scalar1=1.0 / 16, scalar2=None, op0=ALU.mult), reads=[b_tot], writes=[b_g16])
                kb.op(kb.dve, lambda e: e.tensor_tensor(out=tot[:], in0=tot[:], in1=gsr[:], op=ALU.add), reads=[b_tot], writes=[b_tot])
                kb.op(kb.dve, lambda e: e.tensor_scalar(out=g16[:, 1, :, :], in0=tot[:], scalar1=1.0 / 16, scalar2=None, op0=ALU.mult), reads=[b_tot], writes=[b_g16])
                for c in range(2):
                    for i in range(NB):
                        Z, bZ = psZ.next()
                        kb.mm(Z[:, 0:128], [(sp_[:, i, c * 128:(c + 1) * 128], self.cm[:, MK[d], :])], reads=[b_sp[i], self.b_cm], writes=[bZ])
                        dst = Gp[:, c, i * 128:(i + 1) * 128]
                        if i % 2 == 0:
                            kb.op(kb.dve, lambda e: e.tensor_scalar(out=dst, in0=Z[:, 0:128], scalar1=gsr[:, c, i:i + 1], scalar2=None, op0=ALU.add),
                                  reads=[bZ, b_tot], writes=[b_Gp])
                        else:
                            kb.op(kb.act, lambda e: e.activation(out=dst, in_=Z[:, 0:128], func=AF.Identity, bias=gsr[:, c, i:i + 1]),
                                  reads=[bZ, b_tot], writes=[b_Gp])
                kb.op(kb.dve, lambda e: e.tensor_scalar(out=g16[:, 2:4, :, :], in0=g16[:, 0:2, :, :], scalar1=-1.0, scalar2=None, op0=ALU.mult),
                      reads=[b_g16], writes=[b_g16])
                kb.op(kb.dve, lambda e: e.tensor_tensor(out=gmm[:], in0=g16[:, 0, :, :], in1=g16[:, 1, :, :], op=ALU.subtract), reads=[b_g16], writes=[b_gmm])
                kb.op(kb.act, lambda e: e.activation(out=gmm[:], in_=gmm[:], func=AF.Exp), reads=[b_gmm], writes=[b_gmm])
                for c in range(2):
                    kb.dma(kb.sp, qT[:, c, :], self.projFM[2 * h + c], s_qT, reads=[b_pFM], writes=[b_qT])
                    kb.dma(kb.sp, kT[:, c, :], self.projFM[16 + 2 * h + c], s_kT, reads=[b_pFM], writes=[b_kT])
                for c in range(2):
                    for i in range(NB):
                        blk = slice(i * 128, (i + 1) * 128)
                        for which, src, dstt, bd, sc_, bias in ((0, qT, qd, b_qd, -1.0 / 16, g16[:, 0, c, i:i + 1]),
                                                               (1, kT, ke, b_ke, 1.0 / 16, g16[:, 3, c, i:i + 1]),
                                                               (2, kT, ki, b_ki, 1.0 / 16, g16[:, 2, c, i:i + 1])):
                            E, bE = Er.next()
                            kb.op(kb.act, lambda e: e.activation(out=E[:], in_=Gp[:, c, blk], func=AF.Exp, scale=sc_, bias=bias),
                                  reads=[b_Gp, b_g16], writes=[bE])
                            kb.op(kb.dve, lambda e: e.tensor_tensor(out=dstt[:, c, blk], in0=src[:, c, blk], in1=E[:], op=ALU.mult),
                                  reads=[bE, b_qT if which == 0 else b_kT], writes=[bd])
                kb.op(kb.dve, lambda e: e.memset(S_f[:], 0.0), writes=[b_Sf])
                for pos, i in enumerate(ORD[d]):
                    blk = slice(i * 128, (i + 1) * 128)
                    S, bS = psS.next()
                    kb.mm(S[:, 0:128], [(ki[:, c, blk], qd[:, c, blk]) for c in range(2)], reads=[b_ki, b_qd], writes=[bS])
                    P_, bP = PT.next()
                    kb.op(kb.dve, lambda e: e.tensor_tensor(out=P_[:], in0=S[:, 0:128], in1=self.cm[:, MK[d], :], op=ALU.mult),
                          reads=[bS, self.b_cm], writes=[bP])
                    N_, bN = psN.next()
                    pairs = [(P_[:], v[:, i, :])]
                    rd = [bP, b_v]
                    if pos > 0:
                        pairs += [(qd[:, c, blk], S_b[:, c, :]) for c in range(2)]
                        rd += [b_qd, b_Sb]
                    kb.mm(N_[:, 0:512], pairs, reads=rd, writes=[bN])
                    if d == 0:
                        kb.op(kb.act, lambda e: e.activation(out=hsum[:, i, :], in_=N_[:, 0:512], func=AF.Copy), reads=[bN], writes=[b_hs[i]])
                    else:
                        kb.op(kb.dve, lambda e: e.tensor_tensor(out=hsum[:, i, :], in0=N_[:, 0:512], in1=hsum[:, i, :], op=ALU.add),
                              reads=[bN, b_hs[i]], writes=[b_hs[i]])
                    if pos < NB - 1:
                        T_, bT = psT.next()
                        for c in range(2):
                            kb.op(kb.pe, lambda e: e.transpose(out=T_[:, c * 128:(c + 1) * 128], in_=ke[:, c, blk], identity=self.cmb[:, 0, :]),
                                  reads=[b_ke, self.b_cm], writes=[bT])
                        keT, bkeT = keTr.next()
                        kb.op(kb.act, lambda e: e.activation(out=keT[:], in_=T_[:, 0:256], func=AF.Copy), reads=[bT], writes=[bkeT])
                        for c in range(2):
                            U, bU = psZ.next()
                            kb.mm(U[:, 0:512], [(keT[:, c * 128:(c + 1) * 128], v[:, i, :])], reads=[bkeT, b_v], writes=[bU])
                            kb.op(kb.dve, lambda e: e.scalar_tensor_tensor(out=S_f[:, c, :], in0=S_f[:, c, :], scalar=gmm[:, c, i:i + 1], in1=U[:, 0:512],
                                                                            op0=ALU.mult, op1=ALU.add), reads=[bU, b_gmm, b_Sf], writes=[b_Sf])
                            kb.op(kb.act, lambda e: e.activation(out=S_b[:, c, :], in_=S_f[:, c, :], func=AF.Copy), reads=[b_Sf], writes=[b_Sb])
            kb.op(kb.dve, lambda e: e.memset(ss[:], 0.0), writes=[b_ss])
            for i in range(NB):
                kb.op(kb.act, lambda e: e.activation(out=junk[:], in_=hsum[:, i, :], func=AF.Square, accum_out=ss[:, i:i + 1]),
                      reads=[b_hs[i]], writes=[b_junk, b_ss])
            kb.op(kb.act, lambda e: e.activation(out=rs[:], in_=ss[:], func=AF.Sqrt, scale=1.0 / 512, bias=EPS), reads=[b_ss], writes=[b_rs])
            kb.op(kb.dve, lambda e: e.reciprocal(out=rs[:], in_=rs[:]), reads=[b_rs], writes=[b_rs])
            for i in range(NB):
                t_, bt = tmpo.next()
                o_, bo, so = ost.next()
                kb.op(kb.dve, lambda e: e.scalar_tensor_tensor(out=t_[:], in0=hsum[:, i, :], scalar=rs[:, i:i + 1], in1=gain[:], op0=ALU.mult, op1=ALU.mult),
                      reads=[b_hs[i], b_rs, b_gain], writes=[bt])
                kb.op(kb.pool, lambda e: e.tensor_tensor(out=o_[:], in0=t_[:], in1=rg[:, i, :], op=ALU.mult), reads=[bt, b_rg], writes=[bo])
                kb.dma(kb.sp, mixv[:, i, h * 512:(h + 1) * 512], o_[:], so, reads=[bo], writes=[b_mix])
        ph.close()

    def phase_outproj(self, l, skip_ctx=False):
        kb = self.kb
        w_dram = self.ab_w_out if l == 0 else self.gla_w_out
        b_mix, b_y = self.sbuf_of("mixTM"), self.sbuf_of("yscr")
        HALVES = [(0, 1152, [(0, 512), (512, 1024), (1024, 1152)]), (1152, 2304, [(1152, 1664), (1664, 2176), (2176, 2304)])]
        if skip_ctx:
            HALVES[0] = (0, 1152, [(256, 768), (768, 1152)])
        for hi_, (lo, hi, tiles) in enumerate(HALVES):
            nh = hi - lo
            tg = "%d_%d" % (l, hi_)
            ph = Phase(kb, "op" + tg)
            mixT = ph.sb("mixT", [128, NDC, nh], BF16)
            b_mT = [kb.buf("mixT%d" % i) for i in range(nh // 128)]
            rowr = Ring(kb, "oprow" + tg, [ph.sb("row%d" % i, [128, D], BF16) for i in range(2)], with_sem=True)
            pst = Ring(kb, "oppt" + tg, [ph.ps("pt%d" % i, [128, 512], BF16) for i in range(2)])
            for tb in range(nh // 128):
                if skip_ctx and lo + tb * 128 < LC:
                    continue
                row, brow_, srow = rowr.next()
                kb.dma(kb.sp, row[:], self.mixTM[lo + tb * 128:lo + (tb + 1) * 128, :], srow, reads=[b_mix], writes=[brow_])
                for c4 in range(8):
                    pt, bpt = pst.next()
                    for k in range(4):
                        c = c4 * 4 + k
                        kb.op(kb.pe, lambda e: e.transpose(out=pt[:, k * 128:(k + 1) * 128], in_=row[:, c * 128:(c + 1) * 128], identity=self.cmb[:, 0, :]),
                              reads=[brow_, self.b_cm], writes=[bpt])
                    dst = mixT[:, c4 * 4:(c4 + 1) * 4, tb * 128:(tb + 1) * 128]
                    src = pt[:, :].rearrange("p (a b) -> p a b", a=4)
                    if c4 % 2 == 0:
                        kb.op(kb.act, lambda e: e.activation(out=dst, in_=src, func=AF.Copy), reads=[bpt], writes=[b_mT[tb]])
                    else:
                        kb.op(kb.dve, lambda e: e.tensor_copy(out=dst, in_=src), reads=[bpt], writes=[b_mT[tb]])
            wr = Ring(kb, "opw" + tg, [ph.sb("w%d" % i, [128, NDC, 128], BF16) for i in range(2)], with_sem=True)
            psr = Ring(kb, "opps" + tg, [ph.ps("p%d" % i, [128, 512], F32) for i in range(6)])
            yst = Ring(kb, "opy" + tg, [ph.sb("y%d" % i, [128, nh], F32) for i in range(2)], with_sem=True, multi=True)
            for dc in range(NDC):
                w, bw, sw = wr.next()
                kb.dma(kb.pool, w[:], w_dram[dc], sw, writes=[bw])
                y, by, sy = yst.next()
                pbs = [psr.next() for _ in tiles]
                kb.mm_multi([pb[0][:, 0:t1 - t0] for pb, (t0, t1) in zip(pbs, tiles)], NDC,
                            lambda kc: w[:, kc, :], lambda kc, ti: mixT[:, kc, tiles[ti][0] - lo:tiles[ti][1] - lo],
                            reads=[bw] + b_mT, writes=[pb[1] for pb in pbs])
                for ti, (t0, t1) in enumerate(tiles):
                    n = t1 - t0
                    p, bp = pbs[ti]
                    if ti % 2 == 0:
                        kb.op(kb.act, lambda e: e.activation(out=y[:, t0 - lo:t1 - lo], in_=p[:, 0:n], func=AF.Copy), reads=[bp], writes=[by])
                    else:
                        kb.op(kb.dve, lambda e: e.tensor_copy(out=y[:, t0 - lo:t1 - lo], in_=p[:, 0:n]), reads=[bp], writes=[by])
                y0 = tiles[0][0]
                kb.dma(kb.sp, self.yscr[dc][:, y0:hi], y[:, y0 - lo:], sy, reads=[by], writes=[b_y])
            ph.close()

    def phase_resid(self, l, kgate, xsrc, b_xsrc, xdst, b_xdst, final=False):
        kb = self.kb
        ph = Phase(kb, "res%d_%d" % (l, kgate))
        b_y = self.sbuf_of("yscr")
        yr = Ring(kb, "ry", [ph.sb("y%d" % i, [128, T], F32) for i in range(2)], with_sem=True)
        xr = Ring(kb, "rx", [ph.sb("x%d" % i, [128, T], F32) for i in range(2)], with_sem=True)
        orr = Ring(kb, "ro", [ph.sb("o%d" % i, [128, T], F32) for i in range(2)], with_sem=True, multi=True)
        acc = ph.sb("acc", [128, T], F32); tmp = ph.sb("tmp", [128, T], F32); rstd = ph.sb("rstd", [128, T], F32)
        b_acc, b_tmp, b_rstd = kb.buf("racc"), kb.buf("rtmp"), kb.buf("rrstd")
        pss = [ph.ps("ps%d" % i, [128, 512], F32) for i in range(2)]
        b_pss = [kb.buf("rps%d" % i) for i in range(2)]
        for dc in range(NDC):
            y, by, sy = yr.next()
            kb.dma(kb.sp, y[:], self.yscr[dc], sy, reads=[b_y], writes=[by])
            if dc == 0:
                kb.op(kb.dve, lambda e: e.tensor_tensor(out=acc[:], in0=y[:], in1=y[:], op=ALU.mult), reads=[by], writes=[b_acc])
            else:
                kb.op(kb.act, lambda e: e.activation(out=tmp[:], in_=y[:], func=AF.Square), reads=[by], writes=[b_tmp])
                kb.op(kb.dve, lambda e: e.tensor_tensor(out=acc[:], in0=acc[:], in1=tmp[:], op=ALU.add), reads=[b_tmp], writes=[b_acc])
        for i, (t0, t1) in enumerate(TT):
            p = pss[i % 2]
            kb.mm(p[:, 0:t1 - t0], [(self.cm[:, 1, :], acc[:, t0:t1])], reads=[self.b_cm, b_acc], writes=[b_pss[i % 2]])
            kb.op(kb.act, lambda e: e.activation(out=tmp[:, t0:t1], in_=p[:, 0:t1 - t0], func=AF.Sqrt, scale=1.0 / D, bias=EPS),
                  reads=[b_pss[i % 2]], writes=[b_tmp])
        kb.op(kb.dve, lambda e: e.reciprocal(out=rstd[:], in_=tmp[:]), reads=[b_tmp], writes=[b_rstd])
        def _load(dcc):
            y_, by_, sy_ = yr.next()
            x_, bx_, sx_ = xr.next()
            kb.dma(kb.sp, y_[:], self.yscr[dcc], sy_, reads=[b_y], writes=[by_])
            kb.dma(kb.sp, x_[:], xsrc[dcc], sx_, reads=[b_xsrc], writes=[bx_])
            return (y_, by_, x_, bx_)
        nxt = _load(0)
        for dc in range(NDC):
            y, by, x, bx = nxt
            if dc + 1 < NDC:
                nxt = _load(dc + 1)
            o, bo, so = orr.next()
            kb.op(kb.dve, lambda e: e.tensor_tensor(out=y[:], in0=y[:], in1=rstd[:], op=ALU.mult), reads=[b_rstd, by], writes=[by])
            for cl, a0, a1 in cls_cols(0, T):
                kb.op(kb.dve, lambda e: e.scalar_tensor_tensor(out=o[:, a0:a1], in0=y[:, a0:a1], scalar=self.der[:, l, kgate, dc, cl:cl + 1],
                                                                in1=x[:, a0:a1], op0=ALU.mult, op1=ALU.add),
                      reads=[by, bx, self.b_der], writes=[bo])
            if final:
                kb.dma(kb.sp, self.out[dc], o[:, LC:T], so, reads=[bo])
            else:
                kb.dma(kb.sp, xdst[dc], o[:], so, reads=[bo], writes=[b_xdst])
        ph.close()

    def phase_ffn_up(self, l, xsrc, b_x, skip_ctx=False):
        kb = self.kb
        b_act = self.sbuf_of("actscr")
        HALVES = [
            (0, 1153, [(0, 256), (256, 768), (768, 1153)], lambda t: (t + 1) if t < LC else (t + 2), 1156, [(1, 257, 0), (258, 1154, 256)]),
            (1151, 2304, [(1151, 1663), (1663, 2175), (2175, 2304)], lambda t: t - 1151, 1154, [(1, 1153, 1152)]),
        ]
        if skip_ctx:
            h0 = HALVES[0]
            HALVES[0] = (h0[0], h0[1], h0[2][1:], h0[3], h0[4], h0[5][1:])
        for hi_, (lo, hi, tiles, colof, ncols, outs) in enumerate(HALVES):
            nh = hi - lo
            tg = "%d_%d" % (l, hi_)
            ph = Phase(kb, "fu" + tg)
            hT = ph.sb("hT", [128, NDC, nh], BF16)
            b_hT = [kb.buf("fhT%d" % i) for i in range(NDC)]
            ph2 = Phase(kb, "fun" + tg)
            self.norm_mod(ph2, xsrc, b_x, l, 3, 4, hT, b_hT, lo, hi)
            ph2.close()
            cw = ph.sb("cw", [128, 3, 2 * NFC], F32); cbt = ph.sb("cb", [128, 2 * NFC], F32)
            b_cw = kb.buf("cw")
            kb.dma(kb.sp, cw[:], self.ffn_cw[l], kb.dsem('m'), writes=[b_cw])
            kb.dma(kb.sp, cbt[:], self.ffn_cb[l], kb.dsem('m'), writes=[b_cw])
            wr = Ring(kb, "fuw" + tg, [ph.sb("w%d" % i, [128, NDC, 128], BF16) for i in range(6)], with_sem=True)
            psr = Ring(kb, "fups" + tg, [ph.ps("p%d" % i, [128, 512], F32) for i in range(6)])
            ur = Ring(kb, "fuu" + tg, [ph.sb("u%d" % i, [128, ncols], F32) for i in range(2)], multi=True)
            cgr = Ring(kb, "fucg" + tg, [ph.sb("cg%d" % i, [128, ncols], F32) for i in range(2)])
            sg = ph.sb("sg", [128, ncols], F32); b_sg = kb.buf("sg")
            ast = Ring(kb, "fua" + tg, [ph.sb("a%d" % i, [128, ncols], BF16) for i in range(2)], with_sem=True)
            for u in ur.tiles:
                kb.op(kb.pool, lambda e: e.memset(u[:], 0.0), writes=[ur.bufs[ur.tiles.index(u)]])
            W_ = ncols - 2
            for c in range(NFC):
                cgs = []
                for kind in range(2):
                    ch = c + kind * NFC
                    w, bw, sw = wr.next()
                    kb.dma(kb.pool, w[:], self.ffn_w_up[l][ch], sw, writes=[bw])
                    u, bu = ur.next()
                    pbs = [psr.next() for _ in tiles]
                    kb.mm_multi([pb[0][:, 0:t1 - t0] for pb, (t0, t1) in zip(pbs, tiles)], NDC,
                                lambda kc: w[:, kc, :], lambda kc, ti: hT[:, kc, tiles[ti][0] - lo:tiles[ti][1] - lo],
                                reads=[bw] + b_hT, writes=[pb[1] for pb in pbs])
                    for ti, (t0, t1) in enumerate(tiles):
                        n = t1 - t0
                        p, bp = pbs[ti]
                        c0 = colof(t0)
                        kb.op(kb.act, lambda e: e.activation(out=u[:, c0:c0 + n], in_=p[:, 0:n], func=AF.Copy), reads=[bp], writes=[bu])
                    cg, bcg = cgr.next()
                    kb.op(kb.dve, lambda e: e.tensor_scalar(out=cg[:, 0:W_], in0=u[:, 1:1 + W_], scalar1=cw[:, 1, ch:ch + 1], scalar2=cbt[:, ch:ch + 1],
                                                           op0=ALU.mult, op1=ALU.add), reads=[bu, b_cw], writes=[bcg])
                    kb.op(kb.dve, lambda e: e.scalar_tensor_tensor(out=cg[:, 0:W_], in0=u[:, 0:W_], scalar=cw[:, 0, ch:ch + 1], in1=cg[:, 0:W_],
                                                                    op0=ALU.mult, op1=ALU.add), reads=[bu, b_cw, bcg], writes=[bcg])
                    kb.op(kb.dve, lambda e: e.scalar_tensor_tensor(out=cg[:, 0:W_], in0=u[:, 2:2 + W_], scalar=cw[:, 2, ch:ch + 1], in1=cg[:, 0:W_],
                                                                    op0=ALU.mult, op1=ALU.add), reads=[bu, b_cw, bcg], writes=[bcg])
                    cgs.append((cg, bcg))
                (cgg, bcgg), (cgv, bcgv) = cgs
                kb.op(kb.act, lambda e: e.activation(out=sg[:, 0:W_], in_=cgg[:, 0:W_], func=AF.Silu), reads=[bcgg], writes=[b_sg])
                a, ba, sa = ast.next()
                kb.op(kb.dve, lambda e: e.tensor_tensor(out=a[:, 0:W_], in0=sg[:, 0:W_], in1=cgv[:, 0:W_], op=ALU.mult), reads=[b_sg, bcgv], writes=[ba])
                for (oc0, oc1, tk0) in outs:
                    kb.dma(kb.sp, self.actscr[c][:, tk0:tk0 + (oc1 - oc0)], a[:, oc0 - 1:oc1 - 1], sa, reads=[ba], writes=[b_act])
            ph.close()

    def phase_ffn_down(self, l, skip_ctx=False):
        kb = self.kb
        b_act, b_y = self.sbuf_of("actscr"), self.sbuf_of("yscr")
        ph = Phase(kb, "fd%d" % l)
        at = ph.sb("at", [128, NFC, 512], BF16); b_at = kb.buf("at", multi=True); s_at = kb.dsem("at")
        wr = Ring(kb, "fdw%d" % l, [ph.sb("w%d" % i, [128, NFC, 128], BF16) for i in range(3)], with_sem=True)
        psr = Ring(kb, "fdps%d" % l, [ph.ps("p%d" % i, [128, 512], F32) for i in range(4)])
        yst = Ring(kb, "fdy%d" % l, [ph.sb("y%d" % i, [128, 512], F32) for i in range(3)], with_sem=True)
        for (t0, t1) in (TT[1:] if skip_ctx else TT):
            n = t1 - t0
            for c0 in range(0, NFC, 8):
                c1 = min(NFC, c0 + 8)
                kb.dma(kb.sp, at[:, c0:c1, 0:n], self.actscr[c0:c1, :, t0:t1].rearrange("c p t -> p c t"), s_at, reads=[b_act], writes=[b_at])
            for dc in range(NDC):
                w, bw, sw = wr.next()
                kb.dma(kb.pool, w[:], self.ffn_w_down[l][dc], sw, writes=[bw])
                p, bp = psr.next()
                kb.mm(p[:, 0:n], [(w[:, kc, :], at[:, kc, 0:n]) for kc in range(NFC)], reads=[bw, b_at], writes=[bp])
                y, by, sy = yst.next()
                if dc % 2 == 0:
                    kb.op(kb.act, lambda e: e.activation(out=y[:, 0:n], in_=p[:, 0:n], func=AF.Copy), reads=[bp], writes=[by])
                else:
                    kb.op(kb.dve, lambda e: e.tensor_copy(out=y[:, 0:n], in_=p[:, 0:n]), reads=[bp], writes=[by])
                kb.dma(kb.sp, self.yscr[dc][:, t0:t1], y[:, 0:n], sy, reads=[by], writes=[b_y])
        ph.close()

    def finish(self):
        kb = self.kb
        kb.barrier()
        SRC = {"projFM": self.projFM, "projTM": self.projTM, "gatesTM": self.gatesTM, "mixTM": self.mixTM,
               "xA": self.xA, "xB": self.xB, "yscr": self.yscr, "actscr": self.actscr, "rlowFM": self.rlowFM}
        for name, shape, dtp in self.dbg:
            if name in SRC:
                kb.dma(kb.sp, self.dbgout[name], SRC[name], self.s_out)
            elif name == "mods":
                kb.dma(kb.sp, self.dbgout[name], self.mods[:], self.s_out)
            elif name == "der":
                kb.dma(kb.sp, self.dbgout[name], self.der[:], self.s_out)
        if self.s_out.v > 0:
            kb.sp.e.wait_ge(self.s_out.h, self.s_out.v)


def _rope_tables():
    rows = LL // 64
    row = np.repeat(np.arange(rows), 64).astype(np.float32)
    col = np.tile(np.arange(64), rows).astype(np.float32)
    inv = (10000.0 ** (-np.arange(32, dtype=np.float32) / 32)).astype(np.float32)
    ar = row[:, None] * inv
    ac = col[:, None] * inv
    C = np.concatenate([np.cos(ar), np.cos(ar), np.cos(ac), np.cos(ac)], axis=1).T
    S = np.concatenate([np.sin(ar), np.sin(ar), np.sin(ac), np.sin(ac)], axis=1).T
    return np.ascontiguousarray(C, np.float32), np.ascontiguousarray(S, np.float32)


def _cmat():
    m = np.zeros((6, 128, 128), np.float32)
    m[0] = np.eye(128)
    m[1] = 1.0
    idx = np.arange(128)
    m[2] = (idx[:, None] <= idx[None, :])
    m[3] = (idx[:, None] >= idx[None, :])
    R = np.zeros((128, 128), np.float32)
    for base in (0, 64):
        for i in range(32):
            R[base + i, base + 32 + i] = -1.0
            R[base + 32 + i, base + i] = 1.0
    m[4] = R.T
    return m


def _grp(w, ng):
    k, n = w.shape
    wp = np.zeros((k, ng * 256), np.float32)
    wp[:, :n] = w
    return np.ascontiguousarray(wp.reshape(NDC, 128, ng, 256).transpose(2, 1, 0, 3))


def _blk(w, kc, nc_):
    return np.ascontiguousarray(w.reshape(kc, 128, nc_, 128).transpose(2, 1, 0, 3))


def prep_shared(inp, names):
    sh = {}
    f = lambda a: np.asarray(a, np.float32)
    if "ada_w" in names: sh["ada_w"] = f(inp["ada_w"])
    if "ada_b" in names: sh["ada_b"] = np.ascontiguousarray(f(inp["ada_b"]).reshape(2, 192, 128).transpose(0, 2, 1))
    if "norm_w" in names: sh["norm_w"] = np.ascontiguousarray(f(inp["norm_w"]).reshape(2, 4, NDC, 128).transpose(0, 3, 1, 2))
    if "ab_w_in" in names: sh["ab_w_in"] = _grp(f(inp["ab_w_in"])[0], AB_G)
    if "ab_gate_b" in names: sh["ab_gate_b"] = f(inp["ab_gate_b"]).reshape(1, 16)
    if "ab_sink" in names: sh["ab_sink"] = f(inp["ab_sink"]).reshape(1, 16)
    if "ab_head_norm" in names: sh["ab_head_norm"] = f(inp["ab_head_norm"]).reshape(1, 2048)
    if "ab_w_out" in names: sh["ab_w_out"] = _blk(f(inp["ab_w_out"])[0], NDC, NDC)
    if "gla_w_in" in names: sh["gla_w_in"] = _grp(f(inp["gla_w_in"])[0], GLA_G)
    if "gla_gw2" in names:
        sh["gla_gw2"] = np.ascontiguousarray(np.concatenate([f(inp["gla_gate_w2"])[0], f(inp["gla_gate_b"])[0][:, None, :]], axis=1))
    if "gla_head_norm" in names: sh["gla_head_norm"] = f(inp["gla_head_norm"]).reshape(1, 4096)
    if "gla_w_out" in names: sh["gla_w_out"] = _blk(f(inp["gla_w_out"])[0], NDC, NDC)
    if "ffn_w_up" in names:
        sh["ffn_w_up"] = np.stack([_blk(f(inp["ffn_w_up"])[l], NDC, 2 * NFC) for l in range(2)])
    if "ffn_cw" in names:
        sh["ffn_cw"] = np.ascontiguousarray(f(inp["ffn_conv_w"]).reshape(2, 3, 2 * NFC, 128).transpose(0, 3, 1, 2))
    if "ffn_cb" in names:
        sh["ffn_cb"] = np.ascontiguousarray(f(inp["ffn_conv_b"]).reshape(2, 2 * NFC, 128).transpose(0, 2, 1))
    if "ffn_w_down" in names:
        sh["ffn_w_down"] = np.stack([_blk(f(inp["ffn_w_down"])[l], NFC, NDC) for l in range(2)])
    C, S = _rope_tables()
    sh["ropeC"], sh["ropeS"], sh["cmat"] = C, S, _cmat()
    return sh


def prep_core(inp, b, sh):
    m = dict(sh)
    xt = np.concatenate([np.asarray(inp["ctx"][b], np.float32), np.asarray(inp["x"][b], np.float32)], axis=0)
    m["xT"] = np.ascontiguousarray(xt.T.reshape(NDC, 128, T))
    cs = np.stack([np.asarray(inp["c"][b], np.float32), np.asarray(inp["c_ctx"], np.float32)], axis=-1)
    m["csT"] = np.ascontiguousarray(cs.reshape(NDC, 128, 2).transpose(1, 0, 2))
    return m


def build_program(level=5, dbg=()):
    nc = bass.Bass("TRN2", target_bir_lowering=False)
    P = Prog(nc, level, dbg)
    kb = P.kb
    with nc.Block():
        P.phase_consts_ada()
        b_x0 = kb.buf("xT")
        b_xA, b_xB = P.sbuf_of("xA"), P.sbuf_of("xB")
        P.phase_inproj(0, P.xT, b_x0)
        if level >= 2:
            P.phase_mix_ab()
        if level >= 3:
            P.phase_outproj(0)
            P.phase_resid(0, 2, P.xT, b_x0, P.xA, b_xA)
        if level >= 4:
            P.phase_ffn_up(0, P.xA, b_xA)
            P.phase_ffn_down(0)
            P.phase_resid(0, 5, P.xA, b_xA, P.xB, b_xB)
        if level >= 5:
            P.phase_inproj(1, P.xB, b_xB)
            P.phase_mix_gla()
            P.phase_outproj(1, skip_ctx=True)
            P.phase_resid(1, 2, P.xB, b_xB, P.xA, b_xA)
            P.phase_ffn_up(1, P.xA, b_xA, skip_ctx=True)
            P.phase_ffn_down(1, skip_ctx=True)
            P.phase_resid(1, 5, P.xA, b_xA, None, None, final=True)
        P.finish()
    return nc, P


_CACHE = {}


def kernel(**inputs):
    ncores = 4
    if "prog" not in _CACHE:
        _CACHE["prog"] = build_program(level=5, dbg=())
    nc, P = _CACHE["prog"]
    names = set(P.in_names)
    sh = prep_shared(inputs, names)
    in_maps = []
    for b in range(ncores):
        m = prep_core(inputs, b, sh)
        in_maps.append({k: m[k] for k in P.in_names})
    res = run_bass_kernel_spmd(nc, in_maps, core_ids=list(range(ncores)))
    outs = []
    for b in range(ncores):
        o = np.asarray(res.results[b]["out"], np.float32).reshape(D, LL)
        outs.append(np.ascontiguousarray(o.T))
    return np.stack(outs, axis=0)
```
